# Optimizing a Trainium2 kernel written in Bass

```python
import jax
import jax.numpy as jnp
from jax import lax
import numpy as np

D_MODEL = 2048
BATCH = 4
SEQ = 2048
DEPTH = 2

CTX_LEN = 256
GRID_W = 64
ROPE_THETA = 10000.0
NORM_EPS = 1e-6
Q_BLOCK = 128
MOD_CHUNKS = 6

MLA_HEADS = 4
MLA_Q_LORA = 512
MLA_KV_LORA = 512
MLA_NOPE = 128
MLA_ROPE = 64
MLA_V = 128
MLA_SCALE = (MLA_NOPE + MLA_ROPE) ** -0.5

GQA_HEADS = 4
GQA_KV_HEADS = 2
GQA_HEAD_DIM = 128
GQA_SCALE = GQA_HEAD_DIM ** -0.5

NA_HEADS = 4
NA_HEAD_DIM = 128
NA_WIN_H_MAX = 8
NA_WIN_W = 16
NA_SCALE = NA_HEAD_DIM ** -0.5

CONV_CH = 512
CONV_WIDTH = 3

BRANCH_WIDTH = 512
N_BRANCHES = 4

A_COLS = MLA_Q_LORA + MLA_KV_LORA + MLA_ROPE
B_COLS = (GQA_HEADS + 2 * GQA_KV_HEADS) * GQA_HEAD_DIM
C_COLS = 3 * NA_HEADS * NA_HEAD_DIM
D_COLS = 3 * CONV_CH
G_COLS = N_BRANCHES * D_MODEL
IN_COLS = A_COLS + B_COLS + C_COLS + D_COLS + G_COLS
IN_CUTS = [A_COLS, A_COLS + B_COLS, A_COLS + B_COLS + C_COLS, A_COLS + B_COLS + C_COLS + D_COLS]

N_GROUPS = 8
EXPERTS_PER_GROUP = 8
N_EXPERTS = N_GROUPS * EXPERTS_PER_GROUP
TOP_K = 2
EXPERT_HIDDEN = 512
MOE_BLOCK = 128

kernel_name = 'hybrid_parallel_dit_ctx_prefix'


def rms_norm(x, g):
    xf = x.astype(jnp.float32)
    y = xf * lax.rsqrt(jnp.mean(xf * xf, axis=-1, keepdims=True) + NORM_EPS)
    return (y * g.astype(jnp.float32)).astype(x.dtype)


def modulate(h, shift, scale):
    return h * (1 + scale) + shift


def axial_rope_tables(n_tokens, rot_dim):
    quarter = rot_dim // 4
    t = jnp.arange(n_tokens)
    rows = (t // GRID_W).astype(jnp.float32)
    cols = (t % GRID_W).astype(jnp.float32)
    inv_freq = ROPE_THETA ** (-jnp.arange(quarter, dtype=jnp.float32) / quarter)
    ang_r = rows[:, None] * inv_freq[None, :]
    ang_c = cols[:, None] * inv_freq[None, :]
    return (jnp.cos(ang_r), jnp.sin(ang_r), jnp.cos(ang_c), jnp.sin(ang_c))


def _rotate_half(x, cos, sin):
    x1, x2 = jnp.split(x, 2, axis=-1)
    return jnp.concatenate([x1 * cos - x2 * sin, x1 * sin + x2 * cos], axis=-1)


def apply_axial_rope(x, tables):
    cos_r, sin_r, cos_c, sin_c = [t[:, None, :] for t in tables]
    xr, xc = jnp.split(x.astype(jnp.float32), 2, axis=-1)
    out = jnp.concatenate([_rotate_half(xr, cos_r, sin_r), _rotate_half(xc, cos_c, sin_c)], axis=-1)
    return out.astype(x.dtype)


def attention_core(q, k, v, scale):
    s = jnp.einsum('bqgrd,bkgd->bgrqk', q, k).astype(jnp.float32) * scale
    p = jax.nn.softmax(s, axis=-1).astype(v.dtype)
    return jnp.einsum('bgrqk,bkgd->bqgrd', p, v)


def blocked_attention(q, k, v, scale):
    bsz, n = q.shape[:2]
    nb = n // Q_BLOCK
    qb = jnp.moveaxis(q.reshape((bsz, nb, Q_BLOCK) + q.shape[2:]), 1, 0)
    ob = lax.map(lambda qi: attention_core(qi, k, v, scale), qb)
    return jnp.moveaxis(ob, 0, 1).reshape((bsz, n) + ob.shape[3:])


def mla_project(p, w_uq, g_qa, w_ukv, g_kva, rope):
    bsz, n = p.shape[:2]
    c_q, c_kv, k_pe = jnp.split(p, [MLA_Q_LORA, MLA_Q_LORA + MLA_KV_LORA], axis=-1)
    q = (rms_norm(c_q, g_qa) @ w_uq).reshape(bsz, n, MLA_HEADS, MLA_NOPE + MLA_ROPE)
    kv = (rms_norm(c_kv, g_kva) @ w_ukv).reshape(bsz, n, MLA_HEADS, MLA_NOPE + MLA_V)
    q_nope, q_pe = jnp.split(q, [MLA_NOPE], axis=-1)
    k_nope, v = jnp.split(kv, [MLA_NOPE], axis=-1)
    k_pe = k_pe[:, :, None, :]
    if rope is not None:
        q_pe = apply_axial_rope(q_pe, rope)
        k_pe = apply_axial_rope(k_pe, rope)
    q = jnp.concatenate([q_nope, q_pe], axis=-1)[:, :, :, None, :]
    k = jnp.concatenate([k_nope, jnp.broadcast_to(k_pe, (bsz, n, MLA_HEADS, MLA_ROPE))], axis=-1)
    return q, k, v


def gqa_project(p, g_qn, g_kn, rope):
    bsz, n = p.shape[:2]
    q, k, v = jnp.split(p, [GQA_HEADS * GQA_HEAD_DIM, (GQA_HEADS + GQA_KV_HEADS) * GQA_HEAD_DIM], axis=-1)
    q = rms_norm(q.reshape(bsz, n, GQA_HEADS, GQA_HEAD_DIM), g_qn)
    k = rms_norm(k.reshape(bsz, n, GQA_KV_HEADS, GQA_HEAD_DIM), g_kn)
    v = v.reshape(bsz, n, GQA_KV_HEADS, GQA_HEAD_DIM)
    if rope is not None:
        q = apply_axial_rope(q, rope)
        k = apply_axial_rope(k, rope)
    q = q.reshape(bsz, n, GQA_KV_HEADS, GQA_HEADS // GQA_KV_HEADS, GQA_HEAD_DIM)
    return q, k, v


def na_project(p):
    bsz, n = p.shape[:2]
    q, k, v = jnp.split(p, 3, axis=-1)
    shape = (bsz, n, NA_HEADS, NA_HEAD_DIM)
    return q.reshape(shape), k.reshape(shape), v.reshape(shape)


def neighbourhood_attention(q, k, v, k_ctx, v_ctx, rpb):
    bsz, n, heads, hd = q.shape
    rows = n // GRID_W
    win_h = min(NA_WIN_H_MAX, rows)
    band = win_h * GRID_W
    qg = jnp.moveaxis(q.reshape(bsz, rows, GRID_W, heads, hd), 1, 0)
    kg = k.reshape(bsz, rows, GRID_W, heads, hd)
    vg = v.reshape(bsz, rows, GRID_W, heads, hd)
    cols = jnp.arange(GRID_W)
    col_start = jnp.clip(cols - NA_WIN_W // 2, 0, GRID_W - NA_WIN_W)
    col_in = (cols[None, :] >= col_start[:, None]) & (cols[None, :] < col_start[:, None] + NA_WIN_W)
    dc_idx = jnp.clip(cols[None, :] - cols[:, None] + NA_WIN_W - 1, 0, 2 * NA_WIN_W - 2)
    rpb_cols = rpb.astype(jnp.float32)[:, :, dc_idx]

    def one_row(args):
        r, q_row = args
        r0 = jnp.clip(r - win_h // 2, 0, rows - win_h)
        k_band = lax.dynamic_slice_in_dim(kg, r0, win_h, axis=1)
        v_band = lax.dynamic_slice_in_dim(vg, r0, win_h, axis=1)
        dr_idx = r0 - r + jnp.arange(win_h) + NA_WIN_H_MAX - 1
        bias = jnp.transpose(jnp.take(rpb_cols, dr_idx, axis=1), (0, 2, 1, 3))
        bias = jnp.where(col_in[None, :, None, :], bias, -jnp.inf)
        s_loc = jnp.einsum('bjhd,buvhd->bhjuv', q_row, k_band).astype(jnp.float32) * NA_SCALE + bias
        s_ctx = jnp.einsum('bjhd,blhd->bhjl', q_row, k_ctx).astype(jnp.float32) * NA_SCALE
        s = jnp.concatenate([s_loc.reshape(bsz, heads, GRID_W, band), s_ctx], axis=-1)
        p = jax.nn.softmax(s, axis=-1).astype(v.dtype)
        p_loc = p[..., :band].reshape(bsz, heads, GRID_W, win_h, GRID_W)
        return (jnp.einsum('bhjuv,buvhd->bjhd', p_loc, v_band)
                + jnp.einsum('bhjl,blhd->bjhd', p[..., band:], v_ctx))

    out = lax.map(one_row, (jnp.arange(rows), qg))
    return jnp.moveaxis(out, 0, 1).reshape(bsz, n, heads * hd)


def short_conv(p, w_conv):
    b_gate, c_gate, u = jnp.split(p, 3, axis=-1)
    n = p.shape[1]
    z = jnp.pad(c_gate * u, ((0, 0), (CONV_WIDTH // 2, CONV_WIDTH // 2), (0, 0)))
    conv = sum(z[:, i:i + n] * w_conv[i] for i in range(CONV_WIDTH))
    return b_gate * conv


def merge_branches(ys, gate_logits, w_branch, w_o):
    y = jnp.stack(ys, axis=-2)
    proj = jnp.einsum('bnkc,kcd->bnkd', y, w_branch)
    gates = jax.nn.sigmoid(gate_logits.astype(jnp.float32)).astype(y.dtype)
    gates = gates.reshape(gates.shape[:-1] + (N_BRANCHES, D_MODEL))
    return jnp.sum(gates * proj, axis=-2) @ w_o


def token_mixers(h_lat, h_ctx, w_in, w_uq, g_qa, w_ukv, g_kva, g_qn, g_kn, rpb, w_conv, w_branch, w_o,
                 rope_mla, rope_gqa, with_ctx_out):
    bsz, n, _ = h_lat.shape
    n_ctx = h_ctx.shape[1]
    a_l, b_l, c_l, d_l, g_l = jnp.split(h_lat @ w_in, IN_CUTS, axis=-1)
    a_c, b_c, c_c, d_c, g_c = jnp.split(h_ctx @ w_in, IN_CUTS, axis=-1)

    qa_l, ka_l, va_l = mla_project(a_l, w_uq, g_qa, w_ukv, g_kva, rope_mla)
    qa_c, ka_c, va_c = mla_project(a_c, w_uq, g_qa, w_ukv, g_kva, None)
    ya_l = blocked_attention(qa_l, jnp.concatenate([ka_c, ka_l], 1), jnp.concatenate([va_c, va_l], 1),
                             MLA_SCALE).reshape(bsz, n, BRANCH_WIDTH)

    qb_l, kb_l, vb_l = gqa_project(b_l, g_qn, g_kn, rope_gqa)
    qb_c, kb_c, vb_c = gqa_project(b_c, g_qn, g_kn, None)
    yb_l = blocked_attention(qb_l, jnp.concatenate([kb_c, kb_l], 1), jnp.concatenate([vb_c, vb_l], 1),
                             GQA_SCALE).reshape(bsz, n, BRANCH_WIDTH)

    qc_l, kc_l, vc_l = na_project(c_l)
    qc_c, kc_c, vc_c = na_project(c_c)
    yc_l = neighbourhood_attention(qc_l, kc_l, vc_l, kc_c, vc_c, rpb)

    yd_l = short_conv(d_l, w_conv)

    out_lat = merge_branches([ya_l, yb_l, yc_l, yd_l], g_l, w_branch, w_o)
    if not with_ctx_out:
        return out_lat, None
    ya_c = attention_core(qa_c, ka_c, va_c, MLA_SCALE).reshape(bsz, n_ctx, BRANCH_WIDTH)
    yb_c = attention_core(qb_c, kb_c, vb_c, GQA_SCALE).reshape(bsz, n_ctx, BRANCH_WIDTH)
    yc_c = attention_core(qc_c[:, :, :, None, :], kc_c, vc_c, NA_SCALE).reshape(bsz, n_ctx, BRANCH_WIDTH)
    yd_c = short_conv(d_c, w_conv)
    out_ctx = merge_branches([ya_c, yb_c, yc_c, yd_c], g_c, w_branch, w_o)
    return out_lat, out_ctx


def hierarchical_moe(x, w_group, b_group, w_router, b_router, w_gate_e, w_up_e, w_down_e):
    n_tok = x.shape[0]
    g_prob = jax.nn.softmax((x @ w_group).astype(jnp.float32), axis=-1)
    g_sel = jnp.argmax(g_prob + b_group.astype(jnp.float32)[None, :], axis=-1)
    p_group = jnp.take_along_axis(g_prob, g_sel[:, None], axis=-1)
    e_logits = (x @ w_router).astype(jnp.float32).reshape(n_tok, N_GROUPS, EXPERTS_PER_GROUP)
    e_logits = jnp.take_along_axis(e_logits, g_sel[:, None, None], axis=1)[:, 0]
    e_prob = jax.nn.softmax(e_logits, axis=-1)
    e_bias = b_router.astype(jnp.float32).reshape(N_GROUPS, EXPERTS_PER_GROUP)[g_sel]
    _, e_local = lax.top_k(e_prob + e_bias, TOP_K)
    e_top = jnp.take_along_axis(e_prob, e_local, axis=-1)
    weights = p_group * e_top / jnp.sum(e_top, axis=-1, keepdims=True)
    expert_id = g_sel[:, None] * EXPERTS_PER_GROUP + e_local

    n_assign = n_tok * TOP_K
    flat_e = expert_id.reshape(-1)
    flat_t = jnp.repeat(jnp.arange(n_tok, dtype=jnp.int32), TOP_K)
    flat_w = weights.reshape(-1).astype(x.dtype)
    order = jnp.argsort(flat_e)
    se = flat_e[order]
    counts = jnp.bincount(flat_e, length=N_EXPERTS)
    padded = (counts + MOE_BLOCK - 1) // MOE_BLOCK * MOE_BLOCK
    pad_end = jnp.cumsum(padded)
    pad_start = pad_end - padded
    start = jnp.cumsum(counts) - counts
    dest = pad_start[se] + jnp.arange(n_assign) - start[se]
    n_blocks = (n_assign + N_EXPERTS * (MOE_BLOCK - 1)) // MOE_BLOCK
    n_rows = n_blocks * MOE_BLOCK
    tok_buf = jnp.full((n_rows,), n_tok, jnp.int32).at[dest].set(flat_t[order])
    w_buf = jnp.zeros((n_rows,), x.dtype).at[dest].set(flat_w[order])
    block_expert = jnp.clip(jnp.searchsorted(pad_end, jnp.arange(n_blocks) * MOE_BLOCK, side='right'),
                            0, N_EXPERTS - 1)
    x_pad = jnp.concatenate([x, jnp.zeros((1, x.shape[1]), x.dtype)], axis=0)
    xs = x_pad[tok_buf].reshape(n_blocks, MOE_BLOCK, x.shape[1])

    def expert_block(args):
        xb, e = args
        hid = jax.nn.silu(xb @ w_gate_e[e]) * (xb @ w_up_e[e])
        return hid @ w_down_e[e]

    ys = lax.map(expert_block, (xs, block_expert)).reshape(n_rows, x.shape[1])
    out = jnp.zeros((n_tok + 1, x.shape[1]), x.dtype).at[tok_buf].add(ys * w_buf[:, None])
    return out[:n_tok]


def setup_inputs(seed: int = 0) -> dict:
    key = jax.random.key(seed)
    ks = jax.random.split(key, 32)

    def nrm(k, shape, scale):
        return jax.random.normal(k, shape, jnp.float32) * scale

    def gain(k, shape):
        return 1.0 + 0.02 * jax.random.normal(k, shape, jnp.float32)

    d = D_MODEL
    return {
        'x': nrm(ks[0], (BATCH, SEQ, d), 1.0),
        'c': nrm(ks[1], (BATCH, d), 1.0),
        'ctx': nrm(ks[2], (BATCH, CTX_LEN, d), 1.0),
        'c_ctx': nrm(ks[3], (d,), 1.0),
        'w_mod': nrm(ks[4], (DEPTH, d, MOD_CHUNKS * d), 0.5 * d ** -0.5),
        'b_mod': nrm(ks[5], (DEPTH, MOD_CHUNKS * d), 0.01),
        'g_mix': gain(ks[6], (DEPTH, d)),
        'g_ffn': gain(ks[7], (DEPTH, d)),
        'w_in': nrm(ks[8], (DEPTH, d, IN_COLS), d ** -0.5),
        'w_uq': nrm(ks[9], (DEPTH, MLA_Q_LORA, MLA_HEADS * (MLA_NOPE + MLA_ROPE)), MLA_Q_LORA ** -0.5),
        'g_qa': gain(ks[10], (DEPTH, MLA_Q_LORA)),
        'w_ukv': nrm(ks[11], (DEPTH, MLA_KV_LORA, MLA_HEADS * (MLA_NOPE + MLA_V)), MLA_KV_LORA ** -0.5),
        'g_kva': gain(ks[12], (DEPTH, MLA_KV_LORA)),
        'g_qn': gain(ks[13], (DEPTH, GQA_HEAD_DIM)),
        'g_kn': gain(ks[14], (DEPTH, GQA_HEAD_DIM)),
        'rpb': nrm(ks[15], (DEPTH, NA_HEADS, 2 * NA_WIN_H_MAX - 1, 2 * NA_WIN_W - 1), 0.1),
        'w_conv': nrm(ks[16], (DEPTH, CONV_WIDTH, CONV_CH), CONV_WIDTH ** -0.5),
        'w_branch': nrm(ks[17], (DEPTH, N_BRANCHES, BRANCH_WIDTH, d), BRANCH_WIDTH ** -0.5),
        'w_o': nrm(ks[18], (DEPTH, d, d), d ** -0.5),
        'w_group': nrm(ks[19], (DEPTH, d, N_GROUPS), d ** -0.5),
        'b_group': nrm(ks[20], (DEPTH, N_GROUPS), 0.01),
        'w_router': nrm(ks[21], (DEPTH, d, N_EXPERTS), d ** -0.5),
        'b_router': nrm(ks[22], (DEPTH, N_EXPERTS), 0.01),
        'w_gate_e': nrm(ks[23], (DEPTH, N_EXPERTS, d, EXPERT_HIDDEN), d ** -0.5),
        'w_up_e': nrm(ks[24], (DEPTH, N_EXPERTS, d, EXPERT_HIDDEN), d ** -0.5),
        'w_down_e': nrm(ks[25], (DEPTH, N_EXPERTS, EXPERT_HIDDEN, d), EXPERT_HIDDEN ** -0.5),
        'g_final': gain(ks[26], (d,)),
    }


def reference(x, c, ctx, c_ctx, w_mod, b_mod, g_mix, g_ffn, w_in, w_uq, g_qa, w_ukv, g_kva, g_qn, g_kn,
              rpb, w_conv, w_branch, w_o, w_group, b_group, w_router, b_router, w_gate_e, w_up_e, w_down_e,
              g_final):
    bsz, seq, _ = x.shape
    rope_mla = axial_rope_tables(seq, MLA_ROPE)
    rope_gqa = axial_rope_tables(seq, GQA_HEAD_DIM)
    s_c = jax.nn.silu(c)
    s_cc = jax.nn.silu(c_ctx)
    x_lat, x_ctx = x, ctx
    n_lat = bsz * seq
    for l in range(DEPTH):
        last = l == DEPTH - 1
        sh1, sc1, ga1, sh2, sc2, ga2 = jnp.split((s_c @ w_mod[l] + b_mod[l])[:, None, :], MOD_CHUNKS, axis=-1)
        csh1, csc1, cga1, csh2, csc2, cga2 = jnp.split((s_cc @ w_mod[l] + b_mod[l])[None, None, :],
                                                      MOD_CHUNKS, axis=-1)
        h_lat = modulate(rms_norm(x_lat, g_mix[l]), sh1, sc1)
        h_ctx = modulate(rms_norm(x_ctx, g_mix[l]), csh1, csc1)
        o_lat, o_ctx = token_mixers(h_lat, h_ctx, w_in[l], w_uq[l], g_qa[l], w_ukv[l], g_kva[l], g_qn[l],
                                    g_kn[l], rpb[l], w_conv[l], w_branch[l], w_o[l], rope_mla, rope_gqa,
                                    not last)
        x_lat = x_lat + ga1 * o_lat
        h2_lat = modulate(rms_norm(x_lat, g_ffn[l]), sh2, sc2)
        moe_w = (w_group[l], b_group[l], w_router[l], b_router[l], w_gate_e[l], w_up_e[l], w_down_e[l])
        if last:
            y = hierarchical_moe(h2_lat.reshape(n_lat, D_MODEL), *moe_w)
            x_lat = x_lat + ga2 * y.reshape(x_lat.shape)
        else:
            x_ctx = x_ctx + cga1 * o_ctx
            h2_ctx = modulate(rms_norm(x_ctx, g_ffn[l]), csh2, csc2)
            tokens = jnp.concatenate([h2_lat.reshape(n_lat, D_MODEL), h2_ctx.reshape(-1, D_MODEL)], axis=0)
            y = hierarchical_moe(tokens, *moe_w)
            x_lat = x_lat + ga2 * y[:n_lat].reshape(x_lat.shape)
            x_ctx = x_ctx + cga2 * y[n_lat:].reshape(x_ctx.shape)
    return rms_norm(x_lat, g_final)
```

```python
import numpy as np
import concourse.bass as bass
import concourse.mybir as mybir
from concourse.bass_utils import run_bass_kernel_spmd

F32 = mybir.dt.float32
BF16 = mybir.dt.bfloat16
I32 = mybir.dt.int32
U32 = mybir.dt.uint32
AF = mybir.ActivationFunctionType
ALU = mybir.AluOpType
AX = mybir.AxisListType

D = 2048
KC = 16
NCORES = 8
EPS = 1e-6
IN_COLS = 13376
CA, CB, CC, CD, CG = 0, 1088, 2112, 3648, 5184


class Buf:
    def __init__(self, name, t):
        self.name = name
        self.t = t
        self.w = None
        self.r = []
        self.sem = None
        self.cnt = 0


class Prog:
    def __init__(self, nc):
        self.nc = nc
        self.eng = {"pe": nc.tensor, "act": nc.scalar, "dve": nc.vector, "pool": nc.gpsimd, "sp": nc.sync}
        self.sem = {k: nc.alloc_semaphore("sem_" + k) for k in ("pe", "act", "dve", "pool")}
        self.cnt = {k: 0 for k in ("pe", "act", "dve", "pool")}
        self.seen = {k: {} for k in self.eng}
        self.pend = {k: ([], []) for k in self.eng}
        self.dma_bufs = []
        self.nbuf = 0

    def sb(self, name, shape, dt):
        return Buf(name, self.nc.alloc_sbuf_tensor(name, list(shape), dt))

    def ps(self, name, shape, dt):
        return Buf(name, self.nc.alloc_psum_tensor(name, list(shape), dt))

    def dram(self, name, shape, dt, kind="Internal"):
        return Buf(name, self.nc.dram_tensor(name, list(shape), dt, kind=kind).ap())

    def _wait(self, ek, evs):
        e = self.eng[ek]
        need = {}
        for ev in evs:
            if ev is None:
                continue
            sk, sem, val, src = ev
            if src == "pe" and ek == "pe":
                continue
            if self.seen[ek].get(sk, 0) >= val:
                continue
            if sk not in need or need[sk][1] < val:
                need[sk] = (sem, val)
        for sk, (sem, val) in need.items():
            e.wait_ge(sem, val)
            self.seen[ek][sk] = val

    def op(self, ek, fn, R=(), W=(), inc=True):
        evs = []
        for b in R:
            evs.append(b.w)
        for b in W:
            evs.append(b.w)
            evs.extend(b.r)
        self._wait(ek, evs)
        ins = fn(self.eng[ek])
        pr, pw = self.pend[ek]
        pr.extend(R)
        pw.extend(W)
        if not inc:
            return
        self.cnt[ek] += 1
        ev = ("e_" + ek, self.sem[ek], self.cnt[ek], ek)
        ins.then_inc(self.sem[ek], 1)
        for b in pr:
            b.r.append(ev)
        for b in pw:
            b.w = ev
            b.r = []
        self.pend[ek] = ([], [])

    def dma(self, q, fn, R, W, sb):
        if sb.sem is None:
            sb.sem = self.nc.alloc_semaphore("dsem_%d" % len(self.dma_bufs))
            sb.semkey = "d_%d" % len(self.dma_bufs)
            self.dma_bufs.append(sb)
        evs = []
        for b in R:
            evs.append(b.w)
        for b in W:
            evs.append(b.w)
            evs.extend(b.r)
        if sb.cnt > 0:
            evs.append((sb.semkey, sb.sem, sb.cnt, "dma"))
        self._wait(q, evs)
        ins = fn(self.eng[q])
        sb.cnt += 16
        ins.then_inc(sb.sem, 16)
        ev = (sb.semkey, sb.sem, sb.cnt, "dma")
        for b in R:
            b.r.append(ev)
        for b in W:
            b.w = ev
            b.r = []

    def finish(self):
        evs = [(b.semkey, b.sem, b.cnt, "dma") for b in self.dma_bufs]
        self._wait("sp", evs)


def bcast_rows(ap_row, nparts):
    return ap_row.partition_broadcast(nparts)


MCOL = 12288 // NCORES


def build_M():
    nc = bass.Bass("TRN2", target_bir_lowering=False)
    P = Prog(nc)
    cT = P.dram("cT", [128, KC, 5], F32, "ExternalInput")
    wm = P.dram("wm", [2, D, MCOL], F32, "ExternalInput")
    bm = P.dram("bm", [2, MCOL], F32, "ExternalInput")
    out = P.dram("mod", [2, 5, MCOL], F32, "ExternalOutput")
    c_sb = P.sb("c_sb", [128, KC, 5], F32)
    s_sb = P.sb("s_sb", [128, KC, 5], F32)
    wts = [P.sb("wt%d" % i, [128, KC, 512], F32) for i in range(2)]
    b_sb = P.sb("b_sb", [5, 2, MCOL], F32)
    o_sb = P.sb("o_sb", [5, 2, MCOL], F32)
    pss = [P.ps("ps%d" % i, [128, 512], F32) for i in range(2)]
    P.dma("sp", lambda e: e.dma_start(out=c_sb.t[:], in_=cT.t[:, :, :]), [cT], [c_sb], c_sb)
    for l in range(2):
        P.dma("sp", lambda e, l=l: e.dma_start(out=b_sb.t[:, l, :], in_=bm.t[l:l + 1, :].partition_broadcast(5)),
              [bm], [b_sb], b_sb)
    P.op("act", lambda e: e.activation(out=s_sb.t[:], in_=c_sb.t[:], func=AF.Sigmoid), [c_sb], [s_sb])
    P.op("dve", lambda e: e.tensor_tensor(out=s_sb.t[:], in0=s_sb.t[:], in1=c_sb.t[:], op=ALU.mult), [s_sb, c_sb], [s_sb])
    i = 0
    for l in range(2):
        for g in range(MCOL // 512):
            wt = wts[i % 2]
            ps = pss[i % 2]
            i += 1
            P.dma("sp", lambda e, wt=wt, l=l, g=g: e.dma_start(
                out=wt.t[:], in_=wm.t[l, :, g * 512:(g + 1) * 512].rearrange("(kc p) n -> p kc n", p=128)),
                [wm], [wt], wt)
            for kc in range(KC):
                P.op("pe", lambda e, wt=wt, ps=ps, kc=kc: e.matmul(
                    ps.t[0:5, :], lhsT=s_sb.t[:, kc, :], rhs=wt.t[:, kc, :], start=(kc == 0), stop=(kc == KC - 1)),
                    [s_sb, wt], [ps], inc=(kc == KC - 1))
            P.op("dve", lambda e, ps=ps, l=l, g=g: e.tensor_tensor(
                out=o_sb.t[:, l, g * 512:(g + 1) * 512], in0=ps.t[0:5, :], in1=b_sb.t[:, l, g * 512:(g + 1) * 512],
                op=ALU.add), [ps, b_sb], [o_sb])
    for l in range(2):
        P.dma("sp", lambda e, l=l: e.dma_start(out=out.t[l, :, :], in_=o_sb.t[:, l, :]), [o_sb], [out], o_sb)
    P.finish()
    return nc


NOWN = 1152
ARENA_N = 37376
DEBUG_Y = False
FM_GMIX, FM_SC1, FM_SH1, FM_GQA, FM_GKVA, FM_GQN, FM_GKN, FM_WCONV, FM_FLAGB, FM_FLAGA, FM_N = 0, 16, 48, 80, 84, 88, 89, 90, 102, 103, 104
ROW_GFFN, ROW_SC2, ROW_SH2, ROW_GA1 = 0, 1, 3, 5
MLA_SCALE = 192 ** -0.5
HD_SCALE = 128 ** -0.5
OWN_CHUNKS = [(0, 128), (128, 512), (640, 512)]


def build_A():
    nc = bass.Bass("TRN2", target_bir_lowering=False)
    P = Prog(nc)
    x_own = P.dram("x_own", [NOWN, D], F32, "ExternalInput")
    x_oth = P.dram("x_oth", [NOWN, D], F32, "ExternalInput")
    w_in = P.dram("w_in", [D, IN_COLS], F32, "ExternalInput")
    w_uq = P.dram("w_uq", [512, 768], F32, "ExternalInput")
    w_ukv = P.dram("w_ukv", [512, 1024], F32, "ExternalInput")
    fm_d = P.dram("fm", [128, FM_N], F32, "ExternalInput")
    rows_d = P.dram("rows", [7, D], F32, "ExternalInput")
    ropeA = P.dram("ropeA", [64, 2, 2, 1024], F32, "ExternalInput")
    ropeB = P.dram("ropeB", [128, 2, 2, 1024], F32, "ExternalInput")
    consts_d = P.dram("consts", [128, 4, 128], BF16, "ExternalInput")
    nab_d = P.dram("nab", [128, 4, 15, 64], F32, "ExternalInput")
    ind_d = P.dram("ind", [128, 16, 6], F32, "ExternalInput")
    wr_d = P.dram("wr", [D, 72], F32, "ExternalInput")
    brow_d = P.dram("brow", [1, 72], F32, "ExternalInput")
    w_br = P.dram("w_branch", [4, 512, D], F32, "ExternalInput")
    w_o = P.dram("w_o", [D, D], F32, "ExternalInput")
    x1_o = P.dram("x1", [NOWN, D], F32, "ExternalOutput")
    h2_o = P.dram("h2", [NOWN, D], BF16, "ExternalOutput")
    wr_o = P.dram("wrout", [NOWN, 64], F32, "ExternalOutput")

    arena2 = nc.alloc_sbuf_tensor("arena2", [128, 8 * 4 * NOWN], BF16)
    hT = Buf("hT", arena2[:, 0:KC * NOWN].rearrange("p (a b) -> p a b", b=NOWN))
    yT = [Buf("yT%d" % k, arena2[:, (KC + 4 * k) * NOWN:(KC + 4 * k + 4) * NOWN].rearrange("p (a b) -> p a b", b=NOWN)) for k in range(4)]
    arena = nc.alloc_sbuf_tensor("arena", [128, ARENA_N], BF16)
    wb = [P.sb("wb%d" % i, [128, KC, 256], BF16) for i in range(2)]
    consts = P.sb("consts_sb", [128, 4, 128], BF16)
    ident, ones, RBt, RAt = (consts.t[:, i, :] for i in range(4))
    fm = P.sb("fm_sb", [128, FM_N], F32)
    a1T = P.sb("a1T", [128, 32], F32)
    ind = P.sb("ind_sb", [128, 16, 6], F32)
    xt = [P.sb("xt%d" % i, [128, D], F32) for i in range(1)]
    xs = [P.sb("xs%d" % i, [128, D], BF16) for i in range(1)]
    st = [P.sb("st%d" % i, [128, 4], F32) for i in range(2)]
    ck = P.sb("ck", [128, 4, 512], BF16)
    sq = P.sb("sq", [128, 4, 512], BF16)
    rr = P.sb("rr", [128, 512], F32)
    tA = P.sb("tA", [128, 512], F32)
    tB = P.sb("tB", [128, 512], F32)
    cs = [P.sb("cs%d" % i, [128, 2, 512], F32) for i in range(1)]
    Pt = [P.sb("Pt%d" % i, [128, 512], BF16) for i in range(2)]
    psM = [P.ps("psM%d" % i, [128, 512], F32) for i in range(2)]
    psS = [P.ps("psS%d" % i, [128, 512], F32) for i in range(2)]
    psO = P.ps("psO", [128, 512], F32)
    psSum = P.ps("psSum", [128, 512], F32)
    psT = P.ps("psT", [128, 1024], BF16)
    psX = P.ps("psX", [128, 512], F32)

    state = {"wi": 0, "pm": 0, "xi": 0, "pt": 0, "ps": 0}

    def arena_buf(name, off, shape):
        n = 1
        for s in shape[1:]:
            n *= s
        ap = arena[:, off:off + n]
        if len(shape) == 3:
            ap = ap.rearrange("p (a b) -> p a b", b=shape[2])
        b = Buf(name, None)
        b.ap = ap
        return b, off + n

    P.dma("sp", lambda e: e.dma_start(out=consts.t[:], in_=consts_d.t[:, :, :]), [consts_d], [consts], consts)
    P.dma("sp", lambda e: e.dma_start(out=fm.t[:], in_=fm_d.t[:, :]), [fm_d], [fm], fm)
    P.dma("sp", lambda e: e.dma_start(out=ind.t[:], in_=ind_d.t[:, :, :]), [ind_d], [ind], ind)
    for who in range(2):
        P.op("dve", lambda e, who=who: e.scalar_tensor_tensor(
            out=a1T.t[:, who * 16:(who + 1) * 16], in0=fm.t[:, FM_SC1 + who * 16:FM_SC1 + (who + 1) * 16], scalar=1.0,
            in1=fm.t[:, FM_GMIX:FM_GMIX + 16], op0=ALU.add, op1=ALU.mult), [fm], [a1T])
    def rstd_from_ss(ss_ap, n, inv, Rb, out_ap, Wb):
        P.op("dve", lambda e: e.tensor_scalar(out=out_ap, in0=ss_ap, scalar1=inv, scalar2=EPS, op0=ALU.mult, op1=ALU.add), Rb, Wb)
        P.op("act", lambda e: e.activation(out=out_ap, in_=out_ap, func=AF.Sqrt), Wb, Wb)
        P.op("dve", lambda e: e.reciprocal(out=out_ap, in_=out_ap), Wb, Wb)

    def build_hT(xsrc):
        for t in range(9):
            who = 1 if t == 0 else 0
            i = 0
            state["xi"] += 1
            xb, xsb, stb = xt[i], xs[i], st[i]
            P.dma("sp", lambda e: e.dma_start(out=xb.t[:], in_=xsrc.t[t * 128:(t + 1) * 128, :]), [xsrc], [xb], xb)
            P.op("act", lambda e: e.activation(out=xsb.t[:], in_=xb.t[:], func=AF.Square, accum_out=stb.t[:, 0:1]), [xb], [xsb, stb])
            rstd_from_ss(stb.t[:, 0:1], 1, 1.0 / D, [stb], stb.t[:, 1:2], [stb])
            P.op("dve", lambda e: e.tensor_scalar(out=xsb.t[:], in0=xb.t[:], scalar1=stb.t[:, 1:2], scalar2=None, op0=ALU.mult), [xb, stb], [xsb])
            for half in range(2):
                for j in range(8):
                    kc = half * 8 + j
                    P.op("pe", lambda e, kc=kc, j=j: e.transpose(psT.t[:, j * 128:(j + 1) * 128], xsb.t[:, kc * 128:(kc + 1) * 128], ident),
                         [xsb, consts], [psT], inc=(j == 7))
                for j in range(8):
                    kc = half * 8 + j
                    ek = "dve" if j % 2 == 0 else "act"
                    if ek == "dve":
                        P.op("dve", lambda e, kc=kc, j=j: e.tensor_scalar(
                            out=hT.t[:, kc, t * 128:(t + 1) * 128], in0=psT.t[:, j * 128:(j + 1) * 128],
                            scalar1=a1T.t[:, who * 16 + kc:who * 16 + kc + 1], scalar2=fm.t[:, FM_SH1 + who * 16 + kc:FM_SH1 + who * 16 + kc + 1],
                            op0=ALU.mult, op1=ALU.add), [psT, a1T, fm], [hT])
                    else:
                        P.op("act", lambda e, kc=kc, j=j: e.activation(
                            out=hT.t[:, kc, t * 128:(t + 1) * 128], in_=psT.t[:, j * 128:(j + 1) * 128], func=AF.Identity,
                            scale=a1T.t[:, who * 16 + kc:who * 16 + kc + 1], bias=fm.t[:, FM_SH1 + who * 16 + kc:FM_SH1 + who * 16 + kc + 1]),
                            [psT, a1T, fm], [hT])

    def load_w(src, r_ap, ncols):
        w = wb[state["wi"] % 2]
        state["wi"] += 1
        P.dma("pool", lambda e: e.dma_start(out=w.t[:, :, 0:ncols], in_=r_ap), [src], [w], w)
        return w

    def next_psM():
        p = psM[state["pm"] % 2]
        state["pm"] += 1
        return p

    def proj_F(col0, ncols, chunks, cb, mwidth=128):
        for g0 in range(0, ncols, 256):
            gn = min(256, ncols - g0)
            w = load_w(w_in, w_in.t[:, col0 + g0:col0 + g0 + gn].rearrange("(kc p) n -> p kc n", p=128), gn)
            for m0 in range(0, gn, mwidth):
                m = min(mwidth, gn - m0)
                for (t0, n) in chunks:
                    ps = next_psM()
                    for kc in range(KC):
                        P.op("pe", lambda e, kc=kc: e.matmul(ps.t[0:m, 0:n], lhsT=w.t[:, kc, m0:m0 + m], rhs=hT.t[:, kc, t0:t0 + n],
                                                             start=(kc == 0), stop=(kc == KC - 1)), [w, hT], [ps], inc=(kc == KC - 1))
                    cb(ps, (g0 + m0) // mwidth, m, t0, n)

    def proj_T(col0, ncols, tiles, cb):
        for g0 in range(0, ncols, 256):
            gn = min(256, ncols - g0)
            w = load_w(w_in, w_in.t[:, col0 + g0:col0 + g0 + gn].rearrange("(kc p) n -> p kc n", p=128), gn)
            for t0 in tiles:
                ps = next_psM()
                for kc in range(KC):
                    P.op("pe", lambda e, kc=kc: e.matmul(ps.t[:, 0:gn], lhsT=hT.t[:, kc, t0:t0 + 128], rhs=w.t[:, kc, 0:gn],
                                                         start=(kc == 0), stop=(kc == KC - 1)), [w, hT], [ps], inc=(kc == KC - 1))
                cb(ps, g0, gn, t0)

    def copy_out(ek, dst_ap, src_ap, Rb, Wb):
        if ek == "act":
            P.op("act", lambda e: e.copy(out=dst_ap, in_=src_ap), Rb, Wb)
        else:
            P.op(ek, lambda e: e.tensor_copy(out=dst_ap, in_=src_ap), Rb, Wb)

    def load_rope(tab, npart, which, l0, n):
        c = cs[0]
        P.dma("sp", lambda e: e.dma_start(out=c.t[0:npart, :, 0:n], in_=tab.t[:, :, which, l0:l0 + n]), [tab], [c], c)
        return c

    def rope_apply(x_ap, xbuf, npart, Rt, c, n, dst_ap, dstbuf):
        P.op("pe", lambda e: e.matmul(psX.t[0:npart, 0:n], lhsT=Rt[0:npart, 0:npart], rhs=x_ap, start=True, stop=True), [xbuf, consts], [psX])
        P.op("dve", lambda e: e.tensor_tensor(out=tA.t[0:npart, 0:n], in0=x_ap, in1=c.t[0:npart, 0, 0:n], op=ALU.mult), [xbuf, c], [tA])
        P.op("dve", lambda e: e.tensor_tensor(out=tB.t[0:npart, 0:n], in0=psX.t[0:npart, 0:n], in1=c.t[0:npart, 1, 0:n], op=ALU.mult), [psX, c], [tB])
        P.op("dve", lambda e: e.tensor_tensor(out=dst_ap, in0=tA.t[0:npart, 0:n], in1=tB.t[0:npart, 0:n], op=ALU.add), [tA, tB], [dstbuf])

    def lat_off(t0):
        return t0 - 128

    def attention(QT_list, KT_list, Vbuf, vcol, key_tiles, q0, qn, scale, dst, dstbuf, Rbufs):
        nk = len(key_tiles)

        def emit_S(i):
            ps = psS[(state["ps"] + i) % 2]
            kt = key_tiles[i]
            for j, (qf, kf) in enumerate(zip(QT_list, KT_list)):
                P.op("pe", lambda e, j=j: e.matmul(ps.t[:, 0:qn], lhsT=kf(kt), rhs=qf(q0, qn), start=(j == 0), stop=(j == len(QT_list) - 1)),
                     Rbufs, [ps], inc=(j == len(QT_list) - 1))
            return ps
        pss = {0: emit_S(0)}
        for i in range(nk):
            if i + 1 < nk:
                pss[i + 1] = emit_S(i + 1)
            ps = pss.pop(i)
            pt = Pt[state["pt"] % 2]
            state["pt"] += 1
            P.op("act", lambda e: e.activation(out=pt.t[:, 0:qn], in_=ps.t[:, 0:qn], func=AF.Exp, scale=scale), [ps], [pt])
            kt = key_tiles[i]
            P.op("pe", lambda e: e.matmul(psO.t[:, 0:qn], lhsT=Vbuf.ap[:, kt, vcol:vcol + 128], rhs=pt.t[:, 0:qn], start=(i == 0), stop=(i == nk - 1)),
                 [Vbuf, pt], [psO], inc=False)
            P.op("pe", lambda e: e.matmul(psSum.t[:, 0:qn], lhsT=ones, rhs=pt.t[:, 0:qn], start=(i == 0), stop=(i == nk - 1)),
                 [consts, pt], [psSum], inc=True)
        state["ps"] += nk
        P.op("dve", lambda e: e.reciprocal(out=rr.t[:, 0:qn], in_=psSum.t[:, 0:qn]), [psSum], [rr])
        P.op("dve", lambda e: e.tensor_tensor(out=dst, in0=psO.t[:, 0:qn], in1=rr.t[:, 0:qn], op=ALU.mult), [psO, rr], [dstbuf])

    def barrier():
        evs = [("e_" + k, P.sem[k], P.cnt[k], "bar") for k in P.cnt if P.cnt[k] > 0]
        evs += [(b.semkey, b.sem, b.cnt, "dma") for b in P.dma_bufs]
        for k in P.eng:
            P._wait(k, evs)

    def mla_norm(gcol, n):
        for c in range(4):
            P.op("pe", lambda e, c=c: e.matmul(psX.t[:, 0:n], lhsT=ones, rhs=sq.t[:, c, 0:n], start=(c == 0), stop=(c == 3)), [consts, sq], [psX], inc=(c == 3))
        rstd_from_ss(psX.t[:, 0:n], n, 1.0 / 512, [psX], rr.t[:, 0:n], [rr])
        for c in range(4):
            P.op("dve", lambda e, c=c: e.scalar_tensor_tensor(out=sq.t[:, c, 0:n], in0=ck.t[:, c, 0:n], scalar=fm.t[:, gcol + c:gcol + c + 1],
                                                             in1=rr.t[:, 0:n], op0=ALU.mult, op1=ALU.mult), [ck, fm, rr], [sq])

    def cb_ck(ps, ci, m, t0, n):
        P.op("act", lambda e: e.copy(out=ck.t[:, ci, 0:n], in_=ps.t[:, 0:n]), [ps], [ck])
        P.op("act", lambda e: e.activation(out=sq.t[:, ci, 0:n], in_=ps.t[:, 0:n], func=AF.Square), [ps], [sq])

    off = 0
    KnT, off = arena_buf("KnT", off, [128, 4, 2304])
    kpeT, off = arena_buf("kpeT", off, [128, 2304])
    Vm, off = arena_buf("Vm", off, [128, 18, 512])
    QnT, off = arena_buf("QnT", off, [128, 4, NOWN])
    QpT, off = arena_buf("QpT", off, [128, 4, NOWN])
    wuq, off = arena_buf("wuq", off, [128, 4, 768])
    wukv, off = arena_buf("wukv", off, [128, 4, 1024])
    assert off <= ARENA_N
    wuq_sem = P.sb("wuq_sem", [128, 2], F32)
    P.dma("pool", lambda e: e.dma_start(out=wuq.ap, in_=w_uq.t[:, :].rearrange("(c p) n -> p c n", p=128)), [w_uq], [wuq], wuq_sem)
    P.dma("pool", lambda e: e.dma_start(out=wukv.ap, in_=w_ukv.t[:, :].rearrange("(c p) n -> p c n", p=128)), [w_ukv], [wukv], wuq_sem)

    def mla_tokens(which, kvbase):
        for (t0, n) in OWN_CHUNKS:
            is_lat = t0 >= 128
            if is_lat:
                c = load_rope(ropeA, 64, which, lat_off(t0), n)
            proj_F(CA + 512, 512, [(t0, n)], cb_ck)
            mla_norm(FM_GKVA, n)
            for h in range(4):
                ps = next_psM()
                for cc in range(4):
                    P.op("pe", lambda e, cc=cc: e.matmul(ps.t[:, 0:n], lhsT=wukv.ap[:, cc, h * 256:h * 256 + 128], rhs=sq.t[:, cc, 0:n],
                                                         start=(cc == 0), stop=(cc == 3)), [wukv, sq], [ps], inc=(cc == 3))
                copy_out("act" if h % 2 else "dve", KnT.ap[:, h, kvbase + t0:kvbase + t0 + n], ps.t[:, 0:n], [ps], [KnT])
            for tt in range(n // 128):
                ps = next_psM()
                for cc in range(4):
                    P.op("pe", lambda e, cc=cc: e.matmul(ps.t[:, 0:512].rearrange("p (h x) -> p h x", x=128), lhsT=sq.t[:, cc, tt * 128:(tt + 1) * 128],
                                                         rhs=wukv.ap[:, cc, :].rearrange("p (h x) -> p h x", x=256)[:, :, 128:256],
                                                         start=(cc == 0), stop=(cc == 3)), [wukv, sq], [ps], inc=(cc == 3))
                copy_out("act" if tt % 2 else "dve", Vm.ap[:, (kvbase + t0) // 128 + tt, :], ps.t[:, 0:512], [ps], [Vm])

            def cb_kpe(ps, ci, m, t0_, n_):
                if not is_lat:
                    copy_out("dve", kpeT.ap[0:64, kvbase + t0:kvbase + t0 + n], ps.t[0:64, 0:n], [ps], [kpeT])
                else:
                    copy_out("dve", ck.t[0:64, 0, 0:n], ps.t[0:64, 0:n], [ps], [ck])
                    rope_apply(ck.t[0:64, 0, 0:n], ck, 64, RAt, c, n, kpeT.ap[0:64, kvbase + t0:kvbase + t0 + n], kpeT)
            proj_F(CA + 1024, 64, [(t0, n)], cb_kpe, mwidth=64)
            if which == 1:
                continue
            proj_F(CA, 512, [(t0, n)], cb_ck)
            mla_norm(FM_GQA, n)
            for h in range(4):
                ps = next_psM()
                for cc in range(4):
                    P.op("pe", lambda e, cc=cc: e.matmul(ps.t[:, 0:n], lhsT=wuq.ap[:, cc, h * 192:h * 192 + 128], rhs=sq.t[:, cc, 0:n],
                                                         start=(cc == 0), stop=(cc == 3)), [wuq, sq], [ps], inc=(cc == 3))
                copy_out("act", QnT.ap[:, h, t0:t0 + n], ps.t[:, 0:n], [ps], [QnT])
                ps = next_psM()
                for cc in range(4):
                    P.op("pe", lambda e, cc=cc: e.matmul(ps.t[0:64, 0:n], lhsT=wuq.ap[:, cc, h * 192 + 128:h * 192 + 192], rhs=sq.t[:, cc, 0:n],
                                                         start=(cc == 0), stop=(cc == 3)), [wuq, sq], [ps], inc=(cc == 3))
                if not is_lat:
                    copy_out("dve", QpT.ap[0:64, h, t0:t0 + n], ps.t[0:64, 0:n], [ps], [QpT])
                else:
                    copy_out("dve", ck.t[0:64, 0, 0:n], ps.t[0:64, 0:n], [ps], [ck])
                    rope_apply(ck.t[0:64, 0, 0:n], ck, 64, RAt, c, n, QpT.ap[0:64, h, t0:t0 + n], QpT)

    def run_attn_full(QTs, KTs, Vb, vcolf, scale, ydst):
        for (q0, qn) in OWN_CHUNKS:
            kts = [0, 9] if q0 == 0 else list(range(18))
            for h in range(4):
                pairs = QTs(h)
                attention([p[0] for p in pairs], [p[1] for p in pairs], Vb, vcolf(h), kts, q0, qn, scale,
                          ydst.t[:, h, q0:q0 + qn], ydst, [b for b in KTs])

    build_hT(x_oth)
    mla_tokens(1, NOWN)
    build_hT(x_own)
    mla_tokens(0, 0)
    run_attn_full(lambda h: [(lambda q0, qn: QnT.ap[:, h, q0:q0 + qn], lambda kt: KnT.ap[:, h, kt * 128:(kt + 1) * 128]),
                             (lambda q0, qn: QpT.ap[0:64, h, q0:q0 + qn], lambda kt: kpeT.ap[0:64, kt * 128:(kt + 1) * 128])],
                  [KnT, kpeT, QnT, QpT], Vm, lambda h: h * 128, MLA_SCALE, yT[0])
    barrier()

    off = 0
    KTb, off = arena_buf("KTb", off, [128, 2, 2304])
    Vb, off = arena_buf("Vb", off, [128, 18, 256])
    QTb, off = arena_buf("QTb", off, [128, 4, NOWN])

    def qknorm(ps, n, gcol, c, dst_ap, dstbuf):
        P.op("act", lambda e: e.copy(out=ck.t[:, 0, 0:n], in_=ps.t[:, 0:n]), [ps], [ck])
        P.op("act", lambda e: e.activation(out=sq.t[:, 0, 0:n], in_=ps.t[:, 0:n], func=AF.Square), [ps], [sq])
        P.op("pe", lambda e: e.matmul(psX.t[:, 0:n], lhsT=ones, rhs=sq.t[:, 0, 0:n], start=True, stop=True), [consts, sq], [psX])
        rstd_from_ss(psX.t[:, 0:n], n, 1.0 / 128, [psX], rr.t[:, 0:n], [rr])
        if c is None:
            P.op("dve", lambda e: e.scalar_tensor_tensor(out=dst_ap, in0=ck.t[:, 0, 0:n], scalar=fm.t[:, gcol:gcol + 1], in1=rr.t[:, 0:n],
                                                         op0=ALU.mult, op1=ALU.mult), [ck, fm, rr], [dstbuf])
        else:
            P.op("dve", lambda e: e.scalar_tensor_tensor(out=sq.t[:, 1, 0:n], in0=ck.t[:, 0, 0:n], scalar=fm.t[:, gcol:gcol + 1], in1=rr.t[:, 0:n],
                                                         op0=ALU.mult, op1=ALU.mult), [ck, fm, rr], [sq])
            rope_apply(sq.t[:, 1, 0:n], sq, 128, RBt, c, n, dst_ap, dstbuf)

    def gqa_tokens(which, kvbase):
        for (t0, n) in OWN_CHUNKS:
            c = load_rope(ropeB, 128, which, lat_off(t0), n) if t0 >= 128 else None
            proj_F(CB + 512, 256, [(t0, n)], lambda ps, ci, m, t0_, n_: qknorm(ps, n, FM_GKN, c, KTb.ap[:, ci, kvbase + t0:kvbase + t0 + n], KTb))
            if which == 0:
                proj_F(CB, 512, [(t0, n)], lambda ps, ci, m, t0_, n_: qknorm(ps, n, FM_GQN, c, QTb.ap[:, ci, t0:t0 + n], QTb))
        proj_T(CB + 768, 256, [t * 128 for t in range(9)],
               lambda ps, g0, gn, t0: copy_out("act" if (t0 // 128) % 2 else "dve", Vb.ap[:, (kvbase + t0) // 128, :], ps.t[:, 0:256], [ps], [Vb]))

    build_hT(x_oth)
    gqa_tokens(1, NOWN)
    build_hT(x_own)
    gqa_tokens(0, 0)
    run_attn_full(lambda h: [(lambda q0, qn: QTb.ap[:, h, q0:q0 + qn], lambda kt: KTb.ap[:, h // 2, kt * 128:(kt + 1) * 128])],
                  [KTb, QTb], Vb, lambda h: (h // 2) * 128, HD_SCALE, yT[1])
    barrier()

    off = 0
    KTc, off = arena_buf("KTc", off, [128, 4, 1792])
    Vctx, off = arena_buf("Vctx", off, [128, 2, 512])
    Vev, off = arena_buf("Vev", off, [128, 12, 512])
    QTc, off = arena_buf("QTc", off, [128, 4, NOWN])
    E2, off = arena_buf("E2", off, [128, 60, 64])
    for h in range(4):
        P.dma("sp", lambda e: e.dma_start(out=xt[0].t[:, 0:960], in_=nab_d.t[:, h, :, :].rearrange("p a b -> p (a b)")),
              [nab_d], [xt[0]], xt[0])
        P.op("act", lambda e: e.activation(out=E2.ap[:, h * 15:(h + 1) * 15, :].rearrange("p a b -> p (a b)"), in_=xt[0].t[:, 0:960], func=AF.Exp),
             [xt[0]], [E2])

    def kc_dst(which, t0):
        if which == 0:
            return 0 if t0 == 0 else 512 + (t0 - 128)
        if t0 == 0:
            return 128
        return 256 if t0 == 896 else 1536

    build_hT(x_oth)
    oth_chunks = [(0, 128), (896, 256), (128, 256)]
    proj_F(CC + 512, 512, oth_chunks, lambda ps, ci, m, t0, n: copy_out("act" if ci % 2 else "dve", KTc.ap[:, ci, kc_dst(1, t0):kc_dst(1, t0) + n], ps.t[:, 0:n], [ps], [KTc]))

    def cb_v_oth(ps, g0, gn, t0):
        if t0 == 0:
            dst = Vctx.ap[:, 1, g0:g0 + gn]
            db = Vctx
        else:
            ti = {896: 0, 1024: 1, 128: 10, 256: 11}[t0]
            dst = Vev.ap[:, ti, g0:g0 + gn]
            db = Vev
        copy_out("dve", dst, ps.t[:, 0:gn], [ps], [db])
    proj_T(CC + 1024, 512, [0, 896, 1024, 128, 256], cb_v_oth)
    build_hT(x_own)
    proj_F(CC, 512, OWN_CHUNKS, lambda ps, ci, m, t0, n: copy_out("act" if ci % 2 else "dve", QTc.ap[:, ci, t0:t0 + n], ps.t[:, 0:n], [ps], [QTc]))
    proj_F(CC + 512, 512, OWN_CHUNKS, lambda ps, ci, m, t0, n: copy_out("act" if ci % 2 else "dve", KTc.ap[:, ci, kc_dst(0, t0):kc_dst(0, t0) + n], ps.t[:, 0:n], [ps], [KTc]))

    def cb_v_own(ps, g0, gn, t0):
        if t0 == 0:
            copy_out("dve", Vctx.ap[:, 0, g0:g0 + gn], ps.t[:, 0:gn], [ps], [Vctx])
        else:
            copy_out("dve", Vev.ap[:, 2 + (t0 - 128) // 128, g0:g0 + gn], ps.t[:, 0:gn], [ps], [Vev])
    proj_T(CC + 1024, 512, [t * 128 for t in range(9)], cb_v_own)
    for h in range(4):
        attention([lambda q0, qn: QTc.ap[:, h, q0:q0 + qn]], [lambda kt: KTc.ap[:, h, kt * 128:(kt + 1) * 128]], Vctx, h * 128, [0, 1], 0, 128,
                  HD_SCALE, yT[2].t[:, h, 0:128], yT[2], [KTc, QTc])
    for h in range(4):
        for lr in range(16):
            Rs = lr if lr <= 12 else 12
            Re = 11 if lr < 4 else lr + 7
            R0 = Rs - Rs % 2
            R1 = Re | 1
            npair = (R1 - R0 + 1) // 2
            nsl = npair + 2
            ps = psS[state["ps"] % 2]
            state["ps"] += 1
            qap = QTc.ap[:, h, 128 + lr * 64:128 + (lr + 1) * 64]
            for s_ in range(nsl):
                if s_ < npair:
                    R = R0 + 2 * s_
                    kap = KTc.ap[:, h, 256 + 64 * R:256 + 64 * R + 128]
                else:
                    kap = KTc.ap[:, h, (s_ - npair) * 128:(s_ - npair + 1) * 128]
                P.op("pe", lambda e: e.matmul(ps.t[:, s_ * 64:(s_ + 1) * 64], lhsT=kap, rhs=qap, start=True, stop=True), [KTc, QTc], [ps], inc=(s_ == nsl - 1))
            pt = Pt[state["pt"] % 2]
            state["pt"] += 1
            P.op("act", lambda e: e.activation(out=pt.t[:, 0:nsl * 64], in_=ps.t[:, 0:nsl * 64], func=AF.Exp, scale=HD_SCALE), [ps], [pt])
            for s_ in range(npair):
                R = R0 + 2 * s_
                m = R + 3 - lr
                P.op("dve", lambda e: e.scalar_tensor_tensor(out=pt.t[:, s_ * 64:(s_ + 1) * 64], in0=pt.t[:, s_ * 64:(s_ + 1) * 64], scalar=ind.t[:, lr, s_:s_ + 1],
                                                             in1=E2.ap[:, h * 15 + m, :], op0=ALU.mult, op1=ALU.mult), [pt, ind, E2], [pt])
            for s_ in range(nsl):
                if s_ < npair:
                    vap = Vev.ap[:, (R0 + 2 * s_) // 2, h * 128:(h + 1) * 128]
                else:
                    vap = Vctx.ap[:, s_ - npair, h * 128:(h + 1) * 128]
                P.op("pe", lambda e: e.matmul(psO.t[:, 0:64], lhsT=vap, rhs=pt.t[:, s_ * 64:(s_ + 1) * 64], start=(s_ == 0), stop=(s_ == nsl - 1)),
                     [Vev, Vctx, pt], [psO], inc=False)
                P.op("pe", lambda e: e.matmul(psSum.t[:, 0:64], lhsT=ones, rhs=pt.t[:, s_ * 64:(s_ + 1) * 64], start=(s_ == 0), stop=(s_ == nsl - 1)),
                     [consts, pt], [psSum], inc=True)
            P.op("dve", lambda e: e.reciprocal(out=rr.t[:, 0:64], in_=psSum.t[:, 0:64]), [psSum], [rr])
            P.op("dve", lambda e: e.tensor_tensor(out=yT[2].t[:, h, 128 + lr * 64:128 + (lr + 1) * 64], in0=psO.t[:, 0:64], in1=rr.t[:, 0:64], op=ALU.mult),
                 [psO, rr], [yT[2]])
    barrier()

    off = 0
    bg, off = arena_buf("bg", off, [128, 4, NOWN])
    cg, off = arena_buf("cg", off, [128, 4, NOWN])
    zlat, off = arena_buf("zlat", off, [128, 4, 1026])
    zctx, off = arena_buf("zctx", off, [128, 4, 130])
    cgo, off = arena_buf("cgo", off, [128, 4, 384])
    zo, off = arena_buf("zo", off, [128, 4, 384])
    och = [(0, 128), (128, 128), (1024, 128)]
    oslot = {0: 0, 128: 1, 1024: 2}
    build_hT(x_oth)
    proj_F(CD + 512, 512, och, lambda ps, ci, m, t0, n: copy_out("act", cgo.ap[:, ci, oslot[t0] * 128:(oslot[t0] + 1) * 128], ps.t[:, 0:n], [ps], [cgo]))
    proj_F(CD + 1024, 512, och, lambda ps, ci, m, t0, n: P.op("dve", lambda e: e.tensor_tensor(
        out=zo.ap[:, ci, oslot[t0] * 128:(oslot[t0] + 1) * 128], in0=ps.t[:, 0:n], in1=cgo.ap[:, ci, oslot[t0] * 128:(oslot[t0] + 1) * 128], op=ALU.mult), [ps, cgo], [zo]))
    for (dst, dcol, scol, fl) in [(zlat, 0, 383, FM_FLAGB), (zlat, 1025, 128, FM_FLAGA), (zctx, 0, 127, FM_FLAGB), (zctx, 129, 0, FM_FLAGA)]:
        P.op("dve", lambda e: e.tensor_scalar(out=dst.ap[:, :, dcol:dcol + 1], in0=zo.ap[:, :, scol:scol + 1], scalar1=fm.t[:, fl:fl + 1], scalar2=None, op0=ALU.mult),
             [zo, fm], [dst])
    build_hT(x_own)
    proj_F(CD, 512, OWN_CHUNKS, lambda ps, ci, m, t0, n: copy_out("act", bg.ap[:, ci, t0:t0 + n], ps.t[:, 0:n], [ps], [bg]))
    proj_F(CD + 512, 512, OWN_CHUNKS, lambda ps, ci, m, t0, n: copy_out("act", cg.ap[:, ci, t0:t0 + n], ps.t[:, 0:n], [ps], [cg]))

    def zdst(ci, t0, n):
        return (zctx, zctx.ap[:, ci, 1:1 + n]) if t0 == 0 else (zlat, zlat.ap[:, ci, 1 + t0 - 128:1 + t0 - 128 + n])
    proj_F(CD + 1024, 512, OWN_CHUNKS, lambda ps, ci, m, t0, n: P.op("dve", lambda e: e.tensor_tensor(
        out=zdst(ci, t0, n)[1], in0=ps.t[:, 0:n], in1=cg.ap[:, ci, t0:t0 + n], op=ALU.mult), [ps, cg], [zdst(ci, t0, n)[0]]))
    for ci in range(4):
        for (t0, n) in OWN_CHUNKS:
            zb, zoff = (zctx, 0) if t0 == 0 else (zlat, t0 - 128)
            wc = FM_WCONV + ci * 3
            P.op("dve", lambda e: e.tensor_scalar(out=tA.t[:, 0:n], in0=zb.ap[:, ci, zoff:zoff + n], scalar1=fm.t[:, wc:wc + 1], scalar2=None, op0=ALU.mult), [zb, fm], [tA])
            for tap in (1, 2):
                P.op("dve", lambda e: e.scalar_tensor_tensor(out=tA.t[:, 0:n], in0=zb.ap[:, ci, zoff + tap:zoff + tap + n], scalar=fm.t[:, wc + tap:wc + tap + 1],
                                                             in1=tA.t[:, 0:n], op0=ALU.mult, op1=ALU.add), [zb, fm, tA], [tA])
            P.op("dve", lambda e: e.tensor_tensor(out=yT[3].t[:, ci, t0:t0 + n], in0=tA.t[:, 0:n], in1=bg.ap[:, ci, t0:t0 + n], op=ALU.mult), [tA, bg], [yT[3]])
    barrier()
    if DEBUG_Y:
        ydbg = P.dram("ydbg", [4, 128, 4, NOWN], BF16, "ExternalOutput")
        for k in range(4):
            P.dma("sp", lambda e: e.dma_start(out=ydbg.t[k, :, :, :], in_=yT[k].t[:]), [yT[k]], [ydbg], yT[k])

    off = 0
    mT, off = arena_buf("mT", off, [128, KC, NOWN])
    macc = Buf("macc", None)
    macc.ap = arena[:, off:off + 2 * 2 * NOWN].bitcast(F32).rearrange("p (a b) -> p a b", b=NOWN)
    off += 4 * NOWN
    wbr = []
    for i in range(2):
        b_, off = arena_buf("wbr%d" % i, off, [128, 4, 256])
        b_.sembuf = P.sb("wbrsem%d" % i, [128, 2], F32)
        wbr.append(b_)
    assert off <= ARENA_N
    wi2 = 0
    for dg in range(8):
        for k in range(4):
            w = load_w(w_in, w_in.t[:, CG + k * D + dg * 256:CG + k * D + (dg + 1) * 256].rearrange("(kc p) n -> p kc n", p=128), 256)
            wbk = wbr[wi2 % 2]
            wi2 += 1
            P.dma("pool", lambda e: e.dma_start(out=wbk.ap, in_=w_br.t[k, :, dg * 256:(dg + 1) * 256].rearrange("(c p) n -> p c n", p=128)), [w_br], [wbk], wbk.sembuf)
            for dc in range(2):
                for (t0, n) in OWN_CHUNKS:
                    ps = next_psM()
                    for kc in range(KC):
                        P.op("pe", lambda e, kc=kc: e.matmul(ps.t[:, 0:n], lhsT=w.t[:, kc, dc * 128:(dc + 1) * 128], rhs=hT.t[:, kc, t0:t0 + n],
                                                             start=(kc == 0), stop=(kc == KC - 1)), [w, hT], [ps], inc=(kc == KC - 1))
                    ps2 = next_psM()
                    for cc in range(4):
                        P.op("pe", lambda e, cc=cc: e.matmul(ps2.t[:, 0:n], lhsT=wbk.ap[:, cc, dc * 128:(dc + 1) * 128], rhs=yT[k].t[:, cc, t0:t0 + n],
                                                             start=(cc == 0), stop=(cc == 3)), [wbk, yT[k]], [ps2], inc=(cc == 3))
                    P.op("act", lambda e: e.activation(out=tA.t[:, 0:n], in_=ps.t[:, 0:n], func=AF.Sigmoid), [ps], [tA])
                    if k == 0:
                        P.op("dve", lambda e: e.tensor_tensor(out=macc.ap[:, dc, t0:t0 + n], in0=ps2.t[:, 0:n], in1=tA.t[:, 0:n], op=ALU.mult), [ps2, tA], [macc])
                    else:
                        P.op("dve", lambda e: e.tensor_tensor(out=tB.t[:, 0:n], in0=ps2.t[:, 0:n], in1=tA.t[:, 0:n], op=ALU.mult), [ps2, tA], [tB])
                        if k < 3:
                            P.op("dve", lambda e: e.tensor_tensor(out=macc.ap[:, dc, t0:t0 + n], in0=macc.ap[:, dc, t0:t0 + n], in1=tB.t[:, 0:n], op=ALU.add), [macc, tB], [macc])
                        else:
                            P.op("dve", lambda e: e.tensor_tensor(out=mT.ap[:, dg * 2 + dc, t0:t0 + n], in0=macc.ap[:, dc, t0:t0 + n], in1=tB.t[:, 0:n], op=ALU.add), [macc, tB], [mT])
    barrier()

    wo = Buf("wo", None)
    wo.ap = arena2[:, 0:KC * D].rearrange("p (a b) -> p a b", b=D)
    wo.sembuf = P.sb("wosem", [128, 2], F32)
    for og in range(4):
        P.dma("pool", lambda e: e.dma_start(out=wo.ap[:, :, og * 512:(og + 1) * 512], in_=w_o.t[:, og * 512:(og + 1) * 512].rearrange("(kc p) n -> p kc n", p=128)),
              [w_o], [wo], wo.sembuf)
    reps = []
    for i in range(3):
        b_ = Buf("rep%d" % i, None)
        b_.ap = arena[:, off + i * 2 * D:off + (i + 1) * 2 * D].bitcast(F32)
        b_.sembuf = P.sb("repsem%d" % i, [128, 2], F32)
        reps.append(b_)
    off += 6 * D
    assert off <= ARENA_N
    repA, repB, repC = reps
    wrs = P.sb("wrs", [128, KC, 72], BF16)
    P.dma("pool", lambda e: e.dma_start(out=wrs.t[:], in_=wr_d.t[:, :].rearrange("(kc p) n -> p kc n", p=128)), [wr_d], [wrs], wrs)
    brep = P.sb("brep", [128, 72], F32)
    P.dma("sp", lambda e: e.dma_start(out=brep.t[:], in_=brow_d.t[0:1, :].partition_broadcast(128)), [brow_d], [brep], brep)
    rt = P.sb("rt", [128, 256], F32)
    wf = P.sb("wf", [128, 64], F32)

    def load_reps(who):
        P.dma("sp", lambda e: e.dma_start(out=repA.ap, in_=rows_d.t[ROW_SC2 + who:ROW_SC2 + who + 1, :].partition_broadcast(128)), [rows_d], [repA], repA.sembuf)
        P.dma("sp", lambda e: e.dma_start(out=repC.ap, in_=rows_d.t[ROW_GFFN:ROW_GFFN + 1, :].partition_broadcast(128)), [rows_d], [repC], repC.sembuf)
        P.op("dve", lambda e: e.scalar_tensor_tensor(out=repA.ap, in0=repA.ap, scalar=1.0, in1=repC.ap, op0=ALU.add, op1=ALU.mult), [repA, repC], [repA])
        P.dma("sp", lambda e: e.dma_start(out=repC.ap, in_=rows_d.t[ROW_SH2 + who:ROW_SH2 + who + 1, :].partition_broadcast(128)), [rows_d], [repC], repC.sembuf)
        P.dma("sp", lambda e: e.dma_start(out=repB.ap, in_=rows_d.t[ROW_GA1 + who:ROW_GA1 + who + 1, :].partition_broadcast(128)), [rows_d], [repB], repB.sembuf)

    xb, xsb, stb = xt[0], xs[0], st[0]
    for t in range(9):
        if t < 2:
            load_reps(1 if t == 0 else 0)
        P.dma("sp", lambda e: e.dma_start(out=xb.t[:], in_=x_own.t[t * 128:(t + 1) * 128, :]), [x_own], [xb], xb)
        for og in range(4):
            ps = next_psM()
            for kc in range(KC):
                P.op("pe", lambda e, kc=kc: e.matmul(ps.t[:, 0:512], lhsT=mT.ap[:, kc, t * 128:(t + 1) * 128], rhs=wo.ap[:, kc, og * 512:(og + 1) * 512],
                                                     start=(kc == 0), stop=(kc == KC - 1)), [mT, wo], [ps], inc=(kc == KC - 1))
            P.op("dve", lambda e: e.tensor_tensor(out=tA.t[:, 0:512], in0=ps.t[:, 0:512], in1=repB.ap[:, og * 512:(og + 1) * 512], op=ALU.mult), [ps, repB], [tA])
            P.op("dve", lambda e: e.tensor_tensor(out=xb.t[:, og * 512:(og + 1) * 512], in0=xb.t[:, og * 512:(og + 1) * 512], in1=tA.t[:, 0:512], op=ALU.add), [xb, tA], [xb])
        P.dma("sp", lambda e: e.dma_start(out=x1_o.t[t * 128:(t + 1) * 128, :], in_=xb.t[:]), [xb], [x1_o], xb)
        P.op("act", lambda e: e.activation(out=xsb.t[:], in_=xb.t[:], func=AF.Square, accum_out=stb.t[:, 0:1]), [xb], [xsb, stb])
        rstd_from_ss(stb.t[:, 0:1], 1, 1.0 / D, [stb], stb.t[:, 1:2], [stb])
        for og in range(4):
            sl = slice(og * 512, (og + 1) * 512)
            P.op("dve", lambda e: e.scalar_tensor_tensor(out=tA.t[:, 0:512], in0=xb.t[:, sl], scalar=stb.t[:, 1:2], in1=repA.ap[:, sl], op0=ALU.mult, op1=ALU.mult),
                 [xb, stb, repA], [tA])
            P.op("dve", lambda e: e.tensor_tensor(out=xsb.t[:, sl], in0=tA.t[:, 0:512], in1=repC.ap[:, sl], op=ALU.add), [tA, repC], [xsb])
        P.dma("sp", lambda e: e.dma_start(out=h2_o.t[t * 128:(t + 1) * 128, :], in_=xsb.t[:]), [xsb], [h2_o], xsb)
        h2T = ck.t[:].rearrange("p a b -> p (a b)").rearrange("p (k n) -> p k n", n=128)
        for half in range(2):
            for j in range(8):
                kc = half * 8 + j
                P.op("pe", lambda e: e.transpose(psT.t[:, j * 128:(j + 1) * 128], xsb.t[:, kc * 128:(kc + 1) * 128], ident), [xsb, consts], [psT], inc=(j == 7))
            copy_out("act", h2T[:, half * 8:(half + 1) * 8, :], psT.t[:, :].rearrange("p (k n) -> p k n", n=128), [psT], [ck])
        ps = next_psM()
        for kc in range(KC):
            P.op("pe", lambda e, kc=kc: e.matmul(ps.t[:, 0:72], lhsT=h2T[:, kc, :], rhs=wrs.t[:, kc, :], start=(kc == 0), stop=(kc == KC - 1)), [ck, wrs], [ps], inc=(kc == KC - 1))
        lg = rt.t[:, 0:72]
        R_ = [rt]
        P.op("dve", lambda e: e.tensor_copy(out=lg, in_=ps.t[:, 0:72]), [ps], R_)
        sm = rt.t[:, 200:216]

        def softmax8(src, dst, mcol):
            P.op("dve", lambda e: e.tensor_reduce(out=sm[:, mcol:mcol + 1], in_=src, axis=AX.X, op=ALU.max, negate=True), R_, R_)
            P.op("act", lambda e: e.activation(out=dst, in_=src, func=AF.Exp, bias=sm[:, mcol:mcol + 1], scale=1.0, accum_out=sm[:, mcol + 1:mcol + 2]), R_, R_)
            P.op("dve", lambda e: e.reciprocal(out=sm[:, mcol + 1:mcol + 2], in_=sm[:, mcol + 1:mcol + 2]), R_, R_)
            P.op("dve", lambda e: e.tensor_scalar(out=dst, in0=dst, scalar1=sm[:, mcol + 1:mcol + 2], scalar2=None, op0=ALU.mult), R_, R_)

        def onehot_max(src, dst, mcol):
            P.op("dve", lambda e: e.tensor_reduce(out=sm[:, mcol:mcol + 1], in_=src, axis=AX.X, op=ALU.max), R_, R_)
            P.op("dve", lambda e: e.tensor_scalar(out=dst, in0=src, scalar1=sm[:, mcol:mcol + 1], scalar2=None, op0=ALU.is_equal), R_, R_)
        gp, gs, og_, es, eb, ep, sel, o1, o2, tmp = (rt.t[:, 72 + 8 * i:80 + 8 * i] for i in range(10))
        softmax8(rt.t[:, 0:8], gp, 0)
        P.op("dve", lambda e: e.tensor_tensor(out=gs, in0=gp, in1=brep.t[:, 0:8], op=ALU.add), R_ + [brep], R_)
        onehot_max(gs, og_, 2)
        P.op("dve", lambda e: e.tensor_tensor(out=tmp, in0=gp, in1=og_, op=ALU.mult), R_, R_)
        P.op("dve", lambda e: e.tensor_reduce(out=sm[:, 3:4], in_=tmp, axis=AX.X, op=ALU.add), R_, R_)
        for g in range(8):
            if g == 0:
                P.op("dve", lambda e: e.tensor_scalar(out=es, in0=rt.t[:, 8:16], scalar1=og_[:, 0:1], scalar2=None, op0=ALU.mult), R_, R_)
                P.op("dve", lambda e: e.tensor_scalar(out=eb, in0=brep.t[:, 8:16], scalar1=og_[:, 0:1], scalar2=None, op0=ALU.mult), R_ + [brep], R_)
            else:
                P.op("dve", lambda e: e.scalar_tensor_tensor(out=es, in0=rt.t[:, 8 + 8 * g:16 + 8 * g], scalar=og_[:, g:g + 1], in1=es, op0=ALU.mult, op1=ALU.add), R_, R_)
                P.op("dve", lambda e: e.scalar_tensor_tensor(out=eb, in0=brep.t[:, 8 + 8 * g:16 + 8 * g], scalar=og_[:, g:g + 1], in1=eb, op0=ALU.mult, op1=ALU.add), R_ + [brep], R_)
        softmax8(es, ep, 4)
        P.op("dve", lambda e: e.tensor_tensor(out=sel, in0=ep, in1=eb, op=ALU.add), R_, R_)
        onehot_max(sel, o1, 6)
        P.op("dve", lambda e: e.scalar_tensor_tensor(out=sel, in0=o1, scalar=-1e9, in1=sel, op0=ALU.mult, op1=ALU.add), R_, R_)
        onehot_max(sel, o2, 7)
        P.op("dve", lambda e: e.tensor_tensor(out=o1, in0=o1, in1=o2, op=ALU.add), R_, R_)
        P.op("dve", lambda e: e.tensor_tensor(out=tmp, in0=ep, in1=o1, op=ALU.mult), R_, R_)
        P.op("dve", lambda e: e.tensor_reduce(out=sm[:, 8:9], in_=tmp, axis=AX.X, op=ALU.add), R_, R_)
        P.op("dve", lambda e: e.reciprocal(out=sm[:, 8:9], in_=sm[:, 8:9]), R_, R_)
        P.op("dve", lambda e: e.tensor_scalar(out=tmp, in0=tmp, scalar1=sm[:, 8:9], scalar2=sm[:, 3:4], op0=ALU.mult, op1=ALU.mult), R_, R_)
        for g in range(8):
            P.op("dve", lambda e: e.tensor_scalar(out=wf.t[:, 8 * g:8 * g + 8], in0=tmp, scalar1=og_[:, g:g + 1], scalar2=None, op0=ALU.mult), R_, [wf])
        P.dma("sp", lambda e: e.dma_start(out=wr_o.t[t * 128:(t + 1) * 128, :], in_=wf.t[:]), [wf], [wr_o], wf)
    P.finish()
    return nc


NTOK = NCORES * NOWN
BCH = 6


def build_B():
    nc = bass.Bass("TRN2", target_bir_lowering=False)
    P = Prog(nc)
    h2 = P.dram("h2", [NTOK, D], BF16, "ExternalInput")
    wsel_d = P.dram("wsel", [NTOK, 8], F32, "ExternalInput")
    wg_d = P.dram("wg", [8, D, 512], F32, "ExternalInput")
    wu_d = P.dram("wu", [8, D, 512], F32, "ExternalInput")
    wd_d = P.dram("wd", [8, 512, D], F32, "ExternalInput")
    ident_d = P.dram("ident", [128, 128], BF16, "ExternalInput")
    out = P.dram("contrib", [NTOK, D], BF16, "ExternalOutput")
    ntile = NTOK // 128
    ident = P.sb("ident_sb", [128, 128], BF16)
    wsel = P.sb("wsel_sb", [128, ntile, 8], F32)
    h2T = P.sb("h2T", [128, KC, BCH * 128], BF16)
    yacc = P.sb("yacc", [128, BCH, D], F32)
    wg = [P.sb("wg%d" % i, [128, KC, 512], BF16) for i in range(2)]
    wu = [P.sb("wu%d" % i, [128, KC, 512], BF16) for i in range(2)]
    wd = [P.sb("wd%d" % i, [128, 4, D], BF16) for i in range(2)]
    ht = P.sb("ht", [128, D], BF16)
    yb = P.sb("yb", [128, D], BF16)
    tA = P.sb("tA", [128, 512], F32)
    tB = P.sb("tB", [128, 512], F32)
    hb = P.sb("hb", [128, 512], BF16)
    hT4 = P.sb("hT4", [128, 4, 128], BF16)
    ps1 = [P.ps("ps1_%d" % i, [128, 512], F32) for i in range(2)]
    ps2 = [P.ps("ps2_%d" % i, [128, 512], F32) for i in range(2)]
    ps3 = [P.ps("ps3_%d" % i, [128, 512], F32) for i in range(2)]
    psT = P.ps("psT", [128, 1024], BF16)
    P.dma("sp", lambda e: e.dma_start(out=ident.t[:], in_=ident_d.t[:, :]), [ident_d], [ident], ident)
    P.dma("sp", lambda e: e.dma_start(out=wsel.t[:], in_=wsel_d.t[:, :].rearrange("(t p) e -> p t e", p=128)), [wsel_d], [wsel], wsel)
    wi = 0
    k3 = 0
    for ch in range(ntile // BCH):
        for tt in range(BCH):
            t = ch * BCH + tt
            P.dma("sp", lambda e: e.dma_start(out=ht.t[:], in_=h2.t[t * 128:(t + 1) * 128, :]), [h2], [ht], ht)
            for half in range(2):
                for j in range(8):
                    kc = half * 8 + j
                    P.op("pe", lambda e: e.transpose(psT.t[:, j * 128:(j + 1) * 128], ht.t[:, kc * 128:(kc + 1) * 128], ident.t[:]), [ht, ident], [psT], inc=(j == 7))
                P.op("act", lambda e: e.copy(out=h2T.t[:, half * 8:(half + 1) * 8, tt * 128:(tt + 1) * 128], in_=psT.t[:, :].rearrange("p (k n) -> p k n", n=128)), [psT], [h2T])
        for ex in range(8):
            g_, u_, d_ = wg[wi % 2], wu[wi % 2], wd[wi % 2]
            wi += 1
            P.dma("pool", lambda e: e.dma_start(out=g_.t[:], in_=wg_d.t[ex, :, :].rearrange("(kc p) n -> p kc n", p=128)), [wg_d], [g_], g_)
            P.dma("pool", lambda e: e.dma_start(out=u_.t[:], in_=wu_d.t[ex, :, :].rearrange("(kc p) n -> p kc n", p=128)), [wu_d], [u_], u_)
            P.dma("pool", lambda e: e.dma_start(out=d_.t[:], in_=wd_d.t[ex, :, :].rearrange("(c p) n -> p c n", p=128)), [wd_d], [d_], d_)
            for tt in range(BCH):
                t = ch * BCH + tt
                p1, p2 = ps1[tt % 2], ps2[tt % 2]
                for kc in range(KC):
                    P.op("pe", lambda e: e.matmul(p1.t[:, :], lhsT=h2T.t[:, kc, tt * 128:(tt + 1) * 128], rhs=g_.t[:, kc, :], start=(kc == 0), stop=(kc == KC - 1)),
                         [h2T, g_], [p1], inc=(kc == KC - 1))
                for kc in range(KC):
                    P.op("pe", lambda e: e.matmul(p2.t[:, :], lhsT=h2T.t[:, kc, tt * 128:(tt + 1) * 128], rhs=u_.t[:, kc, :], start=(kc == 0), stop=(kc == KC - 1)),
                         [h2T, u_], [p2], inc=(kc == KC - 1))
                P.op("act", lambda e: e.activation(out=tA.t[:], in_=p1.t[:, :], func=AF.Sigmoid), [p1], [tA])
                P.op("dve", lambda e: e.tensor_tensor(out=tB.t[:], in0=p1.t[:, :], in1=tA.t[:], op=ALU.mult), [p1, tA], [tB])
                P.op("dve", lambda e: e.scalar_tensor_tensor(out=hb.t[:], in0=tB.t[:], scalar=wsel.t[:, t, ex:ex + 1], in1=p2.t[:, :], op0=ALU.mult, op1=ALU.mult),
                     [tB, wsel, p2], [hb])
                for j in range(4):
                    P.op("pe", lambda e: e.transpose(psT.t[:, j * 128:(j + 1) * 128], hb.t[:, j * 128:(j + 1) * 128], ident.t[:]), [hb, ident], [psT], inc=(j == 3))
                P.op("act", lambda e: e.copy(out=hT4.t[:], in_=psT.t[:, 0:512].rearrange("p (k n) -> p k n", n=128)), [psT], [hT4])
                for og in range(4):
                    p3 = ps3[k3 % 2]
                    k3 += 1
                    for hc in range(4):
                        P.op("pe", lambda e: e.matmul(p3.t[:, :], lhsT=hT4.t[:, hc, :], rhs=d_.t[:, hc, og * 512:(og + 1) * 512], start=(hc == 0), stop=(hc == 3)),
                             [hT4, d_], [p3], inc=(hc == 3))
                    ya = yacc.t[:, tt, og * 512:(og + 1) * 512]
                    if ex == 0:
                        P.op("dve", lambda e: e.tensor_copy(out=ya, in_=p3.t[:, :]), [p3], [yacc])
                    else:
                        P.op("dve", lambda e: e.tensor_tensor(out=ya, in0=ya, in1=p3.t[:, :], op=ALU.add), [p3, yacc], [yacc])
        for tt in range(BCH):
            t = ch * BCH + tt
            P.op("act", lambda e: e.copy(out=yb.t[:], in_=yacc.t[:, tt, :]), [yacc], [yb])
            P.dma("sp", lambda e: e.dma_start(out=out.t[t * 128:(t + 1) * 128, :], in_=yb.t[:]), [yb], [out], yb)
    P.finish()
    return nc


CAPB = 8
NBLK = 8 * CAPB
NSLOT = NBLK * 128
BIGF = 1.0e6


def build_B2():
    nc = bass.Bass("TRN2", target_bir_lowering=False)
    P = Prog(nc)
    ntile = NTOK // 128
    h2 = P.dram("h2", [NTOK, D], BF16, "ExternalInput")
    wsel_d = P.dram("wsel", [NTOK, 8], F32, "ExternalInput")
    wg_d = P.dram("wg", [8, D, 512], F32, "ExternalInput")
    wu_d = P.dram("wu", [8, D, 512], F32, "ExternalInput")
    wd_d = P.dram("wd", [8, 512, D], F32, "ExternalInput")
    cst_d = P.dram("cst", [128, 3, 128], BF16, "ExternalInput")
    thr_d = P.dram("thr", [128, 8], F32, "ExternalInput")
    out = P.dram("contrib", [NTOK, D], BF16, "ExternalOutput")
    xd = P.dram("xd", [NSLOT, D], BF16)
    yd = P.dram("yd", [NSLOT, D], BF16)

    cst = P.sb("cst_sb", [128, 3, 128], BF16)
    ident, ones, utri = (cst.t[:, i, :] for i in range(3))
    thr = P.sb("thr_sb", [128, 8], F32)
    Wt = P.sb("Wt", [128, ntile, 8], F32)
    Mb = P.sb("Mb", [128, ntile * 8], BF16)
    Mf = P.sb("Mf", [128, ntile, 8], F32)
    S = P.sb("S", [128, ntile, 8], F32)
    tot = P.sb("tot", [128, ntile, 8], F32)
    carry = P.sb("carry", [128, ntile + 1, 8], F32)
    slA = P.sb("slA", [128, ntile], F32)
    slB = P.sb("slB", [128, ntile], F32)
    wA = P.sb("wA", [128, ntile], F32)
    wB = P.sb("wB", [128, ntile], F32)
    slAi = P.sb("slAi", [128, ntile], I32)
    slBi = P.sb("slBi", [128, ntile], I32)
    ht = [P.sb("ht%d" % i, [128, D], BF16) for i in range(2)]
    ya = [P.sb("ya%d" % i, [128, D], BF16) for i in range(2)]
    yb = [P.sb("yb%d" % i, [128, D], BF16) for i in range(2)]
    yo = [P.sb("yo%d" % i, [128, D], BF16) for i in range(2)]
    xT = P.sb("xT", [128, KC, 128], BF16)
    wg = [P.sb("wgS%d" % i, [128, KC, 512], BF16) for i in range(2)]
    wu = [P.sb("wuS%d" % i, [128, KC, 512], BF16) for i in range(2)]
    wd = [P.sb("wdS%d" % i, [128, 4, D], BF16) for i in range(2)]
    tA = P.sb("tA", [128, 512], F32)
    tB = P.sb("tB", [128, 512], F32)
    tC = P.sb("tC", [128, D], F32)
    hb = P.sb("hb", [128, 512], BF16)
    hT4 = P.sb("hT4", [128, 4, 128], BF16)
    ps1 = [P.ps("ps1_%d" % i, [128, 512], F32) for i in range(2)]
    ps2 = [P.ps("ps2_%d" % i, [128, 512], F32) for i in range(2)]
    ps3 = [P.ps("ps3_%d" % i, [128, 512], F32) for i in range(2)]
    psT = P.ps("psT", [128, 1024], BF16)
    psX = P.ps("psX", [128, 512], F32)
    bnd = nc.gpsimd.to_reg(NSLOT - 1)

    P.dma("sp", lambda e: e.dma_start(out=cst.t[:], in_=cst_d.t[:, :, :]), [cst_d], [cst], cst)
    P.dma("sp", lambda e: e.dma_start(out=thr.t[:], in_=thr_d.t[:, :]), [thr_d], [thr], thr)
    P.dma("sp", lambda e: e.dma_start(out=Wt.t[:], in_=wsel_d.t[:, :].rearrange("(t p) e -> p t e", p=128)), [wsel_d], [Wt], Wt)

    def load_expert(ex):
        g_, u_, d_ = wg[ex % 2], wu[ex % 2], wd[ex % 2]
        P.dma("pool", lambda e: e.dma_start(out=g_.t[:], in_=wg_d.t[ex, :, :].rearrange("(kc p) n -> p kc n", p=128)), [wg_d], [g_], g_)
        P.dma("pool", lambda e: e.dma_start(out=u_.t[:], in_=wu_d.t[ex, :, :].rearrange("(kc p) n -> p kc n", p=128)), [wu_d], [u_], u_)
        P.dma("pool", lambda e: e.dma_start(out=d_.t[:], in_=wd_d.t[ex, :, :].rearrange("(c p) n -> p c n", p=128)), [wd_d], [d_], d_)
    load_expert(0)
    load_expert(1)
    Wt2 = Wt.t[:].rearrange("p t e -> p (t e)")
    Mf2 = Mf.t[:].rearrange("p t e -> p (t e)")
    S2 = S.t[:].rearrange("p t e -> p (t e)")
    tot2 = tot.t[:].rearrange("p t e -> p (t e)")
    P.op("dve", lambda e: e.tensor_single_scalar(out=Mf2, in_=Wt2, scalar=0.0, op=ALU.is_gt), [Wt], [Mf])
    P.op("dve", lambda e: e.tensor_copy(out=Mb.t[:], in_=Mf2), [Mf], [Mb])
    for (c0, cn) in [(0, 512), (512, 64)]:
        P.op("pe", lambda e: e.matmul(psX.t[:, 0:cn], lhsT=utri, rhs=Mb.t[:, c0:c0 + cn], start=True, stop=True), [cst, Mb], [psX])
        P.op("dve", lambda e: e.tensor_copy(out=S2[:, c0:c0 + cn], in_=psX.t[:, 0:cn]), [psX], [S])
        P.op("pe", lambda e: e.matmul(psX.t[:, 0:cn], lhsT=ones, rhs=Mb.t[:, c0:c0 + cn], start=True, stop=True), [cst, Mb], [psX])
        P.op("dve", lambda e: e.tensor_copy(out=tot2[:, c0:c0 + cn], in_=psX.t[:, 0:cn]), [psX], [tot])
    P.op("dve", lambda e: e.memset(carry.t[:, 0, :], 0.0), [], [carry])
    for i in range(ntile):
        P.op("dve", lambda e: e.tensor_tensor(out=carry.t[:, i + 1, :], in0=carry.t[:, i, :], in1=tot.t[:, i, :], op=ALU.add), [carry, tot], [carry])
    P.op("dve", lambda e: e.tensor_tensor(out=S.t[:], in0=S.t[:], in1=carry.t[:, 0:ntile, :], op=ALU.add), [S, carry], [S])
    P.op("dve", lambda e: e.tensor_single_scalar(out=tot2, in_=S2, scalar=float(CAPB * 128), op=ALU.is_lt), [S], [tot])
    P.op("dve", lambda e: e.tensor_tensor(out=Mf2, in0=Mf2, in1=tot2, op=ALU.mult), [Mf, tot], [Mf])
    for i in range(ntile):
        P.op("dve", lambda e: e.tensor_tensor(out=S.t[:, i, :], in0=S.t[:, i, :], in1=thr.t[:, :], op=ALU.add), [S, thr], [S])
    P.op("dve", lambda e: e.tensor_scalar(out=tot2, in0=S2, scalar1=-BIGF, scalar2=None, op0=ALU.add), [S], [tot])
    P.op("dve", lambda e: e.tensor_tensor(out=tot2, in0=tot2, in1=Mf2, op=ALU.mult), [tot, Mf], [tot])
    P.op("dve", lambda e: e.tensor_scalar(out=tot2, in0=tot2, scalar1=BIGF, scalar2=None, op0=ALU.add), [tot], [tot])
    P.op("dve", lambda e: e.tensor_reduce(out=slA.t[:], in_=tot.t[:], axis=AX.X, op=ALU.min), [tot], [slA])
    P.op("dve", lambda e: e.tensor_scalar(out=tot2, in0=S2, scalar1=1.0, scalar2=None, op0=ALU.add), [S], [tot])
    P.op("dve", lambda e: e.tensor_tensor(out=tot2, in0=tot2, in1=Mf2, op=ALU.mult), [tot, Mf], [tot])
    P.op("dve", lambda e: e.tensor_scalar(out=tot2, in0=tot2, scalar1=-1.0, scalar2=None, op0=ALU.add), [tot], [tot])
    P.op("dve", lambda e: e.tensor_reduce(out=slB.t[:], in_=tot.t[:], axis=AX.X, op=ALU.max), [tot], [slB])
    for (sl, wv) in ((slA, wA), (slB, wB)):
        for i in range(ntile):
            P.op("dve", lambda e: e.tensor_scalar(out=tot.t[:, i, :], in0=S.t[:, i, :], scalar1=sl.t[:, i:i + 1], scalar2=None, op0=ALU.is_equal), [S, sl], [tot])
        P.op("dve", lambda e: e.tensor_tensor(out=tot2, in0=tot2, in1=Wt2, op=ALU.mult), [tot, Wt], [tot])
        P.op("dve", lambda e: e.tensor_reduce(out=wv.t[:], in_=tot.t[:], axis=AX.X, op=ALU.add), [tot], [wv])
    P.op("dve", lambda e: e.tensor_copy(out=slAi.t[:], in_=slA.t[:]), [slA], [slAi])
    P.op("dve", lambda e: e.tensor_copy(out=slBi.t[:], in_=slB.t[:]), [slB], [slBi])

    for i in range(ntile):
        hb_ = ht[i % 2]
        P.dma("sp", lambda e: e.dma_start(out=hb_.t[:], in_=h2.t[i * 128:(i + 1) * 128, :]), [h2], [hb_], hb_)
        for sli in (slAi, slBi):
            P.dma("pool", lambda e: e.indirect_dma_start(
                out=xd.t[:, :], out_offset=bass.IndirectOffsetOnAxis(ap=sli.t[:, i:i + 1], axis=0),
                in_=hb_.t[:, :], in_offset=None, bounds_check=bnd, oob_is_err=False), [hb_, sli], [xd], hb_)

    k3 = 0
    for b in range(NBLK):
        ex = b // CAPB
        if b % CAPB == 0 and ex >= 2:
            load_expert(ex)
        g_, u_, d_ = wg[ex % 2], wu[ex % 2], wd[ex % 2]
        xb_ = ht[b % 2]
        P.dma("sp", lambda e: e.dma_start(out=xb_.t[:], in_=xd.t[b * 128:(b + 1) * 128, :]), [xd], [xb_], xb_)
        for half in range(2):
            for j in range(8):
                kc = half * 8 + j
                P.op("pe", lambda e: e.transpose(psT.t[:, j * 128:(j + 1) * 128], xb_.t[:, kc * 128:(kc + 1) * 128], ident), [xb_, cst], [psT], inc=(j == 7))
            P.op("act", lambda e: e.copy(out=xT.t[:, half * 8:(half + 1) * 8, :], in_=psT.t[:, :].rearrange("p (k n) -> p k n", n=128)), [psT], [xT])
        p1, p2 = ps1[b % 2], ps2[b % 2]
        for kc in range(KC):
            P.op("pe", lambda e: e.matmul(p1.t[:, :], lhsT=xT.t[:, kc, :], rhs=g_.t[:, kc, :], start=(kc == 0), stop=(kc == KC - 1)), [xT, g_], [p1], inc=(kc == KC - 1))
        for kc in range(KC):
            P.op("pe", lambda e: e.matmul(p2.t[:, :], lhsT=xT.t[:, kc, :], rhs=u_.t[:, kc, :], start=(kc == 0), stop=(kc == KC - 1)), [xT, u_], [p2], inc=(kc == KC - 1))
        P.op("act", lambda e: e.activation(out=tA.t[:], in_=p1.t[:, :], func=AF.Sigmoid), [p1], [tA])
        P.op("dve", lambda e: e.tensor_tensor(out=tB.t[:], in0=p1.t[:, :], in1=tA.t[:], op=ALU.mult), [p1, tA], [tB])
        P.op("dve", lambda e: e.tensor_tensor(out=hb.t[:], in0=tB.t[:], in1=p2.t[:, :], op=ALU.mult), [tB, p2], [hb])
        for j in range(4):
            P.op("pe", lambda e: e.transpose(psT.t[:, j * 128:(j + 1) * 128], hb.t[:, j * 128:(j + 1) * 128], ident), [hb, cst], [psT], inc=(j == 3))
        P.op("act", lambda e: e.copy(out=hT4.t[:], in_=psT.t[:, 0:512].rearrange("p (k n) -> p k n", n=128)), [psT], [hT4])
        yo_ = yo[b % 2]
        for og in range(4):
            p3 = ps3[k3 % 2]
            k3 += 1
            for hc in range(4):
                P.op("pe", lambda e: e.matmul(p3.t[:, :], lhsT=hT4.t[:, hc, :], rhs=d_.t[:, hc, og * 512:(og + 1) * 512], start=(hc == 0), stop=(hc == 3)), [hT4, d_], [p3], inc=(hc == 3))
            if og % 2 == 0:
                P.op("act", lambda e: e.copy(out=yo_.t[:, og * 512:(og + 1) * 512], in_=p3.t[:, :]), [p3], [yo_])
            else:
                P.op("dve", lambda e: e.tensor_copy(out=yo_.t[:, og * 512:(og + 1) * 512], in_=p3.t[:, :]), [p3], [yo_])
        P.dma("sp", lambda e: e.dma_start(out=yd.t[b * 128:(b + 1) * 128, :], in_=yo_.t[:]), [yo_], [yd], yo_)

    for i in range(ntile):
        ya_, yb_, yo_ = ya[i % 2], yb[i % 2], yo[i % 2]
        P.op("pool", lambda e: e.memset(ya_.t[:], 0.0), [], [ya_])
        P.op("pool", lambda e: e.memset(yb_.t[:], 0.0), [], [yb_])
        P.dma("pool", lambda e: e.indirect_dma_start(out=ya_.t[:, :], out_offset=None, in_=yd.t[:, :],
                                                     in_offset=bass.IndirectOffsetOnAxis(ap=slAi.t[:, i:i + 1], axis=0),
                                                     bounds_check=bnd, oob_is_err=False), [yd, slAi], [ya_], ya_)
        P.dma("pool", lambda e: e.indirect_dma_start(out=yb_.t[:, :], out_offset=None, in_=yd.t[:, :],
                                                     in_offset=bass.IndirectOffsetOnAxis(ap=slBi.t[:, i:i + 1], axis=0),
                                                     bounds_check=bnd, oob_is_err=False), [yd, slBi], [yb_], yb_)
        P.op("dve", lambda e: e.tensor_scalar(out=tC.t[:], in0=ya_.t[:], scalar1=wA.t[:, i:i + 1], scalar2=None, op0=ALU.mult), [ya_, wA], [tC])
        P.op("dve", lambda e: e.scalar_tensor_tensor(out=yo_.t[:], in0=yb_.t[:], scalar=wB.t[:, i:i + 1], in1=tC.t[:], op0=ALU.mult, op1=ALU.add), [yb_, wB, tC], [yo_])
        P.dma("sp", lambda e: e.dma_start(out=out.t[i * 128:(i + 1) * 128, :], in_=yo_.t[:]), [yo_], [out], yo_)
    P.finish()
    return nc


def _b2_consts():
    import ml_dtypes
    c = np.zeros((128, 3, 128), np.float32)
    c[:, 0, :] = np.eye(128)
    c[:, 1, :] = 1.0
    c[:, 2, :] = np.triu(np.ones((128, 128), np.float32), 1)
    thr = np.zeros((128, 8), np.float32)
    thr[:, :] = (np.arange(8) * CAPB * 128)[None, :]
    return c.astype(ml_dtypes.bfloat16), thr


def build_C(final):
    nc = bass.Bass("TRN2", target_bir_lowering=False)
    P = Prog(nc)
    x1 = P.dram("x1", [NOWN, D], F32, "ExternalInput")
    cb = P.dram("cb", [8, NOWN, D], BF16, "ExternalInput")
    rows = P.dram("rows", [3, D], F32, "ExternalInput")
    out = P.dram("x2", [NOWN, D], F32, "ExternalOutput")
    xt = [P.sb("xt%d" % i, [128, D], F32) for i in range(2)]
    cbs = [P.sb("cbs%d" % i, [128, 8, D], BF16) for i in range(2)]
    acc = P.sb("acc", [128, D], F32)
    sq = P.sb("sq", [128, D], BF16)
    st = P.sb("st", [128, 4], F32)
    rep = [P.sb("rep%d" % i, [128, D], F32) for i in range(3)]
    for i in range(3):
        P.dma("sp", lambda e: e.dma_start(out=rep[i].t[:], in_=rows.t[i:i + 1, :].partition_broadcast(128)), [rows], [rep[i]], rep[i])
    for t in range(9):
        xb, cbb = xt[t % 2], cbs[t % 2]
        who = 1 if t == 0 else 0
        P.dma("sp", lambda e: e.dma_start(out=xb.t[:], in_=x1.t[t * 128:(t + 1) * 128, :]), [x1], [xb], xb)
        P.dma("sp", lambda e: e.dma_start(out=cbb.t[:], in_=cb.t[:, t * 128:(t + 1) * 128, :].rearrange("g p d -> p g d")), [cb], [cbb], cbb)
        P.op("dve", lambda e: e.tensor_tensor(out=acc.t[:], in0=cbb.t[:, 0, :], in1=cbb.t[:, 1, :], op=ALU.add), [cbb], [acc])
        for g in range(2, 8):
            P.op("dve", lambda e: e.tensor_tensor(out=acc.t[:], in0=acc.t[:], in1=cbb.t[:, g, :], op=ALU.add), [cbb, acc], [acc])
        P.op("dve", lambda e: e.tensor_tensor(out=acc.t[:], in0=acc.t[:], in1=rep[who].t[:], op=ALU.mult), [acc, rep[who]], [acc])
        P.op("dve", lambda e: e.tensor_tensor(out=xb.t[:], in0=xb.t[:], in1=acc.t[:], op=ALU.add), [xb, acc], [xb])
        if final:
            P.op("act", lambda e: e.activation(out=sq.t[:], in_=xb.t[:], func=AF.Square, accum_out=st.t[:, 0:1]), [xb], [sq, st])
            P.op("dve", lambda e: e.tensor_scalar(out=st.t[:, 1:2], in0=st.t[:, 0:1], scalar1=1.0 / D, scalar2=EPS, op0=ALU.mult, op1=ALU.add), [st], [st])
            P.op("act", lambda e: e.activation(out=st.t[:, 1:2], in_=st.t[:, 1:2], func=AF.Sqrt), [st], [st])
            P.op("dve", lambda e: e.reciprocal(out=st.t[:, 1:2], in_=st.t[:, 1:2]), [st], [st])
            P.op("dve", lambda e: e.scalar_tensor_tensor(out=xb.t[:], in0=xb.t[:], scalar=st.t[:, 1:2], in1=rep[2].t[:], op0=ALU.mult, op1=ALU.mult), [xb, st, rep[2]], [xb])
        P.dma("sp", lambda e: e.dma_start(out=out.t[t * 128:(t + 1) * 128, :], in_=xb.t[:]), [xb], [out], xb)
    P.finish()
    return nc


def _fm(v, n):
    return np.ascontiguousarray(np.asarray(v, np.float32).reshape(n, 128).T)


def _rope_tab(rot_dim):
    q = rot_dim // 4
    t = np.arange(2048)
    rows = (t // 64).astype(np.float32)
    cols = (t % 64).astype(np.float32)
    inv = (np.float32(10000.0) ** (-np.arange(q, dtype=np.float32) / np.float32(q))).astype(np.float32)
    ar = rows[None, :] * inv[:, None]
    ac = cols[None, :] * inv[:, None]
    ang = np.concatenate([ar, ar, ac, ac], 0).astype(np.float32)
    C = np.cos(ang).astype(np.float32)
    S = np.sin(ang).astype(np.float32)
    R = np.zeros((rot_dim, rot_dim), np.float32)
    for base in (0, 2 * q):
        for i in range(q):
            R[base + i, base + i + q] = -1.0
            R[base + i + q, base + i] = 1.0
    return C, S, R


def _consts():
    import ml_dtypes
    c = np.zeros((128, 4, 128), np.float32)
    c[:, 0, :] = np.eye(128)
    c[:, 1, :] = 1.0
    _, _, RB = _rope_tab(128)
    _, _, RA = _rope_tab(64)
    c[:, 2, :] = RB.T
    c[:64, 3, :64] = RA.T
    return c.astype(ml_dtypes.bfloat16)


def _nab(rpb):
    out = np.full((128, 4, 15, 64), -30000.0, np.float32)
    qc = np.arange(64)
    cs = np.clip(qc - 8, 0, 48)
    kc = np.arange(64)
    col_in = (kc[:, None] >= cs[None, :]) & (kc[:, None] < cs[None, :] + 16)
    dc = np.clip(kc[:, None] - qc[None, :] + 15, 0, 30)
    for a in range(2):
        for m in range(15):
            dr = m - 7 + a
            if dr < -7 or dr > 7:
                continue
            for h in range(4):
                vals = rpb[h, dr + 7][dc]
                out[a * 64:(a + 1) * 64, h, m, :] = np.where(col_in, vals, np.float32(-30000.0))
    return out


def _ind(hf):
    out = np.zeros((128, 16, 6), np.float32)
    base = 16 * hf
    for lr in range(16):
        r = base + lr
        r0 = min(max(r - 4, 0), 24)
        Rs = lr if lr <= 12 else 12
        R0 = Rs - Rs % 2
        for s in range(6):
            for a in range(2):
                ab = base - 4 + R0 + 2 * s + a
                if 0 <= ab <= 31 and r0 <= ab <= r0 + 7:
                    out[a * 64:(a + 1) * 64, lr, s] = 1.0
    return out


_CACHE = {}


def _prog(name, fn):
    if name not in _CACHE:
        _CACHE[name] = fn()
    return _CACHE[name]


def kernel(x, c, ctx, c_ctx, w_mod, b_mod, g_mix, g_ffn, w_in, w_uq, g_qa, w_ukv, g_kva, g_qn, g_kn,
           rpb, w_conv, w_branch, w_o, w_group, b_group, w_router, b_router, w_gate_e, w_up_e, w_down_e, g_final):
    f32 = np.float32
    cores = list(range(NCORES))
    x = np.asarray(x, f32)
    ctx = np.asarray(ctx, f32)
    c5 = np.concatenate([np.asarray(c, f32), np.asarray(c_ctx, f32)[None]], 0)
    cT = np.ascontiguousarray(c5.T.reshape(KC, 128, 5).transpose(1, 0, 2))
    ims = [{"cT": cT, "wm": np.ascontiguousarray(w_mod[:, :, i * MCOL:(i + 1) * MCOL]), "bm": np.ascontiguousarray(b_mod[:, i * MCOL:(i + 1) * MCOL])}
           for i in cores]
    res = run_bass_kernel_spmd(_prog("M", build_M), ims, core_ids=cores)
    mod = np.concatenate([r["mod"] for r in res.results], axis=2)
    consts = _consts()
    CA_, SA_, _ = _rope_tab(64)
    CB_, SB_, _ = _rope_tab(128)
    xlat, xctx = x, ctx
    for l in range(2):
        nab = _nab(np.asarray(rpb[l], f32))
        wr = np.ascontiguousarray(np.concatenate([w_group[l], w_router[l]], 1), f32)
        brow = np.concatenate([b_group[l], b_router[l]])[None].astype(f32)
        ims = []
        for cid in cores:
            b, hf = cid // 2, cid % 2
            o = 1 - hf
            x_own = np.concatenate([xctx[b, hf * 128:(hf + 1) * 128], xlat[b, hf * 1024:(hf + 1) * 1024]], 0)
            x_oth = np.concatenate([xctx[b, o * 128:(o + 1) * 128], xlat[b, o * 1024:(o + 1) * 1024]], 0)
            ml = mod[l, b].reshape(6, D)
            mc = mod[l, 4].reshape(6, D)
            fm = np.zeros((128, FM_N), f32)
            fm[:, FM_GMIX:FM_GMIX + 16] = _fm(g_mix[l], 16)
            fm[:, FM_SC1:FM_SC1 + 16] = _fm(ml[1], 16)
            fm[:, FM_SC1 + 16:FM_SC1 + 32] = _fm(mc[1], 16)
            fm[:, FM_SH1:FM_SH1 + 16] = _fm(ml[0], 16)
            fm[:, FM_SH1 + 16:FM_SH1 + 32] = _fm(mc[0], 16)
            fm[:, FM_GQA:FM_GQA + 4] = _fm(g_qa[l], 4)
            fm[:, FM_GKVA:FM_GKVA + 4] = _fm(g_kva[l], 4)
            fm[:, FM_GQN] = g_qn[l]
            fm[:, FM_GKN] = g_kn[l]
            for ci in range(4):
                for tap in range(3):
                    fm[:, FM_WCONV + ci * 3 + tap] = w_conv[l, tap, ci * 128:(ci + 1) * 128]
            fm[:, FM_FLAGB] = 1.0 if hf == 1 else 0.0
            fm[:, FM_FLAGA] = 1.0 if hf == 0 else 0.0
            rows = np.stack([np.asarray(g_ffn[l], f32), ml[4], mc[4], ml[3], mc[3], ml[2], mc[2]]).astype(f32)
            so, sn = slice(hf * 1024, (hf + 1) * 1024), slice(o * 1024, (o + 1) * 1024)
            ropeA = np.ascontiguousarray(np.stack([np.stack([CA_[:, so], CA_[:, sn]], 1), np.stack([SA_[:, so], SA_[:, sn]], 1)], 1))
            ropeB = np.ascontiguousarray(np.stack([np.stack([CB_[:, so], CB_[:, sn]], 1), np.stack([SB_[:, so], SB_[:, sn]], 1)], 1))
            ims.append({"x_own": x_own, "x_oth": x_oth, "w_in": w_in[l], "w_uq": w_uq[l], "w_ukv": w_ukv[l], "fm": fm, "rows": rows,
                        "ropeA": ropeA, "ropeB": ropeB, "consts": consts, "nab": nab, "ind": _ind(hf), "wr": wr, "brow": brow,
                        "w_branch": w_branch[l], "w_o": w_o[l]})
        resA = run_bass_kernel_spmd(_prog("A", build_A), ims, core_ids=cores).results
        h2_all = np.concatenate([r["h2"] for r in resA], 0)
        wr_all = np.concatenate([r["wrout"] for r in resA], 0)
        cstB, thrB = _b2_consts()
        ims = [{"h2": h2_all, "wsel": np.ascontiguousarray(wr_all[:, 8 * g:8 * g + 8]), "wg": w_gate_e[l, 8 * g:8 * g + 8], "wu": w_up_e[l, 8 * g:8 * g + 8],
                "wd": w_down_e[l, 8 * g:8 * g + 8], "cst": cstB, "thr": thrB} for g in cores]
        resB = run_bass_kernel_spmd(_prog("B2", build_B2), ims, core_ids=cores).results
        ims = []
        for cid in cores:
            b = cid // 2
            cbk = np.stack([resB[g]["contrib"][cid * NOWN:(cid + 1) * NOWN] for g in cores], 0)
            rows = np.stack([mod[l, b].reshape(6, D)[5], mod[l, 4].reshape(6, D)[5], np.asarray(g_final, f32)]).astype(f32)
            ims.append({"x1": resA[cid]["x1"], "cb": cbk, "rows": rows})
        final = (l == 1)
        resC = run_bass_kernel_spmd(_prog("C%d" % final, lambda: build_C(final)), ims, core_ids=cores).results
        nl = np.empty_like(xlat)
        ncx = np.empty_like(xctx)
        for cid in cores:
            b, hf = cid // 2, cid % 2
            ncx[b, hf * 128:(hf + 1) * 128] = resC[cid]["x2"][0:128]
            nl[b, hf * 1024:(hf + 1) * 1024] = resC[cid]["x2"][128:]
        xlat, xctx = nl, ncx
    return xlat
```

```python
import numpy as np
import concourse.bass as bass
import concourse.mybir as mybir
from concourse.bass_utils import run_bass_kernel_spmd

F32 = mybir.dt.float32
BF16 = mybir.dt.bfloat16
I32 = mybir.dt.int32
U32 = mybir.dt.uint32
AF = mybir.ActivationFunctionType
ALU = mybir.AluOpType
AX = mybir.AxisListType

D = 2048
KC = 16
NCORES = 8
EPS = 1e-6
IN_COLS = 13376
CA, CB, CC, CD, CG = 0, 1088, 2112, 3648, 5184


SAME_ENGINE_SYNC = True


class Buf:
    def __init__(self, name, t):
        self.name = name
        self.t = t
        self.w = None
        self.r = []
        self.sem = None
        self.cnt = 0
        self.multi = False
        self.wl = {}


class Prog:
    def __init__(self, nc):
        self.nc = nc
        self.eng = {"pe": nc.tensor, "act": nc.scalar, "dve": nc.vector, "pool": nc.gpsimd, "sp": nc.sync}
        self.sem = {k: nc.alloc_semaphore("sem_" + k) for k in ("pe", "act", "dve", "pool")}
        self.cnt = {k: 0 for k in ("pe", "act", "dve", "pool")}
        self.seen = {k: {} for k in self.eng}
        self.pend = {k: ([], []) for k in self.eng}
        self.dma_bufs = []
        self.nbuf = 0

    def sb(self, name, shape, dt):
        return Buf(name, self.nc.alloc_sbuf_tensor(name, list(shape), dt))

    def ps(self, name, shape, dt):
        return Buf(name, self.nc.alloc_psum_tensor(name, list(shape), dt))

    def dram(self, name, shape, dt, kind="Internal"):
        b = Buf(name, self.nc.dram_tensor(name, list(shape), dt, kind=kind).ap())
        b.multi = True
        return b

    @staticmethod
    def _wev(b):
        return list(b.wl.values()) if b.multi else [b.w]

    @staticmethod
    def _setw(b, ev):
        if b.multi:
            b.wl[ev[0]] = ev
        else:
            b.w = ev
        b.r = []

    def _wait(self, ek, evs):
        e = self.eng[ek]
        need = {}
        for ev in evs:
            if ev is None:
                continue
            sk, sem, val, src = ev
            if src == ek and (ek == "pe" or not SAME_ENGINE_SYNC):
                continue
            if self.seen[ek].get(sk, 0) >= val:
                continue
            if sk not in need or need[sk][1] < val:
                need[sk] = (sem, val)
        for sk, (sem, val) in need.items():
            e.wait_ge(sem, val)
            self.seen[ek][sk] = val

    def op(self, ek, fn, R=(), W=(), inc=True):
        evs = []
        for b in R:
            evs.extend(self._wev(b))
        for b in W:
            evs.extend(self._wev(b))
            evs.extend(b.r)
        self._wait(ek, evs)
        ins = fn(self.eng[ek])
        pr, pw = self.pend[ek]
        pr.extend(R)
        pw.extend(W)
        if not inc:
            return
        self.cnt[ek] += 1
        ev = ("e_" + ek, self.sem[ek], self.cnt[ek], ek)
        ins.then_inc(self.sem[ek], 1)
        for b in pr:
            b.r.append(ev)
        for b in pw:
            self._setw(b, ev)
        self.pend[ek] = ([], [])

    def dma(self, q, fn, R, W, sb):
        if sb.sem is None:
            sb.sem = self.nc.alloc_semaphore("dsem_%d" % len(self.dma_bufs))
            sb.semkey = "d_%d" % len(self.dma_bufs)
            self.dma_bufs.append(sb)
        evs = []
        for b in R:
            evs.extend(self._wev(b))
        for b in W:
            evs.extend(self._wev(b))
            evs.extend(b.r)
        if sb.cnt > 0:
            evs.append((sb.semkey, sb.sem, sb.cnt, "dma"))
        self._wait(q, evs)
        ins = fn(self.eng[q])
        sb.cnt += 16
        ins.then_inc(sb.sem, 16)
        ev = (sb.semkey, sb.sem, sb.cnt, "dma")
        for b in R:
            b.r.append(ev)
        for b in W:
            self._setw(b, ev)

    def finish(self):
        evs = [(b.semkey, b.sem, b.cnt, "dma") for b in self.dma_bufs]
        self._wait("sp", evs)


def bcast_rows(ap_row, nparts):
    return ap_row.partition_broadcast(nparts)


MCOL = 12288 // NCORES


def build_M():
    nc = bass.Bass("TRN2", target_bir_lowering=False)
    P = Prog(nc)
    cT = P.dram("cT", [128, KC, 5], F32, "ExternalInput")
    wm = P.dram("wm", [2, D, MCOL], F32, "ExternalInput")
    bm = P.dram("bm", [2, MCOL], F32, "ExternalInput")
    out = P.dram("mod", [2, 5, MCOL], F32, "ExternalOutput")
    c_sb = P.sb("c_sb", [128, KC, 5], F32)
    s_sb = P.sb("s_sb", [128, KC, 5], F32)
    wts = [P.sb("wt%d" % i, [128, KC, 512], F32) for i in range(2)]
    b_sb = P.sb("b_sb", [5, 2, MCOL], F32)
    o_sb = P.sb("o_sb", [5, 2, MCOL], F32)
    pss = [P.ps("ps%d" % i, [128, 512], F32) for i in range(2)]
    P.dma("sp", lambda e: e.dma_start(out=c_sb.t[:], in_=cT.t[:, :, :]), [cT], [c_sb], c_sb)
    for l in range(2):
        P.dma("sp", lambda e, l=l: e.dma_start(out=b_sb.t[:, l, :], in_=bm.t[l:l + 1, :].partition_broadcast(5)),
              [bm], [b_sb], b_sb)
    P.op("act", lambda e: e.activation(out=s_sb.t[:], in_=c_sb.t[:], func=AF.Sigmoid), [c_sb], [s_sb])
    P.op("dve", lambda e: e.tensor_tensor(out=s_sb.t[:], in0=s_sb.t[:], in1=c_sb.t[:], op=ALU.mult), [s_sb, c_sb], [s_sb])
    i = 0
    for l in range(2):
        for g in range(MCOL // 512):
            wt = wts[i % 2]
            ps = pss[i % 2]
            i += 1
            P.dma("sp", lambda e, wt=wt, l=l, g=g: e.dma_start(
                out=wt.t[:], in_=wm.t[l, :, g * 512:(g + 1) * 512].rearrange("(kc p) n -> p kc n", p=128)),
                [wm], [wt], wt)
            for kc in range(KC):
                P.op("pe", lambda e, wt=wt, ps=ps, kc=kc: e.matmul(
                    ps.t[0:5, :], lhsT=s_sb.t[:, kc, :], rhs=wt.t[:, kc, :], start=(kc == 0), stop=(kc == KC - 1)),
                    [s_sb, wt], [ps], inc=(kc == KC - 1))
            P.op("dve", lambda e, ps=ps, l=l, g=g: e.tensor_tensor(
                out=o_sb.t[:, l, g * 512:(g + 1) * 512], in0=ps.t[0:5, :], in1=b_sb.t[:, l, g * 512:(g + 1) * 512],
                op=ALU.add), [ps, b_sb], [o_sb])
    for l in range(2):
        P.dma("sp", lambda e, l=l: e.dma_start(out=out.t[l, :, :], in_=o_sb.t[:, l, :]), [o_sb], [out], o_sb)
    P.finish()
    return nc


NOWN = 1152
ARENA_N = 37376
DEBUG_Y = False
FM_GMIX, FM_SC1, FM_SH1, FM_GQA, FM_GKVA, FM_GQN, FM_GKN, FM_WCONV, FM_FLAGB, FM_FLAGA, FM_N = 0, 16, 48, 80, 84, 88, 89, 90, 102, 103, 104
ROW_GFFN, ROW_SC2, ROW_SH2, ROW_GA1 = 0, 1, 3, 5
MLA_SCALE = 192 ** -0.5
HD_SCALE = 128 ** -0.5
OWN_CHUNKS = [(0, 128), (128, 512), (640, 512)]


def build_A():
    nc = bass.Bass("TRN2", target_bir_lowering=False)
    P = Prog(nc)
    x_own = P.dram("x_own", [NOWN, D], F32, "ExternalInput")
    x_oth = P.dram("x_oth", [NOWN, D], F32, "ExternalInput")
    w_in = P.dram("w_in", [D, IN_COLS], F32, "ExternalInput")
    w_uq = P.dram("w_uq", [512, 768], F32, "ExternalInput")
    w_ukv = P.dram("w_ukv", [512, 1024], F32, "ExternalInput")
    fm_d = P.dram("fm", [128, FM_N], F32, "ExternalInput")
    rows_d = P.dram("rows", [7, D], F32, "ExternalInput")
    ropeA = P.dram("ropeA", [64, 2, 2, 1024], F32, "ExternalInput")
    ropeB = P.dram("ropeB", [128, 2, 2, 1024], F32, "ExternalInput")
    consts_d = P.dram("consts", [128, 4, 128], BF16, "ExternalInput")
    nab_d = P.dram("nab", [128, 4, 15, 64], F32, "ExternalInput")
    ind_d = P.dram("ind", [128, 16, 6], F32, "ExternalInput")
    wr_d = P.dram("wr", [D, 72], F32, "ExternalInput")
    brow_d = P.dram("brow", [1, 72], F32, "ExternalInput")
    w_br = P.dram("w_branch", [4, 512, D], F32, "ExternalInput")
    w_o = P.dram("w_o", [D, D], F32, "ExternalInput")
    x1_o = P.dram("x1", [NOWN, D], F32, "ExternalOutput")
    h2_o = P.dram("h2", [NOWN, D], BF16, "ExternalOutput")
    wr_o = P.dram("wrout", [NOWN, 64], F32, "ExternalOutput")

    arena2 = nc.alloc_sbuf_tensor("arena2", [128, 8 * 4 * NOWN], BF16)
    hT = Buf("hT", arena2[:, 0:KC * NOWN].rearrange("p (a b) -> p a b", b=NOWN))
    yT = [Buf("yT%d" % k, arena2[:, (KC + 4 * k) * NOWN:(KC + 4 * k + 4) * NOWN].rearrange("p (a b) -> p a b", b=NOWN)) for k in range(4)]
    arena = nc.alloc_sbuf_tensor("arena", [128, ARENA_N], BF16)
    wb = [P.sb("wb%d" % i, [128, KC, 256], BF16) for i in range(2)]
    consts = P.sb("consts_sb", [128, 4, 128], BF16)
    ident, ones, RBt, RAt = (consts.t[:, i, :] for i in range(4))
    fm = P.sb("fm_sb", [128, FM_N], F32)
    a1T = P.sb("a1T", [128, 32], F32)
    ind = P.sb("ind_sb", [128, 16, 6], F32)
    xt = [P.sb("xt%d" % i, [128, D], F32) for i in range(1)]
    xs = [P.sb("xs%d" % i, [128, D], BF16) for i in range(1)]
    st = [P.sb("st%d" % i, [128, 4], F32) for i in range(2)]
    ck = P.sb("ck", [128, 4, 512], BF16)
    sq = P.sb("sq", [128, 4, 512], BF16)
    rr = P.sb("rr", [128, 512], F32)
    tA = P.sb("tA", [128, 512], F32)
    tB = P.sb("tB", [128, 512], F32)
    cs = [P.sb("cs%d" % i, [128, 2, 512], F32) for i in range(1)]
    Pt = [P.sb("Pt%d" % i, [128, 512], BF16) for i in range(2)]
    psM = [P.ps("psM%d" % i, [128, 512], F32) for i in range(2)]
    psS = [P.ps("psS%d" % i, [128, 512], F32) for i in range(2)]
    psO = P.ps("psO", [128, 512], F32)
    psSum = P.ps("psSum", [128, 512], F32)
    psT = P.ps("psT", [128, 1024], BF16)
    psX = P.ps("psX", [128, 512], F32)

    state = {"wi": 0, "pm": 0, "xi": 0, "pt": 0, "ps": 0}

    def arena_buf(name, off, shape):
        n = 1
        for s in shape[1:]:
            n *= s
        ap = arena[:, off:off + n]
        if len(shape) == 3:
            ap = ap.rearrange("p (a b) -> p a b", b=shape[2])
        b = Buf(name, None)
        b.ap = ap
        return b, off + n

    P.dma("sp", lambda e: e.dma_start(out=consts.t[:], in_=consts_d.t[:, :, :]), [consts_d], [consts], consts)
    P.dma("sp", lambda e: e.dma_start(out=fm.t[:], in_=fm_d.t[:, :]), [fm_d], [fm], fm)
    P.dma("sp", lambda e: e.dma_start(out=ind.t[:], in_=ind_d.t[:, :, :]), [ind_d], [ind], ind)
    for who in range(2):
        P.op("dve", lambda e, who=who: e.scalar_tensor_tensor(
            out=a1T.t[:, who * 16:(who + 1) * 16], in0=fm.t[:, FM_SC1 + who * 16:FM_SC1 + (who + 1) * 16], scalar=1.0,
            in1=fm.t[:, FM_GMIX:FM_GMIX + 16], op0=ALU.add, op1=ALU.mult), [fm], [a1T])
    def rstd_from_ss(ss_ap, n, inv, Rb, out_ap, Wb):
        P.op("dve", lambda e: e.tensor_scalar(out=out_ap, in0=ss_ap, scalar1=inv, scalar2=EPS, op0=ALU.mult, op1=ALU.add), Rb, Wb)
        P.op("act", lambda e: e.activation(out=out_ap, in_=out_ap, func=AF.Sqrt), Wb, Wb)
        P.op("dve", lambda e: e.reciprocal(out=out_ap, in_=out_ap), Wb, Wb)

    hT_cache = {}

    def build_hT(xsrc):
        if xsrc.name in hT_cache:
            scr = hT_cache[xsrc.name]
            P.dma("sp", lambda e: e.dma_start(out=hT.t[:, :, :], in_=scr.t[:, :, :]), [scr], [hT], hT)
            return
        build_hT_full(xsrc)
        scr = P.dram("hTscr_" + xsrc.name, [128, KC, NOWN], BF16)
        hT_cache[xsrc.name] = scr
        P.dma("sp", lambda e: e.dma_start(out=scr.t[:, :, :], in_=hT.t[:, :, :]), [hT], [scr], hT)

    def build_hT_full(xsrc):
        for t in range(9):
            who = 1 if t == 0 else 0
            i = 0
            state["xi"] += 1
            xb, xsb, stb = xt[i], xs[i], st[i]
            P.dma("sp", lambda e: e.dma_start(out=xb.t[:], in_=xsrc.t[t * 128:(t + 1) * 128, :]), [xsrc], [xb], xb)
            P.op("act", lambda e: e.activation(out=xsb.t[:], in_=xb.t[:], func=AF.Square, accum_out=stb.t[:, 0:1]), [xb], [xsb, stb])
            rstd_from_ss(stb.t[:, 0:1], 1, 1.0 / D, [stb], stb.t[:, 1:2], [stb])
            P.op("dve", lambda e: e.tensor_scalar(out=xsb.t[:], in0=xb.t[:], scalar1=stb.t[:, 1:2], scalar2=None, op0=ALU.mult), [xb, stb], [xsb])
            for half in range(2):
                for j in range(8):
                    kc = half * 8 + j
                    P.op("pe", lambda e, kc=kc, j=j: e.transpose(psT.t[:, j * 128:(j + 1) * 128], xsb.t[:, kc * 128:(kc + 1) * 128], ident),
                         [xsb, consts], [psT], inc=(j == 7))
                for j in range(8):
                    kc = half * 8 + j
                    ek = "dve" if j % 2 == 0 else "act"
                    if ek == "dve":
                        P.op("dve", lambda e, kc=kc, j=j: e.tensor_scalar(
                            out=hT.t[:, kc, t * 128:(t + 1) * 128], in0=psT.t[:, j * 128:(j + 1) * 128],
                            scalar1=a1T.t[:, who * 16 + kc:who * 16 + kc + 1], scalar2=fm.t[:, FM_SH1 + who * 16 + kc:FM_SH1 + who * 16 + kc + 1],
                            op0=ALU.mult, op1=ALU.add), [psT, a1T, fm], [hT])
                    else:
                        P.op("act", lambda e, kc=kc, j=j: e.activation(
                            out=hT.t[:, kc, t * 128:(t + 1) * 128], in_=psT.t[:, j * 128:(j + 1) * 128], func=AF.Identity,
                            scale=a1T.t[:, who * 16 + kc:who * 16 + kc + 1], bias=fm.t[:, FM_SH1 + who * 16 + kc:FM_SH1 + who * 16 + kc + 1]),
                            [psT, a1T, fm], [hT])

    def load_w(src, r_ap, ncols):
        w = wb[state["wi"] % 2]
        state["wi"] += 1
        P.dma("pool", lambda e: e.dma_start(out=w.t[:, :, 0:ncols], in_=r_ap), [src], [w], w)
        return w

    def next_psM():
        p = psM[state["pm"] % 2]
        state["pm"] += 1
        return p

    def proj_F(col0, ncols, chunks, cb, mwidth=128):
        for g0 in range(0, ncols, 256):
            gn = min(256, ncols - g0)
            w = load_w(w_in, w_in.t[:, col0 + g0:col0 + g0 + gn].rearrange("(kc p) n -> p kc n", p=128), gn)
            for m0 in range(0, gn, mwidth):
                m = min(mwidth, gn - m0)
                for (t0, n) in chunks:
                    ps = next_psM()
                    for kc in range(KC):
                        P.op("pe", lambda e, kc=kc: e.matmul(ps.t[0:m, 0:n], lhsT=w.t[:, kc, m0:m0 + m], rhs=hT.t[:, kc, t0:t0 + n],
                                                             start=(kc == 0), stop=(kc == KC - 1)), [w, hT], [ps], inc=(kc == KC - 1))
                    cb(ps, (g0 + m0) // mwidth, m, t0, n)

    def proj_T(col0, ncols, tiles, cb):
        for g0 in range(0, ncols, 256):
            gn = min(256, ncols - g0)
            w = load_w(w_in, w_in.t[:, col0 + g0:col0 + g0 + gn].rearrange("(kc p) n -> p kc n", p=128), gn)
            for t0 in tiles:
                ps = next_psM()
                for kc in range(KC):
                    P.op("pe", lambda e, kc=kc: e.matmul(ps.t[:, 0:gn], lhsT=hT.t[:, kc, t0:t0 + 128], rhs=w.t[:, kc, 0:gn],
                                                         start=(kc == 0), stop=(kc == KC - 1)), [w, hT], [ps], inc=(kc == KC - 1))
                cb(ps, g0, gn, t0)

    def copy_out(ek, dst_ap, src_ap, Rb, Wb):
        if ek == "act":
            P.op("act", lambda e: e.copy(out=dst_ap, in_=src_ap), Rb, Wb)
        else:
            P.op(ek, lambda e: e.tensor_copy(out=dst_ap, in_=src_ap), Rb, Wb)

    def load_rope(tab, npart, which, l0, n):
        c = cs[0]
        P.dma("sp", lambda e: e.dma_start(out=c.t[0:npart, :, 0:n], in_=tab.t[:, :, which, l0:l0 + n]), [tab], [c], c)
        return c

    def rope_apply(x_ap, xbuf, npart, Rt, c, n, dst_ap, dstbuf):
        P.op("pe", lambda e: e.matmul(psX.t[0:npart, 0:n], lhsT=Rt[0:npart, 0:npart], rhs=x_ap, start=True, stop=True), [xbuf, consts], [psX])
        P.op("dve", lambda e: e.tensor_tensor(out=tA.t[0:npart, 0:n], in0=x_ap, in1=c.t[0:npart, 0, 0:n], op=ALU.mult), [xbuf, c], [tA])
        P.op("dve", lambda e: e.tensor_tensor(out=tB.t[0:npart, 0:n], in0=psX.t[0:npart, 0:n], in1=c.t[0:npart, 1, 0:n], op=ALU.mult), [psX, c], [tB])
        P.op("dve", lambda e: e.tensor_tensor(out=dst_ap, in0=tA.t[0:npart, 0:n], in1=tB.t[0:npart, 0:n], op=ALU.add), [tA, tB], [dstbuf])

    def lat_off(t0):
        return t0 - 128

    def attention(QT_list, KT_list, Vbuf, vcol, key_tiles, q0, qn, scale, dst, dstbuf, Rbufs):
        nk = len(key_tiles)

        def emit_S(i):
            ps = psS[(state["ps"] + i) % 2]
            kt = key_tiles[i]
            for j, (qf, kf) in enumerate(zip(QT_list, KT_list)):
                P.op("pe", lambda e, j=j: e.matmul(ps.t[:, 0:qn], lhsT=kf(kt), rhs=qf(q0, qn), start=(j == 0), stop=(j == len(QT_list) - 1)),
                     Rbufs, [ps], inc=(j == len(QT_list) - 1))
            return ps
        pss = {0: emit_S(0)}
        for i in range(nk):
            if i + 1 < nk:
                pss[i + 1] = emit_S(i + 1)
            ps = pss.pop(i)
            pt = Pt[state["pt"] % 2]
            state["pt"] += 1
            P.op("act", lambda e: e.activation(out=pt.t[:, 0:qn], in_=ps.t[:, 0:qn], func=AF.Exp, scale=scale), [ps], [pt])
            kt = key_tiles[i]
            P.op("pe", lambda e: e.matmul(psO.t[:, 0:qn], lhsT=Vbuf.ap[:, kt, vcol:vcol + 128], rhs=pt.t[:, 0:qn], start=(i == 0), stop=(i == nk - 1)),
                 [Vbuf, pt], [psO], inc=False)
            P.op("pe", lambda e: e.matmul(psSum.t[:, 0:qn], lhsT=ones, rhs=pt.t[:, 0:qn], start=(i == 0), stop=(i == nk - 1)),
                 [consts, pt], [psSum], inc=True)
        state["ps"] += nk
        P.op("dve", lambda e: e.reciprocal(out=rr.t[:, 0:qn], in_=psSum.t[:, 0:qn]), [psSum], [rr])
        P.op("dve", lambda e: e.tensor_tensor(out=dst, in0=psO.t[:, 0:qn], in1=rr.t[:, 0:qn], op=ALU.mult), [psO, rr], [dstbuf])

    def barrier():
        evs = [("e_" + k, P.sem[k], P.cnt[k], "bar") for k in P.cnt if P.cnt[k] > 0]
        evs += [(b.semkey, b.sem, b.cnt, "dma") for b in P.dma_bufs]
        for k in P.eng:
            P._wait(k, evs)

    def mla_norm(gcol, n):
        for c in range(4):
            P.op("pe", lambda e, c=c: e.matmul(psX.t[:, 0:n], lhsT=ones, rhs=sq.t[:, c, 0:n], start=(c == 0), stop=(c == 3)), [consts, sq], [psX], inc=(c == 3))
        rstd_from_ss(psX.t[:, 0:n], n, 1.0 / 512, [psX], rr.t[:, 0:n], [rr])
        for c in range(4):
            P.op("dve", lambda e, c=c: e.scalar_tensor_tensor(out=sq.t[:, c, 0:n], in0=ck.t[:, c, 0:n], scalar=fm.t[:, gcol + c:gcol + c + 1],
                                                             in1=rr.t[:, 0:n], op0=ALU.mult, op1=ALU.mult), [ck, fm, rr], [sq])

    def cb_ck(ps, ci, m, t0, n):
        P.op("act", lambda e: e.copy(out=ck.t[:, ci, 0:n], in_=ps.t[:, 0:n]), [ps], [ck])
        P.op("act", lambda e: e.activation(out=sq.t[:, ci, 0:n], in_=ps.t[:, 0:n], func=AF.Square), [ps], [sq])

    off = 0
    KnT, off = arena_buf("KnT", off, [128, 4, 2304])
    kpeT, off = arena_buf("kpeT", off, [128, 2304])
    Vm, off = arena_buf("Vm", off, [128, 18, 512])
    QnT, off = arena_buf("QnT", off, [128, 4, NOWN])
    QpT, off = arena_buf("QpT", off, [128, 4, NOWN])
    wuq, off = arena_buf("wuq", off, [128, 4, 768])
    wukv, off = arena_buf("wukv", off, [128, 4, 1024])
    assert off <= ARENA_N
    wuq_sem = P.sb("wuq_sem", [128, 2], F32)
    P.dma("pool", lambda e: e.dma_start(out=wuq.ap, in_=w_uq.t[:, :].rearrange("(c p) n -> p c n", p=128)), [w_uq], [wuq], wuq_sem)
    P.dma("pool", lambda e: e.dma_start(out=wukv.ap, in_=w_ukv.t[:, :].rearrange("(c p) n -> p c n", p=128)), [w_ukv], [wukv], wuq_sem)

    def mla_tokens(which, kvbase):
        for (t0, n) in OWN_CHUNKS:
            is_lat = t0 >= 128
            if is_lat:
                c = load_rope(ropeA, 64, which, lat_off(t0), n)
            proj_F(CA + 512, 512, [(t0, n)], cb_ck)
            mla_norm(FM_GKVA, n)
            for h in range(4):
                ps = next_psM()
                for cc in range(4):
                    P.op("pe", lambda e, cc=cc: e.matmul(ps.t[:, 0:n], lhsT=wukv.ap[:, cc, h * 256:h * 256 + 128], rhs=sq.t[:, cc, 0:n],
                                                         start=(cc == 0), stop=(cc == 3)), [wukv, sq], [ps], inc=(cc == 3))
                copy_out("act" if h % 2 else "dve", KnT.ap[:, h, kvbase + t0:kvbase + t0 + n], ps.t[:, 0:n], [ps], [KnT])
            for tt in range(n // 128):
                ps = next_psM()
                for cc in range(4):
                    P.op("pe", lambda e, cc=cc: e.matmul(ps.t[:, 0:512].rearrange("p (h x) -> p h x", x=128), lhsT=sq.t[:, cc, tt * 128:(tt + 1) * 128],
                                                         rhs=wukv.ap[:, cc, :].rearrange("p (h x) -> p h x", x=256)[:, :, 128:256],
                                                         start=(cc == 0), stop=(cc == 3)), [wukv, sq], [ps], inc=(cc == 3))
                copy_out("act" if tt % 2 else "dve", Vm.ap[:, (kvbase + t0) // 128 + tt, :], ps.t[:, 0:512], [ps], [Vm])

            def cb_kpe(ps, ci, m, t0_, n_):
                if not is_lat:
                    copy_out("dve", kpeT.ap[0:64, kvbase + t0:kvbase + t0 + n], ps.t[0:64, 0:n], [ps], [kpeT])
                else:
                    copy_out("dve", ck.t[0:64, 0, 0:n], ps.t[0:64, 0:n], [ps], [ck])
                    rope_apply(ck.t[0:64, 0, 0:n], ck, 64, RAt, c, n, kpeT.ap[0:64, kvbase + t0:kvbase + t0 + n], kpeT)
            proj_F(CA + 1024, 64, [(t0, n)], cb_kpe, mwidth=64)
            if which == 1:
                continue
            proj_F(CA, 512, [(t0, n)], cb_ck)
            mla_norm(FM_GQA, n)
            for h in range(4):
                ps = next_psM()
                for cc in range(4):
                    P.op("pe", lambda e, cc=cc: e.matmul(ps.t[:, 0:n], lhsT=wuq.ap[:, cc, h * 192:h * 192 + 128], rhs=sq.t[:, cc, 0:n],
                                                         start=(cc == 0), stop=(cc == 3)), [wuq, sq], [ps], inc=(cc == 3))
                copy_out("act", QnT.ap[:, h, t0:t0 + n], ps.t[:, 0:n], [ps], [QnT])
                ps = next_psM()
                for cc in range(4):
                    P.op("pe", lambda e, cc=cc: e.matmul(ps.t[0:64, 0:n], lhsT=wuq.ap[:, cc, h * 192 + 128:h * 192 + 192], rhs=sq.t[:, cc, 0:n],
                                                         start=(cc == 0), stop=(cc == 3)), [wuq, sq], [ps], inc=(cc == 3))
                if not is_lat:
                    copy_out("dve", QpT.ap[0:64, h, t0:t0 + n], ps.t[0:64, 0:n], [ps], [QpT])
                else:
                    copy_out("dve", ck.t[0:64, 0, 0:n], ps.t[0:64, 0:n], [ps], [ck])
                    rope_apply(ck.t[0:64, 0, 0:n], ck, 64, RAt, c, n, QpT.ap[0:64, h, t0:t0 + n], QpT)

    def run_attn_full(QTs, KTs, Vb, vcolf, scale, ydst):
        for (q0, qn) in OWN_CHUNKS:
            kts = [0, 9] if q0 == 0 else list(range(18))
            for h in range(4):
                pairs = QTs(h)
                attention([p[0] for p in pairs], [p[1] for p in pairs], Vb, vcolf(h), kts, q0, qn, scale,
                          ydst.t[:, h, q0:q0 + qn], ydst, [b for b in KTs])

    build_hT(x_oth)
    mla_tokens(1, NOWN)
    build_hT(x_own)
    mla_tokens(0, 0)
    run_attn_full(lambda h: [(lambda q0, qn: QnT.ap[:, h, q0:q0 + qn], lambda kt: KnT.ap[:, h, kt * 128:(kt + 1) * 128]),
                             (lambda q0, qn: QpT.ap[0:64, h, q0:q0 + qn], lambda kt: kpeT.ap[0:64, kt * 128:(kt + 1) * 128])],
                  [KnT, kpeT, QnT, QpT], Vm, lambda h: h * 128, MLA_SCALE, yT[0])
    barrier()

    off = 0
    KTb, off = arena_buf("KTb", off, [128, 2, 2304])
    Vb, off = arena_buf("Vb", off, [128, 18, 256])
    QTb, off = arena_buf("QTb", off, [128, 4, NOWN])

    def qknorm(ps, n, gcol, c, dst_ap, dstbuf):
        P.op("act", lambda e: e.copy(out=ck.t[:, 0, 0:n], in_=ps.t[:, 0:n]), [ps], [ck])
        P.op("act", lambda e: e.activation(out=sq.t[:, 0, 0:n], in_=ps.t[:, 0:n], func=AF.Square), [ps], [sq])
        P.op("pe", lambda e: e.matmul(psX.t[:, 0:n], lhsT=ones, rhs=sq.t[:, 0, 0:n], start=True, stop=True), [consts, sq], [psX])
        rstd_from_ss(psX.t[:, 0:n], n, 1.0 / 128, [psX], rr.t[:, 0:n], [rr])
        if c is None:
            P.op("dve", lambda e: e.scalar_tensor_tensor(out=dst_ap, in0=ck.t[:, 0, 0:n], scalar=fm.t[:, gcol:gcol + 1], in1=rr.t[:, 0:n],
                                                         op0=ALU.mult, op1=ALU.mult), [ck, fm, rr], [dstbuf])
        else:
            P.op("dve", lambda e: e.scalar_tensor_tensor(out=sq.t[:, 1, 0:n], in0=ck.t[:, 0, 0:n], scalar=fm.t[:, gcol:gcol + 1], in1=rr.t[:, 0:n],
                                                         op0=ALU.mult, op1=ALU.mult), [ck, fm, rr], [sq])
            rope_apply(sq.t[:, 1, 0:n], sq, 128, RBt, c, n, dst_ap, dstbuf)

    def gqa_tokens(which, kvbase):
        for (t0, n) in OWN_CHUNKS:
            c = load_rope(ropeB, 128, which, lat_off(t0), n) if t0 >= 128 else None
            proj_F(CB + 512, 256, [(t0, n)], lambda ps, ci, m, t0_, n_: qknorm(ps, n, FM_GKN, c, KTb.ap[:, ci, kvbase + t0:kvbase + t0 + n], KTb))
            if which == 0:
                proj_F(CB, 512, [(t0, n)], lambda ps, ci, m, t0_, n_: qknorm(ps, n, FM_GQN, c, QTb.ap[:, ci, t0:t0 + n], QTb))
        proj_T(CB + 768, 256, [t * 128 for t in range(9)],
               lambda ps, g0, gn, t0: copy_out("act" if (t0 // 128) % 2 else "dve", Vb.ap[:, (kvbase + t0) // 128, :], ps.t[:, 0:256], [ps], [Vb]))

    build_hT(x_oth)
    gqa_tokens(1, NOWN)
    build_hT(x_own)
    gqa_tokens(0, 0)
    run_attn_full(lambda h: [(lambda q0, qn: QTb.ap[:, h, q0:q0 + qn], lambda kt: KTb.ap[:, h // 2, kt * 128:(kt + 1) * 128])],
                  [KTb, QTb], Vb, lambda h: (h // 2) * 128, HD_SCALE, yT[1])
    barrier()

    off = 0
    KTc, off = arena_buf("KTc", off, [128, 4, 1792])
    Vctx, off = arena_buf("Vctx", off, [128, 2, 512])
    Vev, off = arena_buf("Vev", off, [128, 12, 512])
    QTc, off = arena_buf("QTc", off, [128, 4, NOWN])
    E2, off = arena_buf("E2", off, [128, 60, 64])
    for h in range(4):
        P.dma("sp", lambda e: e.dma_start(out=xt[0].t[:, 0:960], in_=nab_d.t[:, h, :, :].rearrange("p a b -> p (a b)")),
              [nab_d], [xt[0]], xt[0])
        P.op("act", lambda e: e.activation(out=E2.ap[:, h * 15:(h + 1) * 15, :].rearrange("p a b -> p (a b)"), in_=xt[0].t[:, 0:960], func=AF.Exp),
             [xt[0]], [E2])

    def kc_dst(which, t0):
        if which == 0:
            return 0 if t0 == 0 else 512 + (t0 - 128)
        if t0 == 0:
            return 128
        return 256 if t0 == 896 else 1536

    build_hT(x_oth)
    oth_chunks = [(0, 128), (896, 256), (128, 256)]
    proj_F(CC + 512, 512, oth_chunks, lambda ps, ci, m, t0, n: copy_out("act" if ci % 2 else "dve", KTc.ap[:, ci, kc_dst(1, t0):kc_dst(1, t0) + n], ps.t[:, 0:n], [ps], [KTc]))

    def cb_v_oth(ps, g0, gn, t0):
        if t0 == 0:
            dst = Vctx.ap[:, 1, g0:g0 + gn]
            db = Vctx
        else:
            ti = {896: 0, 1024: 1, 128: 10, 256: 11}[t0]
            dst = Vev.ap[:, ti, g0:g0 + gn]
            db = Vev
        copy_out("dve", dst, ps.t[:, 0:gn], [ps], [db])
    proj_T(CC + 1024, 512, [0, 896, 1024, 128, 256], cb_v_oth)
    build_hT(x_own)
    proj_F(CC, 512, OWN_CHUNKS, lambda ps, ci, m, t0, n: copy_out("act" if ci % 2 else "dve", QTc.ap[:, ci, t0:t0 + n], ps.t[:, 0:n], [ps], [QTc]))
    proj_F(CC + 512, 512, OWN_CHUNKS, lambda ps, ci, m, t0, n: copy_out("act" if ci % 2 else "dve", KTc.ap[:, ci, kc_dst(0, t0):kc_dst(0, t0) + n], ps.t[:, 0:n], [ps], [KTc]))

    def cb_v_own(ps, g0, gn, t0):
        if t0 == 0:
            copy_out("dve", Vctx.ap[:, 0, g0:g0 + gn], ps.t[:, 0:gn], [ps], [Vctx])
        else:
            copy_out("dve", Vev.ap[:, 2 + (t0 - 128) // 128, g0:g0 + gn], ps.t[:, 0:gn], [ps], [Vev])
    proj_T(CC + 1024, 512, [t * 128 for t in range(9)], cb_v_own)
    for h in range(4):
        attention([lambda q0, qn: QTc.ap[:, h, q0:q0 + qn]], [lambda kt: KTc.ap[:, h, kt * 128:(kt + 1) * 128]], Vctx, h * 128, [0, 1], 0, 128,
                  HD_SCALE, yT[2].t[:, h, 0:128], yT[2], [KTc, QTc])
    for h in range(4):
        for lr in range(16):
            Rs = lr if lr <= 12 else 12
            Re = 11 if lr < 4 else lr + 7
            R0 = Rs - Rs % 2
            R1 = Re | 1
            npair = (R1 - R0 + 1) // 2
            nsl = npair + 2
            ps = psS[state["ps"] % 2]
            state["ps"] += 1
            qap = QTc.ap[:, h, 128 + lr * 64:128 + (lr + 1) * 64]
            for s_ in range(nsl):
                if s_ < npair:
                    R = R0 + 2 * s_
                    kap = KTc.ap[:, h, 256 + 64 * R:256 + 64 * R + 128]
                else:
                    kap = KTc.ap[:, h, (s_ - npair) * 128:(s_ - npair + 1) * 128]
                P.op("pe", lambda e: e.matmul(ps.t[:, s_ * 64:(s_ + 1) * 64], lhsT=kap, rhs=qap, start=True, stop=True), [KTc, QTc], [ps], inc=(s_ == nsl - 1))
            pt = Pt[state["pt"] % 2]
            state["pt"] += 1
            P.op("act", lambda e: e.activation(out=pt.t[:, 0:nsl * 64], in_=ps.t[:, 0:nsl * 64], func=AF.Exp, scale=HD_SCALE), [ps], [pt])
            for s_ in range(npair):
                R = R0 + 2 * s_
                m = R + 3 - lr
                P.op("dve", lambda e: e.scalar_tensor_tensor(out=pt.t[:, s_ * 64:(s_ + 1) * 64], in0=pt.t[:, s_ * 64:(s_ + 1) * 64], scalar=ind.t[:, lr, s_:s_ + 1],
                                                             in1=E2.ap[:, h * 15 + m, :], op0=ALU.mult, op1=ALU.mult), [pt, ind, E2], [pt])
            for s_ in range(nsl):
                if s_ < npair:
                    vap = Vev.ap[:, (R0 + 2 * s_) // 2, h * 128:(h + 1) * 128]
                else:
                    vap = Vctx.ap[:, s_ - npair, h * 128:(h + 1) * 128]
                P.op("pe", lambda e: e.matmul(psO.t[:, 0:64], lhsT=vap, rhs=pt.t[:, s_ * 64:(s_ + 1) * 64], start=(s_ == 0), stop=(s_ == nsl - 1)),
                     [Vev, Vctx, pt], [psO], inc=False)
                P.op("pe", lambda e: e.matmul(psSum.t[:, 0:64], lhsT=ones, rhs=pt.t[:, s_ * 64:(s_ + 1) * 64], start=(s_ == 0), stop=(s_ == nsl - 1)),
                     [consts, pt], [psSum], inc=True)
            P.op("dve", lambda e: e.reciprocal(out=rr.t[:, 0:64], in_=psSum.t[:, 0:64]), [psSum], [rr])
            P.op("dve", lambda e: e.tensor_tensor(out=yT[2].t[:, h, 128 + lr * 64:128 + (lr + 1) * 64], in0=psO.t[:, 0:64], in1=rr.t[:, 0:64], op=ALU.mult),
                 [psO, rr], [yT[2]])
    barrier()

    off = 0
    bg, off = arena_buf("bg", off, [128, 4, NOWN])
    cg, off = arena_buf("cg", off, [128, 4, NOWN])
    zlat, off = arena_buf("zlat", off, [128, 4, 1026])
    zctx, off = arena_buf("zctx", off, [128, 4, 130])
    cgo, off = arena_buf("cgo", off, [128, 4, 384])
    zo, off = arena_buf("zo", off, [128, 4, 384])
    och = [(0, 128), (128, 128), (1024, 128)]
    oslot = {0: 0, 128: 1, 1024: 2}
    build_hT(x_oth)
    proj_F(CD + 512, 512, och, lambda ps, ci, m, t0, n: copy_out("act", cgo.ap[:, ci, oslot[t0] * 128:(oslot[t0] + 1) * 128], ps.t[:, 0:n], [ps], [cgo]))
    proj_F(CD + 1024, 512, och, lambda ps, ci, m, t0, n: P.op("dve", lambda e: e.tensor_tensor(
        out=zo.ap[:, ci, oslot[t0] * 128:(oslot[t0] + 1) * 128], in0=ps.t[:, 0:n], in1=cgo.ap[:, ci, oslot[t0] * 128:(oslot[t0] + 1) * 128], op=ALU.mult), [ps, cgo], [zo]))
    for (dst, dcol, scol, fl) in [(zlat, 0, 383, FM_FLAGB), (zlat, 1025, 128, FM_FLAGA), (zctx, 0, 127, FM_FLAGB), (zctx, 129, 0, FM_FLAGA)]:
        P.op("dve", lambda e: e.tensor_scalar(out=dst.ap[:, :, dcol:dcol + 1], in0=zo.ap[:, :, scol:scol + 1], scalar1=fm.t[:, fl:fl + 1], scalar2=None, op0=ALU.mult),
             [zo, fm], [dst])
    build_hT(x_own)
    proj_F(CD, 512, OWN_CHUNKS, lambda ps, ci, m, t0, n: copy_out("act", bg.ap[:, ci, t0:t0 + n], ps.t[:, 0:n], [ps], [bg]))
    proj_F(CD + 512, 512, OWN_CHUNKS, lambda ps, ci, m, t0, n: copy_out("act", cg.ap[:, ci, t0:t0 + n], ps.t[:, 0:n], [ps], [cg]))

    def zdst(ci, t0, n):
        return (zctx, zctx.ap[:, ci, 1:1 + n]) if t0 == 0 else (zlat, zlat.ap[:, ci, 1 + t0 - 128:1 + t0 - 128 + n])
    proj_F(CD + 1024, 512, OWN_CHUNKS, lambda ps, ci, m, t0, n: P.op("dve", lambda e: e.tensor_tensor(
        out=zdst(ci, t0, n)[1], in0=ps.t[:, 0:n], in1=cg.ap[:, ci, t0:t0 + n], op=ALU.mult), [ps, cg], [zdst(ci, t0, n)[0]]))
    for ci in range(4):
        for (t0, n) in OWN_CHUNKS:
            zb, zoff = (zctx, 0) if t0 == 0 else (zlat, t0 - 128)
            wc = FM_WCONV + ci * 3
            P.op("dve", lambda e: e.tensor_scalar(out=tA.t[:, 0:n], in0=zb.ap[:, ci, zoff:zoff + n], scalar1=fm.t[:, wc:wc + 1], scalar2=None, op0=ALU.mult), [zb, fm], [tA])
            for tap in (1, 2):
                P.op("dve", lambda e: e.scalar_tensor_tensor(out=tA.t[:, 0:n], in0=zb.ap[:, ci, zoff + tap:zoff + tap + n], scalar=fm.t[:, wc + tap:wc + tap + 1],
                                                             in1=tA.t[:, 0:n], op0=ALU.mult, op1=ALU.add), [zb, fm, tA], [tA])
            P.op("dve", lambda e: e.tensor_tensor(out=yT[3].t[:, ci, t0:t0 + n], in0=tA.t[:, 0:n], in1=bg.ap[:, ci, t0:t0 + n], op=ALU.mult), [tA, bg], [yT[3]])
    barrier()
    if DEBUG_Y:
        ydbg = P.dram("ydbg", [4, 128, 4, NOWN], BF16, "ExternalOutput")
        for k in range(4):
            P.dma("sp", lambda e: e.dma_start(out=ydbg.t[k, :, :, :], in_=yT[k].t[:]), [yT[k]], [ydbg], yT[k])

    off = 0
    mT, off = arena_buf("mT", off, [128, KC, NOWN])
    macc = Buf("macc", None)
    macc.ap = arena[:, off:off + 2 * 2 * NOWN].bitcast(F32).rearrange("p (a b) -> p a b", b=NOWN)
    off += 4 * NOWN
    wbr = []
    for i in range(2):
        b_, off = arena_buf("wbr%d" % i, off, [128, 4, 256])
        b_.sembuf = P.sb("wbrsem%d" % i, [128, 2], F32)
        wbr.append(b_)
    assert off <= ARENA_N
    wi2 = 0
    for dg in range(8):
        for k in range(4):
            w = load_w(w_in, w_in.t[:, CG + k * D + dg * 256:CG + k * D + (dg + 1) * 256].rearrange("(kc p) n -> p kc n", p=128), 256)
            wbk = wbr[wi2 % 2]
            wi2 += 1
            P.dma("pool", lambda e: e.dma_start(out=wbk.ap, in_=w_br.t[k, :, dg * 256:(dg + 1) * 256].rearrange("(c p) n -> p c n", p=128)), [w_br], [wbk], wbk.sembuf)
            for dc in range(2):
                for (t0, n) in OWN_CHUNKS:
                    ps = next_psM()
                    for kc in range(KC):
                        P.op("pe", lambda e, kc=kc: e.matmul(ps.t[:, 0:n], lhsT=w.t[:, kc, dc * 128:(dc + 1) * 128], rhs=hT.t[:, kc, t0:t0 + n],
                                                             start=(kc == 0), stop=(kc == KC - 1)), [w, hT], [ps], inc=(kc == KC - 1))
                    ps2 = next_psM()
                    for cc in range(4):
                        P.op("pe", lambda e, cc=cc: e.matmul(ps2.t[:, 0:n], lhsT=wbk.ap[:, cc, dc * 128:(dc + 1) * 128], rhs=yT[k].t[:, cc, t0:t0 + n],
                                                             start=(cc == 0), stop=(cc == 3)), [wbk, yT[k]], [ps2], inc=(cc == 3))
                    P.op("act", lambda e: e.activation(out=tA.t[:, 0:n], in_=ps.t[:, 0:n], func=AF.Sigmoid), [ps], [tA])
                    if k == 0:
                        P.op("dve", lambda e: e.tensor_tensor(out=macc.ap[:, dc, t0:t0 + n], in0=ps2.t[:, 0:n], in1=tA.t[:, 0:n], op=ALU.mult), [ps2, tA], [macc])
                    else:
                        P.op("dve", lambda e: e.tensor_tensor(out=tB.t[:, 0:n], in0=ps2.t[:, 0:n], in1=tA.t[:, 0:n], op=ALU.mult), [ps2, tA], [tB])
                        if k < 3:
                            P.op("dve", lambda e: e.tensor_tensor(out=macc.ap[:, dc, t0:t0 + n], in0=macc.ap[:, dc, t0:t0 + n], in1=tB.t[:, 0:n], op=ALU.add), [macc, tB], [macc])
                        else:
                            P.op("dve", lambda e: e.tensor_tensor(out=mT.ap[:, dg * 2 + dc, t0:t0 + n], in0=macc.ap[:, dc, t0:t0 + n], in1=tB.t[:, 0:n], op=ALU.add), [macc, tB], [mT])
    barrier()

    wo = Buf("wo", None)
    wo.ap = arena2[:, 0:KC * D].rearrange("p (a b) -> p a b", b=D)
    wo.sembuf = P.sb("wosem", [128, 2], F32)
    for og in range(4):
        P.dma("pool", lambda e: e.dma_start(out=wo.ap[:, :, og * 512:(og + 1) * 512], in_=w_o.t[:, og * 512:(og + 1) * 512].rearrange("(kc p) n -> p kc n", p=128)),
              [w_o], [wo], wo.sembuf)
    reps = []
    for i in range(3):
        b_ = Buf("rep%d" % i, None)
        b_.ap = arena[:, off + i * 2 * D:off + (i + 1) * 2 * D].bitcast(F32)
        b_.sembuf = P.sb("repsem%d" % i, [128, 2], F32)
        reps.append(b_)
    off += 6 * D
    assert off <= ARENA_N
    repA, repB, repC = reps
    wrs = P.sb("wrs", [128, KC, 72], BF16)
    P.dma("pool", lambda e: e.dma_start(out=wrs.t[:], in_=wr_d.t[:, :].rearrange("(kc p) n -> p kc n", p=128)), [wr_d], [wrs], wrs)
    brep = P.sb("brep", [128, 72], F32)
    P.dma("sp", lambda e: e.dma_start(out=brep.t[:], in_=brow_d.t[0:1, :].partition_broadcast(128)), [brow_d], [brep], brep)
    rt = P.sb("rt", [128, 256], F32)
    wf = P.sb("wf", [128, 64], F32)

    def load_reps(who):
        P.dma("sp", lambda e: e.dma_start(out=repA.ap, in_=rows_d.t[ROW_SC2 + who:ROW_SC2 + who + 1, :].partition_broadcast(128)), [rows_d], [repA], repA.sembuf)
        P.dma("sp", lambda e: e.dma_start(out=repC.ap, in_=rows_d.t[ROW_GFFN:ROW_GFFN + 1, :].partition_broadcast(128)), [rows_d], [repC], repC.sembuf)
        P.op("dve", lambda e: e.scalar_tensor_tensor(out=repA.ap, in0=repA.ap, scalar=1.0, in1=repC.ap, op0=ALU.add, op1=ALU.mult), [repA, repC], [repA])
        P.dma("sp", lambda e: e.dma_start(out=repC.ap, in_=rows_d.t[ROW_SH2 + who:ROW_SH2 + who + 1, :].partition_broadcast(128)), [rows_d], [repC], repC.sembuf)
        P.dma("sp", lambda e: e.dma_start(out=repB.ap, in_=rows_d.t[ROW_GA1 + who:ROW_GA1 + who + 1, :].partition_broadcast(128)), [rows_d], [repB], repB.sembuf)

    xb, xsb, stb = xt[0], xs[0], st[0]
    for t in range(9):
        if t < 2:
            load_reps(1 if t == 0 else 0)
        P.dma("sp", lambda e: e.dma_start(out=xb.t[:], in_=x_own.t[t * 128:(t + 1) * 128, :]), [x_own], [xb], xb)
        for og in range(4):
            ps = next_psM()
            for kc in range(KC):
                P.op("pe", lambda e, kc=kc: e.matmul(ps.t[:, 0:512], lhsT=mT.ap[:, kc, t * 128:(t + 1) * 128], rhs=wo.ap[:, kc, og * 512:(og + 1) * 512],
                                                     start=(kc == 0), stop=(kc == KC - 1)), [mT, wo], [ps], inc=(kc == KC - 1))
            P.op("dve", lambda e: e.tensor_tensor(out=tA.t[:, 0:512], in0=ps.t[:, 0:512], in1=repB.ap[:, og * 512:(og + 1) * 512], op=ALU.mult), [ps, repB], [tA])
            P.op("dve", lambda e: e.tensor_tensor(out=xb.t[:, og * 512:(og + 1) * 512], in0=xb.t[:, og * 512:(og + 1) * 512], in1=tA.t[:, 0:512], op=ALU.add), [xb, tA], [xb])
        P.dma("sp", lambda e: e.dma_start(out=x1_o.t[t * 128:(t + 1) * 128, :], in_=xb.t[:]), [xb], [x1_o], xb)
        P.op("act", lambda e: e.activation(out=xsb.t[:], in_=xb.t[:], func=AF.Square, accum_out=stb.t[:, 0:1]), [xb], [xsb, stb])
        rstd_from_ss(stb.t[:, 0:1], 1, 1.0 / D, [stb], stb.t[:, 1:2], [stb])
        for og in range(4):
            sl = slice(og * 512, (og + 1) * 512)
            P.op("dve", lambda e: e.scalar_tensor_tensor(out=tA.t[:, 0:512], in0=xb.t[:, sl], scalar=stb.t[:, 1:2], in1=repA.ap[:, sl], op0=ALU.mult, op1=ALU.mult),
                 [xb, stb, repA], [tA])
            P.op("dve", lambda e: e.tensor_tensor(out=xsb.t[:, sl], in0=tA.t[:, 0:512], in1=repC.ap[:, sl], op=ALU.add), [tA, repC], [xsb])
        P.dma("sp", lambda e: e.dma_start(out=h2_o.t[t * 128:(t + 1) * 128, :], in_=xsb.t[:]), [xsb], [h2_o], xsb)
        h2T = ck.t[:].rearrange("p a b -> p (a b)").rearrange("p (k n) -> p k n", n=128)
        for half in range(2):
            for j in range(8):
                kc = half * 8 + j
                P.op("pe", lambda e: e.transpose(psT.t[:, j * 128:(j + 1) * 128], xsb.t[:, kc * 128:(kc + 1) * 128], ident), [xsb, consts], [psT], inc=(j == 7))
            copy_out("act", h2T[:, half * 8:(half + 1) * 8, :], psT.t[:, :].rearrange("p (k n) -> p k n", n=128), [psT], [ck])
        ps = next_psM()
        for kc in range(KC):
            P.op("pe", lambda e, kc=kc: e.matmul(ps.t[:, 0:72], lhsT=h2T[:, kc, :], rhs=wrs.t[:, kc, :], start=(kc == 0), stop=(kc == KC - 1)), [ck, wrs], [ps], inc=(kc == KC - 1))
        lg = rt.t[:, 0:72]
        R_ = [rt]
        P.op("dve", lambda e: e.tensor_copy(out=lg, in_=ps.t[:, 0:72]), [ps], R_)
        sm = rt.t[:, 200:216]

        def softmax8(src, dst, mcol):
            P.op("dve", lambda e: e.tensor_reduce(out=sm[:, mcol:mcol + 1], in_=src, axis=AX.X, op=ALU.max, negate=True), R_, R_)
            P.op("act", lambda e: e.activation(out=dst, in_=src, func=AF.Exp, bias=sm[:, mcol:mcol + 1], scale=1.0, accum_out=sm[:, mcol + 1:mcol + 2]), R_, R_)
            P.op("dve", lambda e: e.reciprocal(out=sm[:, mcol + 1:mcol + 2], in_=sm[:, mcol + 1:mcol + 2]), R_, R_)
            P.op("dve", lambda e: e.tensor_scalar(out=dst, in0=dst, scalar1=sm[:, mcol + 1:mcol + 2], scalar2=None, op0=ALU.mult), R_, R_)

        def onehot_max(src, dst, mcol):
            P.op("dve", lambda e: e.tensor_reduce(out=sm[:, mcol:mcol + 1], in_=src, axis=AX.X, op=ALU.max), R_, R_)
            P.op("dve", lambda e: e.tensor_scalar(out=dst, in0=src, scalar1=sm[:, mcol:mcol + 1], scalar2=None, op0=ALU.is_equal), R_, R_)
        gp, gs, og_, es, eb, ep, sel, o1, o2, tmp = (rt.t[:, 72 + 8 * i:80 + 8 * i] for i in range(10))
        softmax8(rt.t[:, 0:8], gp, 0)
        P.op("dve", lambda e: e.tensor_tensor(out=gs, in0=gp, in1=brep.t[:, 0:8], op=ALU.add), R_ + [brep], R_)
        onehot_max(gs, og_, 2)
        P.op("dve", lambda e: e.tensor_tensor(out=tmp, in0=gp, in1=og_, op=ALU.mult), R_, R_)
        P.op("dve", lambda e: e.tensor_reduce(out=sm[:, 3:4], in_=tmp, axis=AX.X, op=ALU.add), R_, R_)
        for g in range(8):
            if g == 0:
                P.op("dve", lambda e: e.tensor_scalar(out=es, in0=rt.t[:, 8:16], scalar1=og_[:, 0:1], scalar2=None, op0=ALU.mult), R_, R_)
                P.op("dve", lambda e: e.tensor_scalar(out=eb, in0=brep.t[:, 8:16], scalar1=og_[:, 0:1], scalar2=None, op0=ALU.mult), R_ + [brep], R_)
            else:
                P.op("dve", lambda e: e.scalar_tensor_tensor(out=es, in0=rt.t[:, 8 + 8 * g:16 + 8 * g], scalar=og_[:, g:g + 1], in1=es, op0=ALU.mult, op1=ALU.add), R_, R_)
                P.op("dve", lambda e: e.scalar_tensor_tensor(out=eb, in0=brep.t[:, 8 + 8 * g:16 + 8 * g], scalar=og_[:, g:g + 1], in1=eb, op0=ALU.mult, op1=ALU.add), R_ + [brep], R_)
        softmax8(es, ep, 4)
        P.op("dve", lambda e: e.tensor_tensor(out=sel, in0=ep, in1=eb, op=ALU.add), R_, R_)
        onehot_max(sel, o1, 6)
        P.op("dve", lambda e: e.scalar_tensor_tensor(out=sel, in0=o1, scalar=-1e9, in1=sel, op0=ALU.mult, op1=ALU.add), R_, R_)
        onehot_max(sel, o2, 7)
        P.op("dve", lambda e: e.tensor_tensor(out=o1, in0=o1, in1=o2, op=ALU.add), R_, R_)
        P.op("dve", lambda e: e.tensor_tensor(out=tmp, in0=ep, in1=o1, op=ALU.mult), R_, R_)
        P.op("dve", lambda e: e.tensor_reduce(out=sm[:, 8:9], in_=tmp, axis=AX.X, op=ALU.add), R_, R_)
        P.op("dve", lambda e: e.reciprocal(out=sm[:, 8:9], in_=sm[:, 8:9]), R_, R_)
        P.op("dve", lambda e: e.tensor_scalar(out=tmp, in0=tmp, scalar1=sm[:, 8:9], scalar2=sm[:, 3:4], op0=ALU.mult, op1=ALU.mult), R_, R_)
        for g in range(8):
            P.op("dve", lambda e: e.tensor_scalar(out=wf.t[:, 8 * g:8 * g + 8], in0=tmp, scalar1=og_[:, g:g + 1], scalar2=None, op0=ALU.mult), R_, [wf])
        P.dma("sp", lambda e: e.dma_start(out=wr_o.t[t * 128:(t + 1) * 128, :], in_=wf.t[:]), [wf], [wr_o], wf)
    P.finish()
    return nc


NTOK = NCORES * NOWN
BCH = 6


def build_B():
    nc = bass.Bass("TRN2", target_bir_lowering=False)
    P = Prog(nc)
    h2 = P.dram("h2", [NTOK, D], BF16, "ExternalInput")
    wsel_d = P.dram("wsel", [NTOK, 8], F32, "ExternalInput")
    wg_d = P.dram("wg", [8, D, 512], F32, "ExternalInput")
    wu_d = P.dram("wu", [8, D, 512], F32, "ExternalInput")
    wd_d = P.dram("wd", [8, 512, D], F32, "ExternalInput")
    ident_d = P.dram("ident", [128, 128], BF16, "ExternalInput")
    out = P.dram("contrib", [NTOK, D], BF16, "ExternalOutput")
    ntile = NTOK // 128
    ident = P.sb("ident_sb", [128, 128], BF16)
    wsel = P.sb("wsel_sb", [128, ntile, 8], F32)
    h2T = P.sb("h2T", [128, KC, BCH * 128], BF16)
    yacc = P.sb("yacc", [128, BCH, D], F32)
    wg = [P.sb("wg%d" % i, [128, KC, 512], BF16) for i in range(2)]
    wu = [P.sb("wu%d" % i, [128, KC, 512], BF16) for i in range(2)]
    wd = [P.sb("wd%d" % i, [128, 4, D], BF16) for i in range(2)]
    ht = P.sb("ht", [128, D], BF16)
    yb = P.sb("yb", [128, D], BF16)
    tA = P.sb("tA", [128, 512], F32)
    tB = P.sb("tB", [128, 512], F32)
    hb = P.sb("hb", [128, 512], BF16)
    hT4 = P.sb("hT4", [128, 4, 128], BF16)
    ps1 = [P.ps("ps1_%d" % i, [128, 512], F32) for i in range(2)]
    ps2 = [P.ps("ps2_%d" % i, [128, 512], F32) for i in range(2)]
    ps3 = [P.ps("ps3_%d" % i, [128, 512], F32) for i in range(2)]
    psT = P.ps("psT", [128, 1024], BF16)
    P.dma("sp", lambda e: e.dma_start(out=ident.t[:], in_=ident_d.t[:, :]), [ident_d], [ident], ident)
    P.dma("sp", lambda e: e.dma_start(out=wsel.t[:], in_=wsel_d.t[:, :].rearrange("(t p) e -> p t e", p=128)), [wsel_d], [wsel], wsel)
    wi = 0
    k3 = 0
    for ch in range(ntile // BCH):
        for tt in range(BCH):
            t = ch * BCH + tt
            P.dma("sp", lambda e: e.dma_start(out=ht.t[:], in_=h2.t[t * 128:(t + 1) * 128, :]), [h2], [ht], ht)
            for half in range(2):
                for j in range(8):
                    kc = half * 8 + j
                    P.op("pe", lambda e: e.transpose(psT.t[:, j * 128:(j + 1) * 128], ht.t[:, kc * 128:(kc + 1) * 128], ident.t[:]), [ht, ident], [psT], inc=(j == 7))
                P.op("act", lambda e: e.copy(out=h2T.t[:, half * 8:(half + 1) * 8, tt * 128:(tt + 1) * 128], in_=psT.t[:, :].rearrange("p (k n) -> p k n", n=128)), [psT], [h2T])
        for ex in range(8):
            g_, u_, d_ = wg[wi % 2], wu[wi % 2], wd[wi % 2]
            wi += 1
            P.dma("pool", lambda e: e.dma_start(out=g_.t[:], in_=wg_d.t[ex, :, :].rearrange("(kc p) n -> p kc n", p=128)), [wg_d], [g_], g_)
            P.dma("pool", lambda e: e.dma_start(out=u_.t[:], in_=wu_d.t[ex, :, :].rearrange("(kc p) n -> p kc n", p=128)), [wu_d], [u_], u_)
            P.dma("pool", lambda e: e.dma_start(out=d_.t[:], in_=wd_d.t[ex, :, :].rearrange("(c p) n -> p c n", p=128)), [wd_d], [d_], d_)
            for tt in range(BCH):
                t = ch * BCH + tt
                p1, p2 = ps1[tt % 2], ps2[tt % 2]
                for kc in range(KC):
                    P.op("pe", lambda e: e.matmul(p1.t[:, :], lhsT=h2T.t[:, kc, tt * 128:(tt + 1) * 128], rhs=g_.t[:, kc, :], start=(kc == 0), stop=(kc == KC - 1)),
                         [h2T, g_], [p1], inc=(kc == KC - 1))
                for kc in range(KC):
                    P.op("pe", lambda e: e.matmul(p2.t[:, :], lhsT=h2T.t[:, kc, tt * 128:(tt + 1) * 128], rhs=u_.t[:, kc, :], start=(kc == 0), stop=(kc == KC - 1)),
                         [h2T, u_], [p2], inc=(kc == KC - 1))
                P.op("act", lambda e: e.activation(out=tA.t[:], in_=p1.t[:, :], func=AF.Sigmoid), [p1], [tA])
                P.op("dve", lambda e: e.tensor_tensor(out=tB.t[:], in0=p1.t[:, :], in1=tA.t[:], op=ALU.mult), [p1, tA], [tB])
                P.op("dve", lambda e: e.scalar_tensor_tensor(out=hb.t[:], in0=tB.t[:], scalar=wsel.t[:, t, ex:ex + 1], in1=p2.t[:, :], op0=ALU.mult, op1=ALU.mult),
                     [tB, wsel, p2], [hb])
                for j in range(4):
                    P.op("pe", lambda e: e.transpose(psT.t[:, j * 128:(j + 1) * 128], hb.t[:, j * 128:(j + 1) * 128], ident.t[:]), [hb, ident], [psT], inc=(j == 3))
                P.op("act", lambda e: e.copy(out=hT4.t[:], in_=psT.t[:, 0:512].rearrange("p (k n) -> p k n", n=128)), [psT], [hT4])
                for og in range(4):
                    p3 = ps3[k3 % 2]
                    k3 += 1
                    for hc in range(4):
                        P.op("pe", lambda e: e.matmul(p3.t[:, :], lhsT=hT4.t[:, hc, :], rhs=d_.t[:, hc, og * 512:(og + 1) * 512], start=(hc == 0), stop=(hc == 3)),
                             [hT4, d_], [p3], inc=(hc == 3))
                    ya = yacc.t[:, tt, og * 512:(og + 1) * 512]
                    if ex == 0:
                        P.op("dve", lambda e: e.tensor_copy(out=ya, in_=p3.t[:, :]), [p3], [yacc])
                    else:
                        P.op("dve", lambda e: e.tensor_tensor(out=ya, in0=ya, in1=p3.t[:, :], op=ALU.add), [p3, yacc], [yacc])
        for tt in range(BCH):
            t = ch * BCH + tt
            P.op("act", lambda e: e.copy(out=yb.t[:], in_=yacc.t[:, tt, :]), [yacc], [yb])
            P.dma("sp", lambda e: e.dma_start(out=out.t[t * 128:(t + 1) * 128, :], in_=yb.t[:]), [yb], [out], yb)
    P.finish()
    return nc


CAPB = 8
NBLK = 8 * CAPB
NSLOT = NBLK * 128
BIGF = 1.0e6


def build_B2():
    nc = bass.Bass("TRN2", target_bir_lowering=False)
    P = Prog(nc)
    ntile = NTOK // 128
    h2 = P.dram("h2", [NTOK, D], BF16, "ExternalInput")
    wsel_d = P.dram("wsel", [NTOK, 8], F32, "ExternalInput")
    wg_d = P.dram("wg", [8, D, 512], F32, "ExternalInput")
    wu_d = P.dram("wu", [8, D, 512], F32, "ExternalInput")
    wd_d = P.dram("wd", [8, 512, D], F32, "ExternalInput")
    cst_d = P.dram("cst", [128, 3, 128], BF16, "ExternalInput")
    thr_d = P.dram("thr", [128, 8], F32, "ExternalInput")
    out = P.dram("contrib", [NTOK, D], BF16, "ExternalOutput")
    xd = P.dram("xd", [NSLOT, D], BF16)
    yd = P.dram("yd", [NSLOT, D], BF16)

    cst = P.sb("cst_sb", [128, 3, 128], BF16)
    ident, ones, utri = (cst.t[:, i, :] for i in range(3))
    thr = P.sb("thr_sb", [128, 8], F32)
    Wt = P.sb("Wt", [128, ntile, 8], F32)
    Mb = P.sb("Mb", [128, ntile * 8], BF16)
    Mf = P.sb("Mf", [128, ntile, 8], F32)
    S = P.sb("S", [128, ntile, 8], F32)
    tot = P.sb("tot", [128, ntile, 8], F32)
    carry = P.sb("carry", [128, ntile + 1, 8], F32)
    slA = P.sb("slA", [128, ntile], F32)
    slB = P.sb("slB", [128, ntile], F32)
    wA = P.sb("wA", [128, ntile], F32)
    wB = P.sb("wB", [128, ntile], F32)
    slAi = P.sb("slAi", [128, ntile], I32)
    slBi = P.sb("slBi", [128, ntile], I32)
    ht = [P.sb("ht%d" % i, [128, D], BF16) for i in range(2)]
    ya = [P.sb("ya%d" % i, [128, D], BF16) for i in range(2)]
    yb = [P.sb("yb%d" % i, [128, D], BF16) for i in range(2)]
    yo = [P.sb("yo%d" % i, [128, D], BF16) for i in range(2)]
    xTs = [P.sb("xT%d" % i, [128, KC, 128], BF16) for i in range(2)]
    wg = [P.sb("wgS%d" % i, [128, KC, 512], BF16) for i in range(2)]
    wu = [P.sb("wuS%d" % i, [128, KC, 512], BF16) for i in range(2)]
    wd = [P.sb("wdS%d" % i, [128, 4, D], BF16) for i in range(2)]
    tAs = [P.sb("tA%d" % i, [128, 512], F32) for i in range(2)]
    tBs = [P.sb("tB%d" % i, [128, 512], F32) for i in range(2)]
    tC = P.sb("tC", [128, D], F32)
    hbs = [P.sb("hb%d" % i, [128, 512], BF16) for i in range(2)]
    hT4s = [P.sb("hT4%d" % i, [128, 4, 128], BF16) for i in range(2)]
    ps1 = [P.ps("ps1_%d" % i, [128, 512], F32) for i in range(2)]
    ps2 = [P.ps("ps2_%d" % i, [128, 512], F32) for i in range(2)]
    ps3 = [P.ps("ps3_%d" % i, [128, 512], F32) for i in range(2)]
    psTs = [P.ps("psT%d" % i, [128, 1024], BF16) for i in range(2)]
    psX = ps3[0]
    npt = [0]

    def next_psT():
        npt[0] += 1
        return psTs[npt[0] % 2]
    bnd = nc.gpsimd.to_reg(NSLOT - 1)

    P.dma("sp", lambda e: e.dma_start(out=cst.t[:], in_=cst_d.t[:, :, :]), [cst_d], [cst], cst)
    P.dma("sp", lambda e: e.dma_start(out=thr.t[:], in_=thr_d.t[:, :]), [thr_d], [thr], thr)
    P.dma("sp", lambda e: e.dma_start(out=Wt.t[:], in_=wsel_d.t[:, :].rearrange("(t p) e -> p t e", p=128)), [wsel_d], [Wt], Wt)

    def load_expert(ex):
        g_, u_, d_ = wg[ex % 2], wu[ex % 2], wd[ex % 2]
        P.dma("pool", lambda e: e.dma_start(out=g_.t[:], in_=wg_d.t[ex, :, :].rearrange("(kc p) n -> p kc n", p=128)), [wg_d], [g_], g_)
        P.dma("pool", lambda e: e.dma_start(out=u_.t[:], in_=wu_d.t[ex, :, :].rearrange("(kc p) n -> p kc n", p=128)), [wu_d], [u_], u_)
        P.dma("pool", lambda e: e.dma_start(out=d_.t[:], in_=wd_d.t[ex, :, :].rearrange("(c p) n -> p c n", p=128)), [wd_d], [d_], d_)
    load_expert(0)
    load_expert(1)
    Wt2 = Wt.t[:].rearrange("p t e -> p (t e)")
    Mf2 = Mf.t[:].rearrange("p t e -> p (t e)")
    S2 = S.t[:].rearrange("p t e -> p (t e)")
    tot2 = tot.t[:].rearrange("p t e -> p (t e)")
    P.op("dve", lambda e: e.tensor_single_scalar(out=Mf2, in_=Wt2, scalar=0.0, op=ALU.is_gt), [Wt], [Mf])
    P.op("dve", lambda e: e.tensor_copy(out=Mb.t[:], in_=Mf2), [Mf], [Mb])
    for (c0, cn) in [(0, 512), (512, 64)]:
        P.op("pe", lambda e: e.matmul(psX.t[:, 0:cn], lhsT=utri, rhs=Mb.t[:, c0:c0 + cn], start=True, stop=True), [cst, Mb], [psX])
        P.op("dve", lambda e: e.tensor_copy(out=S2[:, c0:c0 + cn], in_=psX.t[:, 0:cn]), [psX], [S])
        P.op("pe", lambda e: e.matmul(psX.t[:, 0:cn], lhsT=ones, rhs=Mb.t[:, c0:c0 + cn], start=True, stop=True), [cst, Mb], [psX])
        P.op("dve", lambda e: e.tensor_copy(out=tot2[:, c0:c0 + cn], in_=psX.t[:, 0:cn]), [psX], [tot])
    P.op("dve", lambda e: e.memset(carry.t[:, 0, :], 0.0), [], [carry])
    for i in range(ntile):
        P.op("dve", lambda e: e.tensor_tensor(out=carry.t[:, i + 1, :], in0=carry.t[:, i, :], in1=tot.t[:, i, :], op=ALU.add), [carry, tot], [carry])
    P.op("dve", lambda e: e.tensor_tensor(out=S.t[:], in0=S.t[:], in1=carry.t[:, 0:ntile, :], op=ALU.add), [S, carry], [S])
    P.op("dve", lambda e: e.tensor_single_scalar(out=tot2, in_=S2, scalar=float(CAPB * 128), op=ALU.is_lt), [S], [tot])
    P.op("dve", lambda e: e.tensor_tensor(out=Mf2, in0=Mf2, in1=tot2, op=ALU.mult), [Mf, tot], [Mf])
    for i in range(ntile):
        P.op("dve", lambda e: e.tensor_tensor(out=S.t[:, i, :], in0=S.t[:, i, :], in1=thr.t[:, :], op=ALU.add), [S, thr], [S])
    P.op("dve", lambda e: e.tensor_scalar(out=tot2, in0=S2, scalar1=-BIGF, scalar2=None, op0=ALU.add), [S], [tot])
    P.op("dve", lambda e: e.tensor_tensor(out=tot2, in0=tot2, in1=Mf2, op=ALU.mult), [tot, Mf], [tot])
    P.op("dve", lambda e: e.tensor_scalar(out=tot2, in0=tot2, scalar1=BIGF, scalar2=None, op0=ALU.add), [tot], [tot])
    P.op("dve", lambda e: e.tensor_reduce(out=slA.t[:], in_=tot.t[:], axis=AX.X, op=ALU.min), [tot], [slA])
    P.op("dve", lambda e: e.tensor_scalar(out=tot2, in0=S2, scalar1=1.0, scalar2=None, op0=ALU.add), [S], [tot])
    P.op("dve", lambda e: e.tensor_tensor(out=tot2, in0=tot2, in1=Mf2, op=ALU.mult), [tot, Mf], [tot])
    P.op("dve", lambda e: e.tensor_scalar(out=tot2, in0=tot2, scalar1=-1.0, scalar2=None, op0=ALU.add), [tot], [tot])
    P.op("dve", lambda e: e.tensor_reduce(out=slB.t[:], in_=tot.t[:], axis=AX.X, op=ALU.max), [tot], [slB])
    for (sl, wv) in ((slA, wA), (slB, wB)):
        for i in range(ntile):
            P.op("dve", lambda e: e.tensor_scalar(out=tot.t[:, i, :], in0=S.t[:, i, :], scalar1=sl.t[:, i:i + 1], scalar2=None, op0=ALU.is_equal), [S, sl], [tot])
        P.op("dve", lambda e: e.tensor_tensor(out=tot2, in0=tot2, in1=Wt2, op=ALU.mult), [tot, Wt], [tot])
        P.op("dve", lambda e: e.tensor_reduce(out=wv.t[:], in_=tot.t[:], axis=AX.X, op=ALU.add), [tot], [wv])
    P.op("dve", lambda e: e.tensor_copy(out=slAi.t[:], in_=slA.t[:]), [slA], [slAi])
    P.op("dve", lambda e: e.tensor_copy(out=slBi.t[:], in_=slB.t[:]), [slB], [slBi])

    for i in range(ntile):
        hb_ = ht[i % 2]
        P.dma("sp", lambda e: e.dma_start(out=hb_.t[:], in_=h2.t[i * 128:(i + 1) * 128, :]), [h2], [hb_], hb_)
        for sli in (slAi, slBi):
            P.dma("pool", lambda e: e.indirect_dma_start(
                out=xd.t[:, :], out_offset=bass.IndirectOffsetOnAxis(ap=sli.t[:, i:i + 1], axis=0),
                in_=hb_.t[:, :], in_offset=None, bounds_check=bnd, oob_is_err=False), [hb_, sli], [xd], hb_)

    k3 = [0]

    def st_load(b):
        xb_ = ht[b % 2]
        P.dma("sp", lambda e: e.dma_start(out=xb_.t[:], in_=xd.t[b * 128:(b + 1) * 128, :]), [xd], [xb_], xb_)

    def st_trx(b):
        xb_, xT = ht[b % 2], xTs[b % 2]
        for half in range(2):
            psT = next_psT()
            for j in range(8):
                kc = half * 8 + j
                P.op("pe", lambda e: e.transpose(psT.t[:, j * 128:(j + 1) * 128], xb_.t[:, kc * 128:(kc + 1) * 128], ident), [xb_, cst], [psT], inc=(j == 7))
            P.op("act", lambda e: e.copy(out=xT.t[:, half * 8:(half + 1) * 8, :], in_=psT.t[:, :].rearrange("p (k n) -> p k n", n=128)), [psT], [xT])

    def st_gu(b):
        ex = b // CAPB
        if b % CAPB == 0 and ex >= 2:
            load_expert(ex)
        g_, u_ = wg[ex % 2], wu[ex % 2]
        xT, tA, tB, hb = xTs[b % 2], tAs[b % 2], tBs[b % 2], hbs[b % 2]
        p1, p2 = ps1[b % 2], ps2[b % 2]
        for kc in range(KC):
            P.op("pe", lambda e: e.matmul(p1.t[:, :], lhsT=xT.t[:, kc, :], rhs=g_.t[:, kc, :], start=(kc == 0), stop=(kc == KC - 1)), [xT, g_], [p1], inc=(kc == KC - 1))
        for kc in range(KC):
            P.op("pe", lambda e: e.matmul(p2.t[:, :], lhsT=xT.t[:, kc, :], rhs=u_.t[:, kc, :], start=(kc == 0), stop=(kc == KC - 1)), [xT, u_], [p2], inc=(kc == KC - 1))
        P.op("act", lambda e: e.activation(out=tA.t[:], in_=p1.t[:, :], func=AF.Sigmoid), [p1], [tA])
        P.op("dve", lambda e: e.tensor_tensor(out=tB.t[:], in0=p1.t[:, :], in1=tA.t[:], op=ALU.mult), [p1, tA], [tB])
        P.op("dve", lambda e: e.tensor_tensor(out=hb.t[:], in0=tB.t[:], in1=p2.t[:, :], op=ALU.mult), [tB, p2], [hb])

    def st_trh(b):
        hb, hT4 = hbs[b % 2], hT4s[b % 2]
        psT = next_psT()
        for j in range(4):
            P.op("pe", lambda e: e.transpose(psT.t[:, j * 128:(j + 1) * 128], hb.t[:, j * 128:(j + 1) * 128], ident), [hb, cst], [psT], inc=(j == 3))
        P.op("act", lambda e: e.copy(out=hT4.t[:], in_=psT.t[:, 0:512].rearrange("p (k n) -> p k n", n=128)), [psT], [hT4])

    def st_down(b):
        ex = b // CAPB
        d_ = wd[ex % 2]
        hT4 = hT4s[b % 2]
        yo_ = yo[b % 2]
        for og in range(4):
            p3 = ps3[k3[0] % 2]
            k3[0] += 1
            for hc in range(4):
                P.op("pe", lambda e: e.matmul(p3.t[:, :], lhsT=hT4.t[:, hc, :], rhs=d_.t[:, hc, og * 512:(og + 1) * 512], start=(hc == 0), stop=(hc == 3)), [hT4, d_], [p3], inc=(hc == 3))
            if og % 2 == 0:
                P.op("act", lambda e: e.copy(out=yo_.t[:, og * 512:(og + 1) * 512], in_=p3.t[:, :]), [p3], [yo_])
            else:
                P.op("dve", lambda e: e.tensor_copy(out=yo_.t[:, og * 512:(og + 1) * 512], in_=p3.t[:, :]), [p3], [yo_])
        P.dma("sp", lambda e: e.dma_start(out=yd.t[b * 128:(b + 1) * 128, :], in_=yo_.t[:]), [yo_], [yd], yo_)

    st_load(0)
    st_load(1)
    st_trx(0)
    for b in range(NBLK):
        st_gu(b)
        if b + 1 < NBLK:
            st_trx(b + 1)
        st_trh(b)
        st_down(b)
        if b + 2 < NBLK:
            st_load(b + 2)

    for i in range(ntile):
        ya_, yb_, yo_ = ya[i % 2], yb[i % 2], yo[i % 2]
        if i < 2:
            P.op("pool", lambda e: e.memset(ya_.t[:], 0.0), [], [ya_])
            P.op("pool", lambda e: e.memset(yb_.t[:], 0.0), [], [yb_])
        P.dma("pool", lambda e: e.indirect_dma_start(out=ya_.t[:, :], out_offset=None, in_=yd.t[:, :],
                                                     in_offset=bass.IndirectOffsetOnAxis(ap=slAi.t[:, i:i + 1], axis=0),
                                                     bounds_check=bnd, oob_is_err=False), [yd, slAi], [ya_], ya_)
        P.dma("pool", lambda e: e.indirect_dma_start(out=yb_.t[:, :], out_offset=None, in_=yd.t[:, :],
                                                     in_offset=bass.IndirectOffsetOnAxis(ap=slBi.t[:, i:i + 1], axis=0),
                                                     bounds_check=bnd, oob_is_err=False), [yd, slBi], [yb_], yb_)
        P.op("dve", lambda e: e.tensor_scalar(out=tC.t[:], in0=ya_.t[:], scalar1=wA.t[:, i:i + 1], scalar2=None, op0=ALU.mult), [ya_, wA], [tC])
        P.op("dve", lambda e: e.scalar_tensor_tensor(out=yo_.t[:], in0=yb_.t[:], scalar=wB.t[:, i:i + 1], in1=tC.t[:], op0=ALU.mult, op1=ALU.add), [yb_, wB, tC], [yo_])
        P.dma("sp", lambda e: e.dma_start(out=out.t[i * 128:(i + 1) * 128, :], in_=yo_.t[:]), [yo_], [out], yo_)
    P.finish()
    return nc


def _b2_consts():
    import ml_dtypes
    c = np.zeros((128, 3, 128), np.float32)
    c[:, 0, :] = np.eye(128)
    c[:, 1, :] = 1.0
    c[:, 2, :] = np.triu(np.ones((128, 128), np.float32), 1)
    thr = np.zeros((128, 8), np.float32)
    thr[:, :] = (np.arange(8) * CAPB * 128)[None, :]
    return c.astype(ml_dtypes.bfloat16), thr


def build_C(final):
    nc = bass.Bass("TRN2", target_bir_lowering=False)
    P = Prog(nc)
    x1 = P.dram("x1", [NOWN, D], F32, "ExternalInput")
    cb = P.dram("cb", [8, NOWN, D], BF16, "ExternalInput")
    rows = P.dram("rows", [3, D], F32, "ExternalInput")
    out = P.dram("x2", [NOWN, D], F32, "ExternalOutput")
    xt = [P.sb("xt%d" % i, [128, D], F32) for i in range(2)]
    cbs = [P.sb("cbs%d" % i, [128, 8, D], BF16) for i in range(2)]
    acc = P.sb("acc", [128, D], F32)
    sq = P.sb("sq", [128, D], BF16)
    st = P.sb("st", [128, 4], F32)
    rep = [P.sb("rep%d" % i, [128, D], F32) for i in range(3)]
    for i in range(3):
        P.dma("sp", lambda e: e.dma_start(out=rep[i].t[:], in_=rows.t[i:i + 1, :].partition_broadcast(128)), [rows], [rep[i]], rep[i])
    for t in range(9):
        xb, cbb = xt[t % 2], cbs[t % 2]
        who = 1 if t == 0 else 0
        P.dma("sp", lambda e: e.dma_start(out=xb.t[:], in_=x1.t[t * 128:(t + 1) * 128, :]), [x1], [xb], xb)
        P.dma("sp", lambda e: e.dma_start(out=cbb.t[:], in_=cb.t[:, t * 128:(t + 1) * 128, :].rearrange("g p d -> p g d")), [cb], [cbb], cbb)
        P.op("dve", lambda e: e.tensor_tensor(out=acc.t[:], in0=cbb.t[:, 0, :], in1=cbb.t[:, 1, :], op=ALU.add), [cbb], [acc])
        for g in range(2, 8):
            P.op("dve", lambda e: e.tensor_tensor(out=acc.t[:], in0=acc.t[:], in1=cbb.t[:, g, :], op=ALU.add), [cbb, acc], [acc])
        P.op("dve", lambda e: e.tensor_tensor(out=acc.t[:], in0=acc.t[:], in1=rep[who].t[:], op=ALU.mult), [acc, rep[who]], [acc])
        P.op("dve", lambda e: e.tensor_tensor(out=xb.t[:], in0=xb.t[:], in1=acc.t[:], op=ALU.add), [xb, acc], [xb])
        if final:
            P.op("act", lambda e: e.activation(out=sq.t[:], in_=xb.t[:], func=AF.Square, accum_out=st.t[:, 0:1]), [xb], [sq, st])
            P.op("dve", lambda e: e.tensor_scalar(out=st.t[:, 1:2], in0=st.t[:, 0:1], scalar1=1.0 / D, scalar2=EPS, op0=ALU.mult, op1=ALU.add), [st], [st])
            P.op("act", lambda e: e.activation(out=st.t[:, 1:2], in_=st.t[:, 1:2], func=AF.Sqrt), [st], [st])
            P.op("dve", lambda e: e.reciprocal(out=st.t[:, 1:2], in_=st.t[:, 1:2]), [st], [st])
            P.op("dve", lambda e: e.scalar_tensor_tensor(out=xb.t[:], in0=xb.t[:], scalar=st.t[:, 1:2], in1=rep[2].t[:], op0=ALU.mult, op1=ALU.mult), [xb, st, rep[2]], [xb])
        P.dma("sp", lambda e: e.dma_start(out=out.t[t * 128:(t + 1) * 128, :], in_=xb.t[:]), [xb], [out], xb)
    P.finish()
    return nc


def _fm(v, n):
    return np.ascontiguousarray(np.asarray(v, np.float32).reshape(n, 128).T)


def _rope_tab(rot_dim):
    q = rot_dim // 4
    t = np.arange(2048)
    rows = (t // 64).astype(np.float32)
    cols = (t % 64).astype(np.float32)
    inv = (np.float32(10000.0) ** (-np.arange(q, dtype=np.float32) / np.float32(q))).astype(np.float32)
    ar = rows[None, :] * inv[:, None]
    ac = cols[None, :] * inv[:, None]
    ang = np.concatenate([ar, ar, ac, ac], 0).astype(np.float32)
    C = np.cos(ang).astype(np.float32)
    S = np.sin(ang).astype(np.float32)
    R = np.zeros((rot_dim, rot_dim), np.float32)
    for base in (0, 2 * q):
        for i in range(q):
            R[base + i, base + i + q] = -1.0
            R[base + i + q, base + i] = 1.0
    return C, S, R


def _consts():
    import ml_dtypes
    c = np.zeros((128, 4, 128), np.float32)
    c[:, 0, :] = np.eye(128)
    c[:, 1, :] = 1.0
    _, _, RB = _rope_tab(128)
    _, _, RA = _rope_tab(64)
    c[:, 2, :] = RB.T
    c[:64, 3, :64] = RA.T
    return c.astype(ml_dtypes.bfloat16)


def _nab(rpb):
    out = np.full((128, 4, 15, 64), -30000.0, np.float32)
    qc = np.arange(64)
    cs = np.clip(qc - 8, 0, 48)
    kc = np.arange(64)
    col_in = (kc[:, None] >= cs[None, :]) & (kc[:, None] < cs[None, :] + 16)
    dc = np.clip(kc[:, None] - qc[None, :] + 15, 0, 30)
    for a in range(2):
        for m in range(15):
            dr = m - 7 + a
            if dr < -7 or dr > 7:
                continue
            for h in range(4):
                vals = rpb[h, dr + 7][dc]
                out[a * 64:(a + 1) * 64, h, m, :] = np.where(col_in, vals, np.float32(-30000.0))
    return out


def _ind(hf):
    out = np.zeros((128, 16, 6), np.float32)
    base = 16 * hf
    for lr in range(16):
        r = base + lr
        r0 = min(max(r - 4, 0), 24)
        Rs = lr if lr <= 12 else 12
        R0 = Rs - Rs % 2
        for s in range(6):
            for a in range(2):
                ab = base - 4 + R0 + 2 * s + a
                if 0 <= ab <= 31 and r0 <= ab <= r0 + 7:
                    out[a * 64:(a + 1) * 64, lr, s] = 1.0
    return out


_CACHE = {}


def _prog(name, fn):
    if name not in _CACHE:
        _CACHE[name] = fn()
    return _CACHE[name]


def kernel(x, c, ctx, c_ctx, w_mod, b_mod, g_mix, g_ffn, w_in, w_uq, g_qa, w_ukv, g_kva, g_qn, g_kn,
           rpb, w_conv, w_branch, w_o, w_group, b_group, w_router, b_router, w_gate_e, w_up_e, w_down_e, g_final):
    f32 = np.float32
    cores = list(range(NCORES))
    x = np.asarray(x, f32)
    ctx = np.asarray(ctx, f32)
    c5 = np.concatenate([np.asarray(c, f32), np.asarray(c_ctx, f32)[None]], 0)
    cT = np.ascontiguousarray(c5.T.reshape(KC, 128, 5).transpose(1, 0, 2))
    ims = [{"cT": cT, "wm": np.ascontiguousarray(w_mod[:, :, i * MCOL:(i + 1) * MCOL]), "bm": np.ascontiguousarray(b_mod[:, i * MCOL:(i + 1) * MCOL])}
           for i in cores]
    res = run_bass_kernel_spmd(_prog("M", build_M), ims, core_ids=cores)
    mod = np.concatenate([r["mod"] for r in res.results], axis=2)
    consts = _consts()
    CA_, SA_, _ = _rope_tab(64)
    CB_, SB_, _ = _rope_tab(128)
    xlat, xctx = x, ctx
    for l in range(2):
        nab = _nab(np.asarray(rpb[l], f32))
        wr = np.ascontiguousarray(np.concatenate([w_group[l], w_router[l]], 1), f32)
        brow = np.concatenate([b_group[l], b_router[l]])[None].astype(f32)
        ims = []
        for cid in cores:
            b, hf = cid // 2, cid % 2
            o = 1 - hf
            x_own = np.concatenate([xctx[b, hf * 128:(hf + 1) * 128], xlat[b, hf * 1024:(hf + 1) * 1024]], 0)
            x_oth = np.concatenate([xctx[b, o * 128:(o + 1) * 128], xlat[b, o * 1024:(o + 1) * 1024]], 0)
            ml = mod[l, b].reshape(6, D)
            mc = mod[l, 4].reshape(6, D)
            fm = np.zeros((128, FM_N), f32)
            fm[:, FM_GMIX:FM_GMIX + 16] = _fm(g_mix[l], 16)
            fm[:, FM_SC1:FM_SC1 + 16] = _fm(ml[1], 16)
            fm[:, FM_SC1 + 16:FM_SC1 + 32] = _fm(mc[1], 16)
            fm[:, FM_SH1:FM_SH1 + 16] = _fm(ml[0], 16)
            fm[:, FM_SH1 + 16:FM_SH1 + 32] = _fm(mc[0], 16)
            fm[:, FM_GQA:FM_GQA + 4] = _fm(g_qa[l], 4)
            fm[:, FM_GKVA:FM_GKVA + 4] = _fm(g_kva[l], 4)
            fm[:, FM_GQN] = g_qn[l]
            fm[:, FM_GKN] = g_kn[l]
            for ci in range(4):
                for tap in range(3):
                    fm[:, FM_WCONV + ci * 3 + tap] = w_conv[l, tap, ci * 128:(ci + 1) * 128]
            fm[:, FM_FLAGB] = 1.0 if hf == 1 else 0.0
            fm[:, FM_FLAGA] = 1.0 if hf == 0 else 0.0
            rows = np.stack([np.asarray(g_ffn[l], f32), ml[4], mc[4], ml[3], mc[3], ml[2], mc[2]]).astype(f32)
            so, sn = slice(hf * 1024, (hf + 1) * 1024), slice(o * 1024, (o + 1) * 1024)
            ropeA = np.ascontiguousarray(np.stack([np.stack([CA_[:, so], CA_[:, sn]], 1), np.stack([SA_[:, so], SA_[:, sn]], 1)], 1))
            ropeB = np.ascontiguousarray(np.stack([np.stack([CB_[:, so], CB_[:, sn]], 1), np.stack([SB_[:, so], SB_[:, sn]], 1)], 1))
            ims.append({"x_own": x_own, "x_oth": x_oth, "w_in": w_in[l], "w_uq": w_uq[l], "w_ukv": w_ukv[l], "fm": fm, "rows": rows,
                        "ropeA": ropeA, "ropeB": ropeB, "consts": consts, "nab": nab, "ind": _ind(hf), "wr": wr, "brow": brow,
                        "w_branch": w_branch[l], "w_o": w_o[l]})
        resA = run_bass_kernel_spmd(_prog("A", build_A), ims, core_ids=cores).results
        h2_all = np.concatenate([r["h2"] for r in resA], 0)
        wr_all = np.concatenate([r["wrout"] for r in resA], 0)
        cstB, thrB = _b2_consts()
        ims = [{"h2": h2_all, "wsel": np.ascontiguousarray(wr_all[:, 8 * g:8 * g + 8]), "wg": w_gate_e[l, 8 * g:8 * g + 8], "wu": w_up_e[l, 8 * g:8 * g + 8],
                "wd": w_down_e[l, 8 * g:8 * g + 8], "cst": cstB, "thr": thrB} for g in cores]
        resB = run_bass_kernel_spmd(_prog("B2", build_B2), ims, core_ids=cores).results
        ims = []
        for cid in cores:
            b = cid // 2
            cbk = np.stack([resB[g]["contrib"][cid * NOWN:(cid + 1) * NOWN] for g in cores], 0)
            rows = np.stack([mod[l, b].reshape(6, D)[5], mod[l, 4].reshape(6, D)[5], np.asarray(g_final, f32)]).astype(f32)
            ims.append({"x1": resA[cid]["x1"], "cb": cbk, "rows": rows})
        final = (l == 1)
        resC = run_bass_kernel_spmd(_prog("C%d" % final, lambda: build_C(final)), ims, core_ids=cores).results
        nl = np.empty_like(xlat)
        ncx = np.empty_like(xctx)
        for cid in cores:
            b, hf = cid // 2, cid % 2
            ncx[b, hf * 128:(hf + 1) * 128] = resC[cid]["x2"][0:128]
            nl[b, hf * 1024:(hf + 1) * 1024] = resC[cid]["x2"][128:]
        xlat, xctx = nl, ncx
    return xlat
```

```python
import numpy as np
import concourse.bass as bass
import concourse.mybir as mybir
from concourse.bass_utils import run_bass_kernel_spmd

F32 = mybir.dt.float32
BF16 = mybir.dt.bfloat16
I32 = mybir.dt.int32
U32 = mybir.dt.uint32
AF = mybir.ActivationFunctionType
ALU = mybir.AluOpType
AX = mybir.AxisListType

D = 2048
KC = 16
NCORES = 8
EPS = 1e-6
IN_COLS = 13376
CA, CB, CC, CD, CG = 0, 1088, 2112, 3648, 5184


SAME_ENGINE_SYNC = True


class Buf:
    def __init__(self, name, t):
        self.name = name
        self.t = t
        self.w = None
        self.r = []
        self.sem = None
        self.cnt = 0
        self.multi = False
        self.wl = {}


class Prog:
    def __init__(self, nc):
        self.nc = nc
        self.eng = {"pe": nc.tensor, "act": nc.scalar, "dve": nc.vector, "pool": nc.gpsimd, "sp": nc.sync}
        self.sem = {k: nc.alloc_semaphore("sem_" + k) for k in ("pe", "act", "dve", "pool")}
        self.cnt = {k: 0 for k in ("pe", "act", "dve", "pool")}
        self.seen = {k: {} for k in self.eng}
        self.pend = {k: ([], []) for k in self.eng}
        self.dma_bufs = []
        self.nbuf = 0

    def sb(self, name, shape, dt):
        return Buf(name, self.nc.alloc_sbuf_tensor(name, list(shape), dt))

    def ps(self, name, shape, dt):
        return Buf(name, self.nc.alloc_psum_tensor(name, list(shape), dt))

    def dram(self, name, shape, dt, kind="Internal"):
        b = Buf(name, self.nc.dram_tensor(name, list(shape), dt, kind=kind).ap())
        b.multi = True
        return b

    @staticmethod
    def _wev(b):
        return list(b.wl.values()) if b.multi else [b.w]

    @staticmethod
    def _setw(b, ev):
        if b.multi:
            b.wl[ev[0]] = ev
        else:
            b.w = ev
        b.r = []

    def _wait(self, ek, evs):
        e = self.eng[ek]
        need = {}
        for ev in evs:
            if ev is None:
                continue
            sk, sem, val, src = ev
            if src == ek and (ek == "pe" or not SAME_ENGINE_SYNC):
                continue
            if self.seen[ek].get(sk, 0) >= val:
                continue
            if sk not in need or need[sk][1] < val:
                need[sk] = (sem, val)
        for sk, (sem, val) in need.items():
            e.wait_ge(sem, val)
            self.seen[ek][sk] = val

    def op(self, ek, fn, R=(), W=(), inc=True):
        evs = []
        for b in R:
            evs.extend(self._wev(b))
        for b in W:
            evs.extend(self._wev(b))
            evs.extend(b.r)
        self._wait(ek, evs)
        ins = fn(self.eng[ek])
        pr, pw = self.pend[ek]
        pr.extend(R)
        pw.extend(W)
        if not inc:
            return
        self.cnt[ek] += 1
        ev = ("e_" + ek, self.sem[ek], self.cnt[ek], ek)
        ins.then_inc(self.sem[ek], 1)
        for b in pr:
            b.r.append(ev)
        for b in pw:
            self._setw(b, ev)
        self.pend[ek] = ([], [])

    def dma(self, q, fn, R, W, sb):
        if sb.sem is None:
            sb.sem = self.nc.alloc_semaphore("dsem_%d" % len(self.dma_bufs))
            sb.semkey = "d_%d" % len(self.dma_bufs)
            self.dma_bufs.append(sb)
        evs = []
        for b in R:
            evs.extend(self._wev(b))
        for b in W:
            if not b.multi:
                evs.extend(self._wev(b))
            evs.extend(b.r)
        if sb.cnt > 0:
            evs.append((sb.semkey, sb.sem, sb.cnt, "dma"))
        self._wait(q, evs)
        ins = fn(self.eng[q])
        sb.cnt += 16
        ins.then_inc(sb.sem, 16)
        ev = (sb.semkey, sb.sem, sb.cnt, "dma")
        for b in R:
            b.r.append(ev)
        for b in W:
            self._setw(b, ev)

    def finish(self):
        evs = [(b.semkey, b.sem, b.cnt, "dma") for b in self.dma_bufs]
        self._wait("sp", evs)


def bcast_rows(ap_row, nparts):
    return ap_row.partition_broadcast(nparts)


MCOL = 12288 // NCORES


def build_M():
    nc = bass.Bass("TRN2", target_bir_lowering=False)
    P = Prog(nc)
    cT = P.dram("cT", [128, KC, 5], F32, "ExternalInput")
    wm = P.dram("wm", [2, D, MCOL], F32, "ExternalInput")
    bm = P.dram("bm", [2, MCOL], F32, "ExternalInput")
    out = P.dram("mod", [2, 5, MCOL], F32, "ExternalOutput")
    c_sb = P.sb("c_sb", [128, KC, 5], F32)
    s_sb = P.sb("s_sb", [128, KC, 5], F32)
    wts = [P.sb("wt%d" % i, [128, KC, 512], F32) for i in range(2)]
    b_sb = P.sb("b_sb", [5, 2, MCOL], F32)
    o_sb = P.sb("o_sb", [5, 2, MCOL], F32)
    pss = [P.ps("ps%d" % i, [128, 512], F32) for i in range(2)]
    P.dma("sp", lambda e: e.dma_start(out=c_sb.t[:], in_=cT.t[:, :, :]), [cT], [c_sb], c_sb)
    for l in range(2):
        P.dma("sp", lambda e, l=l: e.dma_start(out=b_sb.t[:, l, :], in_=bm.t[l:l + 1, :].partition_broadcast(5)),
              [bm], [b_sb], b_sb)
    P.op("act", lambda e: e.activation(out=s_sb.t[:], in_=c_sb.t[:], func=AF.Sigmoid), [c_sb], [s_sb])
    P.op("dve", lambda e: e.tensor_tensor(out=s_sb.t[:], in0=s_sb.t[:], in1=c_sb.t[:], op=ALU.mult), [s_sb, c_sb], [s_sb])
    i = 0
    for l in range(2):
        for g in range(MCOL // 512):
            wt = wts[i % 2]
            ps = pss[i % 2]
            i += 1
            P.dma("sp", lambda e, wt=wt, l=l, g=g: e.dma_start(
                out=wt.t[:], in_=wm.t[l, :, g * 512:(g + 1) * 512].rearrange("(kc p) n -> p kc n", p=128)),
                [wm], [wt], wt)
            for kc in range(KC):
                P.op("pe", lambda e, wt=wt, ps=ps, kc=kc: e.matmul(
                    ps.t[0:5, :], lhsT=s_sb.t[:, kc, :], rhs=wt.t[:, kc, :], start=(kc == 0), stop=(kc == KC - 1)),
                    [s_sb, wt], [ps], inc=(kc == KC - 1))
            P.op("dve", lambda e, ps=ps, l=l, g=g: e.tensor_tensor(
                out=o_sb.t[:, l, g * 512:(g + 1) * 512], in0=ps.t[0:5, :], in1=b_sb.t[:, l, g * 512:(g + 1) * 512],
                op=ALU.add), [ps, b_sb], [o_sb])
    for l in range(2):
        P.dma("sp", lambda e, l=l: e.dma_start(out=out.t[l, :, :], in_=o_sb.t[:, l, :]), [o_sb], [out], o_sb)
    P.finish()
    return nc


NOWN = 1152
ARENA_N = 37376
DEBUG_Y = False
FM_GMIX, FM_SC1, FM_SH1, FM_GQA, FM_GKVA, FM_GQN, FM_GKN, FM_WCONV, FM_FLAGB, FM_FLAGA, FM_N = 0, 16, 48, 80, 84, 88, 89, 90, 102, 103, 104
ROW_GFFN, ROW_SC2, ROW_SH2, ROW_GA1 = 0, 1, 3, 5
MLA_SCALE = 192 ** -0.5
HD_SCALE = 128 ** -0.5
OWN_CHUNKS = [(0, 128), (128, 512), (640, 512)]


def build_A():
    nc = bass.Bass("TRN2", target_bir_lowering=False)
    P = Prog(nc)
    x_own = P.dram("x_own", [NOWN, D], F32, "ExternalInput")
    x_oth = P.dram("x_oth", [NOWN, D], F32, "ExternalInput")
    w_in = P.dram("w_in", [D, IN_COLS], F32, "ExternalInput")
    w_uq = P.dram("w_uq", [512, 768], F32, "ExternalInput")
    w_ukv = P.dram("w_ukv", [512, 1024], F32, "ExternalInput")
    fm_d = P.dram("fm", [128, FM_N], F32, "ExternalInput")
    rows_d = P.dram("rows", [7, D], F32, "ExternalInput")
    ropeA = P.dram("ropeA", [64, 2, 2, 1024], F32, "ExternalInput")
    ropeB = P.dram("ropeB", [128, 2, 2, 1024], F32, "ExternalInput")
    consts_d = P.dram("consts", [128, 4, 128], BF16, "ExternalInput")
    nab_d = P.dram("nab", [128, 4, 15, 64], F32, "ExternalInput")
    ind_d = P.dram("ind", [128, 16, 6], F32, "ExternalInput")
    wr_d = P.dram("wr", [D, 72], F32, "ExternalInput")
    brow_d = P.dram("brow", [1, 72], F32, "ExternalInput")
    w_br = P.dram("w_branch", [4, 512, D], F32, "ExternalInput")
    w_o = P.dram("w_o", [D, D], F32, "ExternalInput")
    x1_o = P.dram("x1", [NOWN, D], F32, "ExternalOutput")
    h2_o = P.dram("h2", [NOWN, D], BF16, "ExternalOutput")
    wr_o = P.dram("wrout", [NOWN, 64], F32, "ExternalOutput")

    arena2 = nc.alloc_sbuf_tensor("arena2", [128, 8 * 4 * NOWN], BF16)
    hT = Buf("hT", arena2[:, 0:KC * NOWN].rearrange("p (a b) -> p a b", b=NOWN))
    yT = [Buf("yT%d" % k, arena2[:, (KC + 4 * k) * NOWN:(KC + 4 * k + 4) * NOWN].rearrange("p (a b) -> p a b", b=NOWN)) for k in range(4)]
    arena = nc.alloc_sbuf_tensor("arena", [128, ARENA_N], BF16)
    wb = [P.sb("wb%d" % i, [128, KC, 256], BF16) for i in range(2)]
    consts = P.sb("consts_sb", [128, 4, 128], BF16)
    ident, ones, RBt, RAt = (consts.t[:, i, :] for i in range(4))
    fm = P.sb("fm_sb", [128, FM_N], F32)
    a1T = P.sb("a1T", [128, 32], F32)
    ind = P.sb("ind_sb", [128, 16, 6], F32)
    xt = [P.sb("xt%d" % i, [128, D], F32) for i in range(1)]
    xs = [P.sb("xs%d" % i, [128, D], BF16) for i in range(1)]
    st = [P.sb("st%d" % i, [128, 4], F32) for i in range(2)]
    ck = P.sb("ck", [128, 4, 512], BF16)
    sq = P.sb("sq", [128, 4, 512], BF16)
    rr = P.sb("rr", [128, 512], F32)
    tA = P.sb("tA", [128, 512], F32)
    tB = P.sb("tB", [128, 512], F32)
    cs = [P.sb("cs%d" % i, [128, 2, 512], F32) for i in range(1)]
    Pt = [P.sb("Pt%d" % i, [128, 512], BF16) for i in range(2)]
    psM = [P.ps("psM%d" % i, [128, 512], F32) for i in range(2)]
    psS = [P.ps("psS%d" % i, [128, 512], F32) for i in range(2)]
    psO = P.ps("psO", [128, 512], F32)
    psSum = P.ps("psSum", [128, 512], F32)
    psT = P.ps("psT", [128, 1024], BF16)
    psX = P.ps("psX", [128, 512], F32)

    state = {"wi": 0, "pm": 0, "xi": 0, "pt": 0, "ps": 0}

    def arena_buf(name, off, shape):
        n = 1
        for s in shape[1:]:
            n *= s
        ap = arena[:, off:off + n]
        if len(shape) == 3:
            ap = ap.rearrange("p (a b) -> p a b", b=shape[2])
        b = Buf(name, None)
        b.ap = ap
        return b, off + n

    P.dma("sp", lambda e: e.dma_start(out=consts.t[:], in_=consts_d.t[:, :, :]), [consts_d], [consts], consts)
    P.dma("sp", lambda e: e.dma_start(out=fm.t[:], in_=fm_d.t[:, :]), [fm_d], [fm], fm)
    P.dma("sp", lambda e: e.dma_start(out=ind.t[:], in_=ind_d.t[:, :, :]), [ind_d], [ind], ind)
    for who in range(2):
        P.op("dve", lambda e, who=who: e.scalar_tensor_tensor(
            out=a1T.t[:, who * 16:(who + 1) * 16], in0=fm.t[:, FM_SC1 + who * 16:FM_SC1 + (who + 1) * 16], scalar=1.0,
            in1=fm.t[:, FM_GMIX:FM_GMIX + 16], op0=ALU.add, op1=ALU.mult), [fm], [a1T])
    def rstd_from_ss(ss_ap, n, inv, Rb, out_ap, Wb):
        P.op("dve", lambda e: e.tensor_scalar(out=out_ap, in0=ss_ap, scalar1=inv, scalar2=EPS, op0=ALU.mult, op1=ALU.add), Rb, Wb)
        P.op("act", lambda e: e.activation(out=out_ap, in_=out_ap, func=AF.Sqrt), Wb, Wb)
        P.op("dve", lambda e: e.reciprocal(out=out_ap, in_=out_ap), Wb, Wb)

    hT_cache = {}

    def build_hT(xsrc):
        if xsrc.name in hT_cache:
            scr = hT_cache[xsrc.name]
            P.dma("sp", lambda e: e.dma_start(out=hT.t[:, :, :], in_=scr.t[:, :, :]), [scr], [hT], hT)
            return
        build_hT_full(xsrc)
        scr = P.dram("hTscr_" + xsrc.name, [128, KC, NOWN], BF16)
        hT_cache[xsrc.name] = scr
        P.dma("sp", lambda e: e.dma_start(out=scr.t[:, :, :], in_=hT.t[:, :, :]), [hT], [scr], hT)

    def build_hT_full(xsrc):
        for t in range(9):
            who = 1 if t == 0 else 0
            i = 0
            state["xi"] += 1
            xb, xsb, stb = xt[i], xs[i], st[i]
            P.dma("sp", lambda e: e.dma_start(out=xb.t[:], in_=xsrc.t[t * 128:(t + 1) * 128, :]), [xsrc], [xb], xb)
            P.op("act", lambda e: e.activation(out=xsb.t[:], in_=xb.t[:], func=AF.Square, accum_out=stb.t[:, 0:1]), [xb], [xsb, stb])
            rstd_from_ss(stb.t[:, 0:1], 1, 1.0 / D, [stb], stb.t[:, 1:2], [stb])
            P.op("dve", lambda e: e.tensor_scalar(out=xsb.t[:], in0=xb.t[:], scalar1=stb.t[:, 1:2], scalar2=None, op0=ALU.mult), [xb, stb], [xsb])
            for half in range(2):
                for j in range(8):
                    kc = half * 8 + j
                    P.op("pe", lambda e, kc=kc, j=j: e.transpose(psT.t[:, j * 128:(j + 1) * 128], xsb.t[:, kc * 128:(kc + 1) * 128], ident),
                         [xsb, consts], [psT], inc=(j == 7))
                for j in range(8):
                    kc = half * 8 + j
                    ek = "dve" if j % 2 == 0 else "act"
                    if ek == "dve":
                        P.op("dve", lambda e, kc=kc, j=j: e.tensor_scalar(
                            out=hT.t[:, kc, t * 128:(t + 1) * 128], in0=psT.t[:, j * 128:(j + 1) * 128],
                            scalar1=a1T.t[:, who * 16 + kc:who * 16 + kc + 1], scalar2=fm.t[:, FM_SH1 + who * 16 + kc:FM_SH1 + who * 16 + kc + 1],
                            op0=ALU.mult, op1=ALU.add), [psT, a1T, fm], [hT])
                    else:
                        P.op("act", lambda e, kc=kc, j=j: e.activation(
                            out=hT.t[:, kc, t * 128:(t + 1) * 128], in_=psT.t[:, j * 128:(j + 1) * 128], func=AF.Identity,
                            scale=a1T.t[:, who * 16 + kc:who * 16 + kc + 1], bias=fm.t[:, FM_SH1 + who * 16 + kc:FM_SH1 + who * 16 + kc + 1]),
                            [psT, a1T, fm], [hT])

    def load_w(src, r_ap, ncols):
        w = wb[state["wi"] % 2]
        state["wi"] += 1
        P.dma("pool", lambda e: e.dma_start(out=w.t[:, :, 0:ncols], in_=r_ap), [src], [w], w)
        return w

    def next_psM():
        p = psM[state["pm"] % 2]
        state["pm"] += 1
        return p

    def proj_F(col0, ncols, chunks, cb, mwidth=128):
        for g0 in range(0, ncols, 256):
            gn = min(256, ncols - g0)
            w = load_w(w_in, w_in.t[:, col0 + g0:col0 + g0 + gn].rearrange("(kc p) n -> p kc n", p=128), gn)
            for m0 in range(0, gn, mwidth):
                m = min(mwidth, gn - m0)
                for (t0, n) in chunks:
                    ps = next_psM()
                    for kc in range(KC):
                        P.op("pe", lambda e, kc=kc: e.matmul(ps.t[0:m, 0:n], lhsT=w.t[:, kc, m0:m0 + m], rhs=hT.t[:, kc, t0:t0 + n],
                                                             start=(kc == 0), stop=(kc == KC - 1)), [w, hT], [ps], inc=(kc == KC - 1))
                    cb(ps, (g0 + m0) // mwidth, m, t0, n)

    def proj_T(col0, ncols, tiles, cb):
        for g0 in range(0, ncols, 256):
            gn = min(256, ncols - g0)
            w = load_w(w_in, w_in.t[:, col0 + g0:col0 + g0 + gn].rearrange("(kc p) n -> p kc n", p=128), gn)
            for t0 in tiles:
                ps = next_psM()
                for kc in range(KC):
                    P.op("pe", lambda e, kc=kc: e.matmul(ps.t[:, 0:gn], lhsT=hT.t[:, kc, t0:t0 + 128], rhs=w.t[:, kc, 0:gn],
                                                         start=(kc == 0), stop=(kc == KC - 1)), [w, hT], [ps], inc=(kc == KC - 1))
                cb(ps, g0, gn, t0)

    def copy_out(ek, dst_ap, src_ap, Rb, Wb):
        if ek == "act":
            P.op("act", lambda e: e.copy(out=dst_ap, in_=src_ap), Rb, Wb)
        else:
            P.op(ek, lambda e: e.tensor_copy(out=dst_ap, in_=src_ap), Rb, Wb)

    def load_rope(tab, npart, which, l0, n):
        c = cs[0]
        P.dma("sp", lambda e: e.dma_start(out=c.t[0:npart, :, 0:n], in_=tab.t[:, :, which, l0:l0 + n]), [tab], [c], c)
        return c

    def rope_apply(x_ap, xbuf, npart, Rt, c, n, dst_ap, dstbuf):
        P.op("pe", lambda e: e.matmul(psX.t[0:npart, 0:n], lhsT=Rt[0:npart, 0:npart], rhs=x_ap, start=True, stop=True), [xbuf, consts], [psX])
        P.op("dve", lambda e: e.tensor_tensor(out=tA.t[0:npart, 0:n], in0=x_ap, in1=c.t[0:npart, 0, 0:n], op=ALU.mult), [xbuf, c], [tA])
        P.op("dve", lambda e: e.tensor_tensor(out=tB.t[0:npart, 0:n], in0=psX.t[0:npart, 0:n], in1=c.t[0:npart, 1, 0:n], op=ALU.mult), [psX, c], [tB])
        P.op("dve", lambda e: e.tensor_tensor(out=dst_ap, in0=tA.t[0:npart, 0:n], in1=tB.t[0:npart, 0:n], op=ALU.add), [tA, tB], [dstbuf])

    def lat_off(t0):
        return t0 - 128

    def attention(QT_list, KT_list, Vbuf, vcol, key_tiles, q0, qn, scale, dst, dstbuf, Rbufs):
        nk = len(key_tiles)

        def emit_S(i):
            ps = psS[(state["ps"] + i) % 2]
            kt = key_tiles[i]
            for j, (qf, kf) in enumerate(zip(QT_list, KT_list)):
                P.op("pe", lambda e, j=j: e.matmul(ps.t[:, 0:qn], lhsT=kf(kt), rhs=qf(q0, qn), start=(j == 0), stop=(j == len(QT_list) - 1)),
                     Rbufs, [ps], inc=(j == len(QT_list) - 1))
            return ps
        pss = {0: emit_S(0)}
        for i in range(nk):
            if i + 1 < nk:
                pss[i + 1] = emit_S(i + 1)
            ps = pss.pop(i)
            pt = Pt[state["pt"] % 2]
            state["pt"] += 1
            P.op("act", lambda e: e.activation(out=pt.t[:, 0:qn], in_=ps.t[:, 0:qn], func=AF.Exp, scale=scale), [ps], [pt])
            kt = key_tiles[i]
            P.op("pe", lambda e: e.matmul(psO.t[:, 0:qn], lhsT=Vbuf.ap[:, kt, vcol:vcol + 128], rhs=pt.t[:, 0:qn], start=(i == 0), stop=(i == nk - 1)),
                 [Vbuf, pt], [psO], inc=False)
            P.op("pe", lambda e: e.matmul(psSum.t[:, 0:qn], lhsT=ones, rhs=pt.t[:, 0:qn], start=(i == 0), stop=(i == nk - 1)),
                 [consts, pt], [psSum], inc=True)
        state["ps"] += nk
        P.op("dve", lambda e: e.reciprocal(out=rr.t[:, 0:qn], in_=psSum.t[:, 0:qn]), [psSum], [rr])
        P.op("dve", lambda e: e.tensor_tensor(out=dst, in0=psO.t[:, 0:qn], in1=rr.t[:, 0:qn], op=ALU.mult), [psO, rr], [dstbuf])

    def barrier():
        evs = [("e_" + k, P.sem[k], P.cnt[k], "bar") for k in P.cnt if P.cnt[k] > 0]
        evs += [(b.semkey, b.sem, b.cnt, "dma") for b in P.dma_bufs]
        for k in P.eng:
            P._wait(k, evs)

    def mla_norm(gcol, n):
        for c in range(4):
            P.op("pe", lambda e, c=c: e.matmul(psX.t[:, 0:n], lhsT=ones, rhs=sq.t[:, c, 0:n], start=(c == 0), stop=(c == 3)), [consts, sq], [psX], inc=(c == 3))
        rstd_from_ss(psX.t[:, 0:n], n, 1.0 / 512, [psX], rr.t[:, 0:n], [rr])
        for c in range(4):
            P.op("dve", lambda e, c=c: e.scalar_tensor_tensor(out=sq.t[:, c, 0:n], in0=ck.t[:, c, 0:n], scalar=fm.t[:, gcol + c:gcol + c + 1],
                                                             in1=rr.t[:, 0:n], op0=ALU.mult, op1=ALU.mult), [ck, fm, rr], [sq])

    def cb_ck(ps, ci, m, t0, n):
        P.op("act", lambda e: e.copy(out=ck.t[:, ci, 0:n], in_=ps.t[:, 0:n]), [ps], [ck])
        P.op("act", lambda e: e.activation(out=sq.t[:, ci, 0:n], in_=ps.t[:, 0:n], func=AF.Square), [ps], [sq])

    off = 0
    KnT, off = arena_buf("KnT", off, [128, 4, 2304])
    kpeT, off = arena_buf("kpeT", off, [128, 2304])
    Vm, off = arena_buf("Vm", off, [128, 18, 512])
    QnT, off = arena_buf("QnT", off, [128, 4, NOWN])
    QpT, off = arena_buf("QpT", off, [128, 4, NOWN])
    wuq, off = arena_buf("wuq", off, [128, 4, 768])
    wukv, off = arena_buf("wukv", off, [128, 4, 1024])
    assert off <= ARENA_N
    wuq_sem = P.sb("wuq_sem", [128, 2], F32)
    P.dma("pool", lambda e: e.dma_start(out=wuq.ap, in_=w_uq.t[:, :].rearrange("(c p) n -> p c n", p=128)), [w_uq], [wuq], wuq_sem)
    P.dma("pool", lambda e: e.dma_start(out=wukv.ap, in_=w_ukv.t[:, :].rearrange("(c p) n -> p c n", p=128)), [w_ukv], [wukv], wuq_sem)

    def mla_tokens(which, kvbase):
        for (t0, n) in OWN_CHUNKS:
            is_lat = t0 >= 128
            if is_lat:
                c = load_rope(ropeA, 64, which, lat_off(t0), n)
            proj_F(CA + 512, 512, [(t0, n)], cb_ck)
            mla_norm(FM_GKVA, n)
            for h in range(4):
                ps = next_psM()
                for cc in range(4):
                    P.op("pe", lambda e, cc=cc: e.matmul(ps.t[:, 0:n], lhsT=wukv.ap[:, cc, h * 256:h * 256 + 128], rhs=sq.t[:, cc, 0:n],
                                                         start=(cc == 0), stop=(cc == 3)), [wukv, sq], [ps], inc=(cc == 3))
                copy_out("act" if h % 2 else "dve", KnT.ap[:, h, kvbase + t0:kvbase + t0 + n], ps.t[:, 0:n], [ps], [KnT])
            for tt in range(n // 128):
                ps = next_psM()
                for cc in range(4):
                    P.op("pe", lambda e, cc=cc: e.matmul(ps.t[:, 0:512].rearrange("p (h x) -> p h x", x=128), lhsT=sq.t[:, cc, tt * 128:(tt + 1) * 128],
                                                         rhs=wukv.ap[:, cc, :].rearrange("p (h x) -> p h x", x=256)[:, :, 128:256],
                                                         start=(cc == 0), stop=(cc == 3)), [wukv, sq], [ps], inc=(cc == 3))
                copy_out("act" if tt % 2 else "dve", Vm.ap[:, (kvbase + t0) // 128 + tt, :], ps.t[:, 0:512], [ps], [Vm])

            def cb_kpe(ps, ci, m, t0_, n_):
                if not is_lat:
                    copy_out("dve", kpeT.ap[0:64, kvbase + t0:kvbase + t0 + n], ps.t[0:64, 0:n], [ps], [kpeT])
                else:
                    copy_out("dve", ck.t[0:64, 0, 0:n], ps.t[0:64, 0:n], [ps], [ck])
                    rope_apply(ck.t[0:64, 0, 0:n], ck, 64, RAt, c, n, kpeT.ap[0:64, kvbase + t0:kvbase + t0 + n], kpeT)
            proj_F(CA + 1024, 64, [(t0, n)], cb_kpe, mwidth=64)
            if which == 1:
                continue
            proj_F(CA, 512, [(t0, n)], cb_ck)
            mla_norm(FM_GQA, n)
            for h in range(4):
                ps = next_psM()
                for cc in range(4):
                    P.op("pe", lambda e, cc=cc: e.matmul(ps.t[:, 0:n], lhsT=wuq.ap[:, cc, h * 192:h * 192 + 128], rhs=sq.t[:, cc, 0:n],
                                                         start=(cc == 0), stop=(cc == 3)), [wuq, sq], [ps], inc=(cc == 3))
                copy_out("act", QnT.ap[:, h, t0:t0 + n], ps.t[:, 0:n], [ps], [QnT])
                ps = next_psM()
                for cc in range(4):
                    P.op("pe", lambda e, cc=cc: e.matmul(ps.t[0:64, 0:n], lhsT=wuq.ap[:, cc, h * 192 + 128:h * 192 + 192], rhs=sq.t[:, cc, 0:n],
                                                         start=(cc == 0), stop=(cc == 3)), [wuq, sq], [ps], inc=(cc == 3))
                if not is_lat:
                    copy_out("dve", QpT.ap[0:64, h, t0:t0 + n], ps.t[0:64, 0:n], [ps], [QpT])
                else:
                    copy_out("dve", ck.t[0:64, 0, 0:n], ps.t[0:64, 0:n], [ps], [ck])
                    rope_apply(ck.t[0:64, 0, 0:n], ck, 64, RAt, c, n, QpT.ap[0:64, h, t0:t0 + n], QpT)

    def run_attn_full(QTs, KTs, Vb, vcolf, scale, ydst):
        for (q0, qn) in OWN_CHUNKS:
            kts = [0, 9] if q0 == 0 else list(range(18))
            for h in range(4):
                pairs = QTs(h)
                attention([p[0] for p in pairs], [p[1] for p in pairs], Vb, vcolf(h), kts, q0, qn, scale,
                          ydst.t[:, h, q0:q0 + qn], ydst, [b for b in KTs])

    build_hT(x_oth)
    mla_tokens(1, NOWN)
    build_hT(x_own)
    mla_tokens(0, 0)
    run_attn_full(lambda h: [(lambda q0, qn: QnT.ap[:, h, q0:q0 + qn], lambda kt: KnT.ap[:, h, kt * 128:(kt + 1) * 128]),
                             (lambda q0, qn: QpT.ap[0:64, h, q0:q0 + qn], lambda kt: kpeT.ap[0:64, kt * 128:(kt + 1) * 128])],
                  [KnT, kpeT, QnT, QpT], Vm, lambda h: h * 128, MLA_SCALE, yT[0])
    barrier()

    off = 0
    KTb, off = arena_buf("KTb", off, [128, 2, 2304])
    Vb, off = arena_buf("Vb", off, [128, 18, 256])
    QTb, off = arena_buf("QTb", off, [128, 4, NOWN])

    def qknorm(ps, n, gcol, c, dst_ap, dstbuf):
        P.op("act", lambda e: e.copy(out=ck.t[:, 0, 0:n], in_=ps.t[:, 0:n]), [ps], [ck])
        P.op("act", lambda e: e.activation(out=sq.t[:, 0, 0:n], in_=ps.t[:, 0:n], func=AF.Square), [ps], [sq])
        P.op("pe", lambda e: e.matmul(psX.t[:, 0:n], lhsT=ones, rhs=sq.t[:, 0, 0:n], start=True, stop=True), [consts, sq], [psX])
        rstd_from_ss(psX.t[:, 0:n], n, 1.0 / 128, [psX], rr.t[:, 0:n], [rr])
        if c is None:
            P.op("dve", lambda e: e.scalar_tensor_tensor(out=dst_ap, in0=ck.t[:, 0, 0:n], scalar=fm.t[:, gcol:gcol + 1], in1=rr.t[:, 0:n],
                                                         op0=ALU.mult, op1=ALU.mult), [ck, fm, rr], [dstbuf])
        else:
            P.op("dve", lambda e: e.scalar_tensor_tensor(out=sq.t[:, 1, 0:n], in0=ck.t[:, 0, 0:n], scalar=fm.t[:, gcol:gcol + 1], in1=rr.t[:, 0:n],
                                                         op0=ALU.mult, op1=ALU.mult), [ck, fm, rr], [sq])
            rope_apply(sq.t[:, 1, 0:n], sq, 128, RBt, c, n, dst_ap, dstbuf)

    def gqa_tokens(which, kvbase):
        for (t0, n) in OWN_CHUNKS:
            c = load_rope(ropeB, 128, which, lat_off(t0), n) if t0 >= 128 else None
            proj_F(CB + 512, 256, [(t0, n)], lambda ps, ci, m, t0_, n_: qknorm(ps, n, FM_GKN, c, KTb.ap[:, ci, kvbase + t0:kvbase + t0 + n], KTb))
            if which == 0:
                proj_F(CB, 512, [(t0, n)], lambda ps, ci, m, t0_, n_: qknorm(ps, n, FM_GQN, c, QTb.ap[:, ci, t0:t0 + n], QTb))
        proj_T(CB + 768, 256, [t * 128 for t in range(9)],
               lambda ps, g0, gn, t0: copy_out("act" if (t0 // 128) % 2 else "dve", Vb.ap[:, (kvbase + t0) // 128, :], ps.t[:, 0:256], [ps], [Vb]))

    build_hT(x_oth)
    gqa_tokens(1, NOWN)
    build_hT(x_own)
    gqa_tokens(0, 0)
    run_attn_full(lambda h: [(lambda q0, qn: QTb.ap[:, h, q0:q0 + qn], lambda kt: KTb.ap[:, h // 2, kt * 128:(kt + 1) * 128])],
                  [KTb, QTb], Vb, lambda h: (h // 2) * 128, HD_SCALE, yT[1])
    barrier()

    off = 0
    KTc, off = arena_buf("KTc", off, [128, 4, 1792])
    Vctx, off = arena_buf("Vctx", off, [128, 2, 512])
    Vev, off = arena_buf("Vev", off, [128, 12, 512])
    QTc, off = arena_buf("QTc", off, [128, 4, NOWN])
    E2, off = arena_buf("E2", off, [128, 60, 64])
    for h in range(4):
        P.dma("sp", lambda e: e.dma_start(out=xt[0].t[:, 0:960], in_=nab_d.t[:, h, :, :].rearrange("p a b -> p (a b)")),
              [nab_d], [xt[0]], xt[0])
        P.op("act", lambda e: e.activation(out=E2.ap[:, h * 15:(h + 1) * 15, :].rearrange("p a b -> p (a b)"), in_=xt[0].t[:, 0:960], func=AF.Exp),
             [xt[0]], [E2])

    def kc_dst(which, t0):
        if which == 0:
            return 0 if t0 == 0 else 512 + (t0 - 128)
        if t0 == 0:
            return 128
        return 256 if t0 == 896 else 1536

    build_hT(x_oth)
    oth_chunks = [(0, 128), (896, 256), (128, 256)]
    proj_F(CC + 512, 512, oth_chunks, lambda ps, ci, m, t0, n: copy_out("act" if ci % 2 else "dve", KTc.ap[:, ci, kc_dst(1, t0):kc_dst(1, t0) + n], ps.t[:, 0:n], [ps], [KTc]))

    def cb_v_oth(ps, g0, gn, t0):
        if t0 == 0:
            dst = Vctx.ap[:, 1, g0:g0 + gn]
            db = Vctx
        else:
            ti = {896: 0, 1024: 1, 128: 10, 256: 11}[t0]
            dst = Vev.ap[:, ti, g0:g0 + gn]
            db = Vev
        copy_out("dve", dst, ps.t[:, 0:gn], [ps], [db])
    proj_T(CC + 1024, 512, [0, 896, 1024, 128, 256], cb_v_oth)
    build_hT(x_own)
    proj_F(CC, 512, OWN_CHUNKS, lambda ps, ci, m, t0, n: copy_out("act" if ci % 2 else "dve", QTc.ap[:, ci, t0:t0 + n], ps.t[:, 0:n], [ps], [QTc]))
    proj_F(CC + 512, 512, OWN_CHUNKS, lambda ps, ci, m, t0, n: copy_out("act" if ci % 2 else "dve", KTc.ap[:, ci, kc_dst(0, t0):kc_dst(0, t0) + n], ps.t[:, 0:n], [ps], [KTc]))

    def cb_v_own(ps, g0, gn, t0):
        if t0 == 0:
            copy_out("dve", Vctx.ap[:, 0, g0:g0 + gn], ps.t[:, 0:gn], [ps], [Vctx])
        else:
            copy_out("dve", Vev.ap[:, 2 + (t0 - 128) // 128, g0:g0 + gn], ps.t[:, 0:gn], [ps], [Vev])
    proj_T(CC + 1024, 512, [t * 128 for t in range(9)], cb_v_own)
    for h in range(4):
        attention([lambda q0, qn: QTc.ap[:, h, q0:q0 + qn]], [lambda kt: KTc.ap[:, h, kt * 128:(kt + 1) * 128]], Vctx, h * 128, [0, 1], 0, 128,
                  HD_SCALE, yT[2].t[:, h, 0:128], yT[2], [KTc, QTc])
    for h in range(4):
        for lr in range(16):
            Rs = lr if lr <= 12 else 12
            Re = 11 if lr < 4 else lr + 7
            R0 = Rs - Rs % 2
            R1 = Re | 1
            npair = (R1 - R0 + 1) // 2
            nsl = npair + 2
            ps = psS[state["ps"] % 2]
            state["ps"] += 1
            qap = QTc.ap[:, h, 128 + lr * 64:128 + (lr + 1) * 64]
            for s_ in range(nsl):
                if s_ < npair:
                    R = R0 + 2 * s_
                    kap = KTc.ap[:, h, 256 + 64 * R:256 + 64 * R + 128]
                else:
                    kap = KTc.ap[:, h, (s_ - npair) * 128:(s_ - npair + 1) * 128]
                P.op("pe", lambda e: e.matmul(ps.t[:, s_ * 64:(s_ + 1) * 64], lhsT=kap, rhs=qap, start=True, stop=True), [KTc, QTc], [ps], inc=(s_ == nsl - 1))
            pt = Pt[state["pt"] % 2]
            state["pt"] += 1
            P.op("act", lambda e: e.activation(out=pt.t[:, 0:nsl * 64], in_=ps.t[:, 0:nsl * 64], func=AF.Exp, scale=HD_SCALE), [ps], [pt])
            for s_ in range(npair):
                R = R0 + 2 * s_
                m = R + 3 - lr
                P.op("dve", lambda e: e.scalar_tensor_tensor(out=pt.t[:, s_ * 64:(s_ + 1) * 64], in0=pt.t[:, s_ * 64:(s_ + 1) * 64], scalar=ind.t[:, lr, s_:s_ + 1],
                                                             in1=E2.ap[:, h * 15 + m, :], op0=ALU.mult, op1=ALU.mult), [pt, ind, E2], [pt])
            for s_ in range(nsl):
                if s_ < npair:
                    vap = Vev.ap[:, (R0 + 2 * s_) // 2, h * 128:(h + 1) * 128]
                else:
                    vap = Vctx.ap[:, s_ - npair, h * 128:(h + 1) * 128]
                P.op("pe", lambda e: e.matmul(psO.t[:, 0:64], lhsT=vap, rhs=pt.t[:, s_ * 64:(s_ + 1) * 64], start=(s_ == 0), stop=(s_ == nsl - 1)),
                     [Vev, Vctx, pt], [psO], inc=False)
                P.op("pe", lambda e: e.matmul(psSum.t[:, 0:64], lhsT=ones, rhs=pt.t[:, s_ * 64:(s_ + 1) * 64], start=(s_ == 0), stop=(s_ == nsl - 1)),
                     [consts, pt], [psSum], inc=True)
            P.op("dve", lambda e: e.reciprocal(out=rr.t[:, 0:64], in_=psSum.t[:, 0:64]), [psSum], [rr])
            P.op("dve", lambda e: e.tensor_tensor(out=yT[2].t[:, h, 128 + lr * 64:128 + (lr + 1) * 64], in0=psO.t[:, 0:64], in1=rr.t[:, 0:64], op=ALU.mult),
                 [psO, rr], [yT[2]])
    barrier()

    off = 0
    bg, off = arena_buf("bg", off, [128, 4, NOWN])
    cg, off = arena_buf("cg", off, [128, 4, NOWN])
    zlat, off = arena_buf("zlat", off, [128, 4, 1026])
    zctx, off = arena_buf("zctx", off, [128, 4, 130])
    cgo, off = arena_buf("cgo", off, [128, 4, 384])
    zo, off = arena_buf("zo", off, [128, 4, 384])
    och = [(0, 128), (128, 128), (1024, 128)]
    oslot = {0: 0, 128: 1, 1024: 2}
    build_hT(x_oth)
    proj_F(CD + 512, 512, och, lambda ps, ci, m, t0, n: copy_out("act", cgo.ap[:, ci, oslot[t0] * 128:(oslot[t0] + 1) * 128], ps.t[:, 0:n], [ps], [cgo]))
    proj_F(CD + 1024, 512, och, lambda ps, ci, m, t0, n: P.op("dve", lambda e: e.tensor_tensor(
        out=zo.ap[:, ci, oslot[t0] * 128:(oslot[t0] + 1) * 128], in0=ps.t[:, 0:n], in1=cgo.ap[:, ci, oslot[t0] * 128:(oslot[t0] + 1) * 128], op=ALU.mult), [ps, cgo], [zo]))
    for (dst, dcol, scol, fl) in [(zlat, 0, 383, FM_FLAGB), (zlat, 1025, 128, FM_FLAGA), (zctx, 0, 127, FM_FLAGB), (zctx, 129, 0, FM_FLAGA)]:
        P.op("dve", lambda e: e.tensor_scalar(out=dst.ap[:, :, dcol:dcol + 1], in0=zo.ap[:, :, scol:scol + 1], scalar1=fm.t[:, fl:fl + 1], scalar2=None, op0=ALU.mult),
             [zo, fm], [dst])
    build_hT(x_own)
    proj_F(CD, 512, OWN_CHUNKS, lambda ps, ci, m, t0, n: copy_out("act", bg.ap[:, ci, t0:t0 + n], ps.t[:, 0:n], [ps], [bg]))
    proj_F(CD + 512, 512, OWN_CHUNKS, lambda ps, ci, m, t0, n: copy_out("act", cg.ap[:, ci, t0:t0 + n], ps.t[:, 0:n], [ps], [cg]))

    def zdst(ci, t0, n):
        return (zctx, zctx.ap[:, ci, 1:1 + n]) if t0 == 0 else (zlat, zlat.ap[:, ci, 1 + t0 - 128:1 + t0 - 128 + n])
    proj_F(CD + 1024, 512, OWN_CHUNKS, lambda ps, ci, m, t0, n: P.op("dve", lambda e: e.tensor_tensor(
        out=zdst(ci, t0, n)[1], in0=ps.t[:, 0:n], in1=cg.ap[:, ci, t0:t0 + n], op=ALU.mult), [ps, cg], [zdst(ci, t0, n)[0]]))
    for ci in range(4):
        for (t0, n) in OWN_CHUNKS:
            zb, zoff = (zctx, 0) if t0 == 0 else (zlat, t0 - 128)
            wc = FM_WCONV + ci * 3
            P.op("dve", lambda e: e.tensor_scalar(out=tA.t[:, 0:n], in0=zb.ap[:, ci, zoff:zoff + n], scalar1=fm.t[:, wc:wc + 1], scalar2=None, op0=ALU.mult), [zb, fm], [tA])
            for tap in (1, 2):
                P.op("dve", lambda e: e.scalar_tensor_tensor(out=tA.t[:, 0:n], in0=zb.ap[:, ci, zoff + tap:zoff + tap + n], scalar=fm.t[:, wc + tap:wc + tap + 1],
                                                             in1=tA.t[:, 0:n], op0=ALU.mult, op1=ALU.add), [zb, fm, tA], [tA])
            P.op("dve", lambda e: e.tensor_tensor(out=yT[3].t[:, ci, t0:t0 + n], in0=tA.t[:, 0:n], in1=bg.ap[:, ci, t0:t0 + n], op=ALU.mult), [tA, bg], [yT[3]])
    barrier()
    if DEBUG_Y:
        ydbg = P.dram("ydbg", [4, 128, 4, NOWN], BF16, "ExternalOutput")
        for k in range(4):
            P.dma("sp", lambda e: e.dma_start(out=ydbg.t[k, :, :, :], in_=yT[k].t[:]), [yT[k]], [ydbg], yT[k])

    off = 0
    mT, off = arena_buf("mT", off, [128, KC, NOWN])
    macc = Buf("macc", None)
    macc.ap = arena[:, off:off + 2 * 2 * NOWN].bitcast(F32).rearrange("p (a b) -> p a b", b=NOWN)
    off += 4 * NOWN
    wbr = []
    for i in range(2):
        b_, off = arena_buf("wbr%d" % i, off, [128, 4, 256])
        b_.sembuf = P.sb("wbrsem%d" % i, [128, 2], F32)
        wbr.append(b_)
    assert off <= ARENA_N
    wi2 = 0
    for dg in range(8):
        for k in range(4):
            w = load_w(w_in, w_in.t[:, CG + k * D + dg * 256:CG + k * D + (dg + 1) * 256].rearrange("(kc p) n -> p kc n", p=128), 256)
            wbk = wbr[wi2 % 2]
            wi2 += 1
            P.dma("pool", lambda e: e.dma_start(out=wbk.ap, in_=w_br.t[k, :, dg * 256:(dg + 1) * 256].rearrange("(c p) n -> p c n", p=128)), [w_br], [wbk], wbk.sembuf)
            for dc in range(2):
                for (t0, n) in OWN_CHUNKS:
                    ps = next_psM()
                    for kc in range(KC):
                        P.op("pe", lambda e, kc=kc: e.matmul(ps.t[:, 0:n], lhsT=w.t[:, kc, dc * 128:(dc + 1) * 128], rhs=hT.t[:, kc, t0:t0 + n],
                                                             start=(kc == 0), stop=(kc == KC - 1)), [w, hT], [ps], inc=(kc == KC - 1))
                    ps2 = next_psM()
                    for cc in range(4):
                        P.op("pe", lambda e, cc=cc: e.matmul(ps2.t[:, 0:n], lhsT=wbk.ap[:, cc, dc * 128:(dc + 1) * 128], rhs=yT[k].t[:, cc, t0:t0 + n],
                                                             start=(cc == 0), stop=(cc == 3)), [wbk, yT[k]], [ps2], inc=(cc == 3))
                    P.op("act", lambda e: e.activation(out=tA.t[:, 0:n], in_=ps.t[:, 0:n], func=AF.Sigmoid), [ps], [tA])
                    if k == 0:
                        P.op("dve", lambda e: e.tensor_tensor(out=macc.ap[:, dc, t0:t0 + n], in0=ps2.t[:, 0:n], in1=tA.t[:, 0:n], op=ALU.mult), [ps2, tA], [macc])
                    else:
                        P.op("dve", lambda e: e.tensor_tensor(out=tB.t[:, 0:n], in0=ps2.t[:, 0:n], in1=tA.t[:, 0:n], op=ALU.mult), [ps2, tA], [tB])
                        if k < 3:
                            P.op("dve", lambda e: e.tensor_tensor(out=macc.ap[:, dc, t0:t0 + n], in0=macc.ap[:, dc, t0:t0 + n], in1=tB.t[:, 0:n], op=ALU.add), [macc, tB], [macc])
                        else:
                            P.op("dve", lambda e: e.tensor_tensor(out=mT.ap[:, dg * 2 + dc, t0:t0 + n], in0=macc.ap[:, dc, t0:t0 + n], in1=tB.t[:, 0:n], op=ALU.add), [macc, tB], [mT])
    barrier()

    wo = Buf("wo", None)
    wo.ap = arena2[:, 0:KC * D].rearrange("p (a b) -> p a b", b=D)
    wo.sembuf = P.sb("wosem", [128, 2], F32)
    for og in range(4):
        P.dma("pool", lambda e: e.dma_start(out=wo.ap[:, :, og * 512:(og + 1) * 512], in_=w_o.t[:, og * 512:(og + 1) * 512].rearrange("(kc p) n -> p kc n", p=128)),
              [w_o], [wo], wo.sembuf)
    reps = []
    for i in range(3):
        b_ = Buf("rep%d" % i, None)
        b_.ap = arena[:, off + i * 2 * D:off + (i + 1) * 2 * D].bitcast(F32)
        b_.sembuf = P.sb("repsem%d" % i, [128, 2], F32)
        reps.append(b_)
    off += 6 * D
    assert off <= ARENA_N
    repA, repB, repC = reps
    wrs = P.sb("wrs", [128, KC, 72], BF16)
    P.dma("pool", lambda e: e.dma_start(out=wrs.t[:], in_=wr_d.t[:, :].rearrange("(kc p) n -> p kc n", p=128)), [wr_d], [wrs], wrs)
    brep = P.sb("brep", [128, 72], F32)
    P.dma("sp", lambda e: e.dma_start(out=brep.t[:], in_=brow_d.t[0:1, :].partition_broadcast(128)), [brow_d], [brep], brep)
    rt = P.sb("rt", [128, 256], F32)
    wf = P.sb("wf", [128, 64], F32)

    def load_reps(who):
        P.dma("sp", lambda e: e.dma_start(out=repA.ap, in_=rows_d.t[ROW_SC2 + who:ROW_SC2 + who + 1, :].partition_broadcast(128)), [rows_d], [repA], repA.sembuf)
        P.dma("sp", lambda e: e.dma_start(out=repC.ap, in_=rows_d.t[ROW_GFFN:ROW_GFFN + 1, :].partition_broadcast(128)), [rows_d], [repC], repC.sembuf)
        P.op("dve", lambda e: e.scalar_tensor_tensor(out=repA.ap, in0=repA.ap, scalar=1.0, in1=repC.ap, op0=ALU.add, op1=ALU.mult), [repA, repC], [repA])
        P.dma("sp", lambda e: e.dma_start(out=repC.ap, in_=rows_d.t[ROW_SH2 + who:ROW_SH2 + who + 1, :].partition_broadcast(128)), [rows_d], [repC], repC.sembuf)
        P.dma("sp", lambda e: e.dma_start(out=repB.ap, in_=rows_d.t[ROW_GA1 + who:ROW_GA1 + who + 1, :].partition_broadcast(128)), [rows_d], [repB], repB.sembuf)

    xb, xsb, stb = xt[0], xs[0], st[0]
    for t in range(9):
        if t < 2:
            load_reps(1 if t == 0 else 0)
        P.dma("sp", lambda e: e.dma_start(out=xb.t[:], in_=x_own.t[t * 128:(t + 1) * 128, :]), [x_own], [xb], xb)
        for og in range(4):
            ps = next_psM()
            for kc in range(KC):
                P.op("pe", lambda e, kc=kc: e.matmul(ps.t[:, 0:512], lhsT=mT.ap[:, kc, t * 128:(t + 1) * 128], rhs=wo.ap[:, kc, og * 512:(og + 1) * 512],
                                                     start=(kc == 0), stop=(kc == KC - 1)), [mT, wo], [ps], inc=(kc == KC - 1))
            P.op("dve", lambda e: e.tensor_tensor(out=tA.t[:, 0:512], in0=ps.t[:, 0:512], in1=repB.ap[:, og * 512:(og + 1) * 512], op=ALU.mult), [ps, repB], [tA])
            P.op("dve", lambda e: e.tensor_tensor(out=xb.t[:, og * 512:(og + 1) * 512], in0=xb.t[:, og * 512:(og + 1) * 512], in1=tA.t[:, 0:512], op=ALU.add), [xb, tA], [xb])
        P.dma("sp", lambda e: e.dma_start(out=x1_o.t[t * 128:(t + 1) * 128, :], in_=xb.t[:]), [xb], [x1_o], xb)
        P.op("act", lambda e: e.activation(out=xsb.t[:], in_=xb.t[:], func=AF.Square, accum_out=stb.t[:, 0:1]), [xb], [xsb, stb])
        rstd_from_ss(stb.t[:, 0:1], 1, 1.0 / D, [stb], stb.t[:, 1:2], [stb])
        for og in range(4):
            sl = slice(og * 512, (og + 1) * 512)
            P.op("dve", lambda e: e.scalar_tensor_tensor(out=tA.t[:, 0:512], in0=xb.t[:, sl], scalar=stb.t[:, 1:2], in1=repA.ap[:, sl], op0=ALU.mult, op1=ALU.mult),
                 [xb, stb, repA], [tA])
            P.op("dve", lambda e: e.tensor_tensor(out=xsb.t[:, sl], in0=tA.t[:, 0:512], in1=repC.ap[:, sl], op=ALU.add), [tA, repC], [xsb])
        P.dma("sp", lambda e: e.dma_start(out=h2_o.t[t * 128:(t + 1) * 128, :], in_=xsb.t[:]), [xsb], [h2_o], xsb)
        h2T = ck.t[:].rearrange("p a b -> p (a b)").rearrange("p (k n) -> p k n", n=128)
        for half in range(2):
            for j in range(8):
                kc = half * 8 + j
                P.op("pe", lambda e: e.transpose(psT.t[:, j * 128:(j + 1) * 128], xsb.t[:, kc * 128:(kc + 1) * 128], ident), [xsb, consts], [psT], inc=(j == 7))
            copy_out("act", h2T[:, half * 8:(half + 1) * 8, :], psT.t[:, :].rearrange("p (k n) -> p k n", n=128), [psT], [ck])
        ps = next_psM()
        for kc in range(KC):
            P.op("pe", lambda e, kc=kc: e.matmul(ps.t[:, 0:72], lhsT=h2T[:, kc, :], rhs=wrs.t[:, kc, :], start=(kc == 0), stop=(kc == KC - 1)), [ck, wrs], [ps], inc=(kc == KC - 1))
        lg = rt.t[:, 0:72]
        R_ = [rt]
        P.op("dve", lambda e: e.tensor_copy(out=lg, in_=ps.t[:, 0:72]), [ps], R_)
        sm = rt.t[:, 200:216]

        def softmax8(src, dst, mcol):
            P.op("dve", lambda e: e.tensor_reduce(out=sm[:, mcol:mcol + 1], in_=src, axis=AX.X, op=ALU.max, negate=True), R_, R_)
            P.op("act", lambda e: e.activation(out=dst, in_=src, func=AF.Exp, bias=sm[:, mcol:mcol + 1], scale=1.0, accum_out=sm[:, mcol + 1:mcol + 2]), R_, R_)
            P.op("dve", lambda e: e.reciprocal(out=sm[:, mcol + 1:mcol + 2], in_=sm[:, mcol + 1:mcol + 2]), R_, R_)
            P.op("dve", lambda e: e.tensor_scalar(out=dst, in0=dst, scalar1=sm[:, mcol + 1:mcol + 2], scalar2=None, op0=ALU.mult), R_, R_)

        def onehot_max(src, dst, mcol):
            P.op("dve", lambda e: e.tensor_reduce(out=sm[:, mcol:mcol + 1], in_=src, axis=AX.X, op=ALU.max), R_, R_)
            P.op("dve", lambda e: e.tensor_scalar(out=dst, in0=src, scalar1=sm[:, mcol:mcol + 1], scalar2=None, op0=ALU.is_equal), R_, R_)
        gp, gs, og_, es, eb, ep, sel, o1, o2, tmp = (rt.t[:, 72 + 8 * i:80 + 8 * i] for i in range(10))
        softmax8(rt.t[:, 0:8], gp, 0)
        P.op("dve", lambda e: e.tensor_tensor(out=gs, in0=gp, in1=brep.t[:, 0:8], op=ALU.add), R_ + [brep], R_)
        onehot_max(gs, og_, 2)
        P.op("dve", lambda e: e.tensor_tensor(out=tmp, in0=gp, in1=og_, op=ALU.mult), R_, R_)
        P.op("dve", lambda e: e.tensor_reduce(out=sm[:, 3:4], in_=tmp, axis=AX.X, op=ALU.add), R_, R_)
        for g in range(8):
            if g == 0:
                P.op("dve", lambda e: e.tensor_scalar(out=es, in0=rt.t[:, 8:16], scalar1=og_[:, 0:1], scalar2=None, op0=ALU.mult), R_, R_)
                P.op("dve", lambda e: e.tensor_scalar(out=eb, in0=brep.t[:, 8:16], scalar1=og_[:, 0:1], scalar2=None, op0=ALU.mult), R_ + [brep], R_)
            else:
                P.op("dve", lambda e: e.scalar_tensor_tensor(out=es, in0=rt.t[:, 8 + 8 * g:16 + 8 * g], scalar=og_[:, g:g + 1], in1=es, op0=ALU.mult, op1=ALU.add), R_, R_)
                P.op("dve", lambda e: e.scalar_tensor_tensor(out=eb, in0=brep.t[:, 8 + 8 * g:16 + 8 * g], scalar=og_[:, g:g + 1], in1=eb, op0=ALU.mult, op1=ALU.add), R_ + [brep], R_)
        softmax8(es, ep, 4)
        P.op("dve", lambda e: e.tensor_tensor(out=sel, in0=ep, in1=eb, op=ALU.add), R_, R_)
        onehot_max(sel, o1, 6)
        P.op("dve", lambda e: e.scalar_tensor_tensor(out=sel, in0=o1, scalar=-1e9, in1=sel, op0=ALU.mult, op1=ALU.add), R_, R_)
        onehot_max(sel, o2, 7)
        P.op("dve", lambda e: e.tensor_tensor(out=o1, in0=o1, in1=o2, op=ALU.add), R_, R_)
        P.op("dve", lambda e: e.tensor_tensor(out=tmp, in0=ep, in1=o1, op=ALU.mult), R_, R_)
        P.op("dve", lambda e: e.tensor_reduce(out=sm[:, 8:9], in_=tmp, axis=AX.X, op=ALU.add), R_, R_)
        P.op("dve", lambda e: e.reciprocal(out=sm[:, 8:9], in_=sm[:, 8:9]), R_, R_)
        P.op("dve", lambda e: e.tensor_scalar(out=tmp, in0=tmp, scalar1=sm[:, 8:9], scalar2=sm[:, 3:4], op0=ALU.mult, op1=ALU.mult), R_, R_)
        for g in range(8):
            P.op("dve", lambda e: e.tensor_scalar(out=wf.t[:, 8 * g:8 * g + 8], in0=tmp, scalar1=og_[:, g:g + 1], scalar2=None, op0=ALU.mult), R_, [wf])
        P.dma("sp", lambda e: e.dma_start(out=wr_o.t[t * 128:(t + 1) * 128, :], in_=wf.t[:]), [wf], [wr_o], wf)
    P.finish()
    return nc


NTOK = NCORES * NOWN
BCH = 6


def build_B():
    nc = bass.Bass("TRN2", target_bir_lowering=False)
    P = Prog(nc)
    h2 = P.dram("h2", [NTOK, D], BF16, "ExternalInput")
    wsel_d = P.dram("wsel", [NTOK, 8], F32, "ExternalInput")
    wg_d = P.dram("wg", [8, D, 512], F32, "ExternalInput")
    wu_d = P.dram("wu", [8, D, 512], F32, "ExternalInput")
    wd_d = P.dram("wd", [8, 512, D], F32, "ExternalInput")
    ident_d = P.dram("ident", [128, 128], BF16, "ExternalInput")
    out = P.dram("contrib", [NTOK, D], BF16, "ExternalOutput")
    ntile = NTOK // 128
    ident = P.sb("ident_sb", [128, 128], BF16)
    wsel = P.sb("wsel_sb", [128, ntile, 8], F32)
    h2T = P.sb("h2T", [128, KC, BCH * 128], BF16)
    yacc = P.sb("yacc", [128, BCH, D], F32)
    wg = [P.sb("wg%d" % i, [128, KC, 512], BF16) for i in range(2)]
    wu = [P.sb("wu%d" % i, [128, KC, 512], BF16) for i in range(2)]
    wd = [P.sb("wd%d" % i, [128, 4, D], BF16) for i in range(2)]
    ht = P.sb("ht", [128, D], BF16)
    yb = P.sb("yb", [128, D], BF16)
    tA = P.sb("tA", [128, 512], F32)
    tB = P.sb("tB", [128, 512], F32)
    hb = P.sb("hb", [128, 512], BF16)
    hT4 = P.sb("hT4", [128, 4, 128], BF16)
    ps1 = [P.ps("ps1_%d" % i, [128, 512], F32) for i in range(2)]
    ps2 = [P.ps("ps2_%d" % i, [128, 512], F32) for i in range(2)]
    ps3 = [P.ps("ps3_%d" % i, [128, 512], F32) for i in range(2)]
    psT = P.ps("psT", [128, 1024], BF16)
    P.dma("sp", lambda e: e.dma_start(out=ident.t[:], in_=ident_d.t[:, :]), [ident_d], [ident], ident)
    P.dma("sp", lambda e: e.dma_start(out=wsel.t[:], in_=wsel_d.t[:, :].rearrange("(t p) e -> p t e", p=128)), [wsel_d], [wsel], wsel)
    wi = 0
    k3 = 0
    for ch in range(ntile // BCH):
        for tt in range(BCH):
            t = ch * BCH + tt
            P.dma("sp", lambda e: e.dma_start(out=ht.t[:], in_=h2.t[t * 128:(t + 1) * 128, :]), [h2], [ht], ht)
            for half in range(2):
                for j in range(8):
                    kc = half * 8 + j
                    P.op("pe", lambda e: e.transpose(psT.t[:, j * 128:(j + 1) * 128], ht.t[:, kc * 128:(kc + 1) * 128], ident.t[:]), [ht, ident], [psT], inc=(j == 7))
                P.op("act", lambda e: e.copy(out=h2T.t[:, half * 8:(half + 1) * 8, tt * 128:(tt + 1) * 128], in_=psT.t[:, :].rearrange("p (k n) -> p k n", n=128)), [psT], [h2T])
        for ex in range(8):
            g_, u_, d_ = wg[wi % 2], wu[wi % 2], wd[wi % 2]
            wi += 1
            P.dma("pool", lambda e: e.dma_start(out=g_.t[:], in_=wg_d.t[ex, :, :].rearrange("(kc p) n -> p kc n", p=128)), [wg_d], [g_], g_)
            P.dma("pool", lambda e: e.dma_start(out=u_.t[:], in_=wu_d.t[ex, :, :].rearrange("(kc p) n -> p kc n", p=128)), [wu_d], [u_], u_)
            P.dma("pool", lambda e: e.dma_start(out=d_.t[:], in_=wd_d.t[ex, :, :].rearrange("(c p) n -> p c n", p=128)), [wd_d], [d_], d_)
            for tt in range(BCH):
                t = ch * BCH + tt
                p1, p2 = ps1[tt % 2], ps2[tt % 2]
                for kc in range(KC):
                    P.op("pe", lambda e: e.matmul(p1.t[:, :], lhsT=h2T.t[:, kc, tt * 128:(tt + 1) * 128], rhs=g_.t[:, kc, :], start=(kc == 0), stop=(kc == KC - 1)),
                         [h2T, g_], [p1], inc=(kc == KC - 1))
                for kc in range(KC):
                    P.op("pe", lambda e: e.matmul(p2.t[:, :], lhsT=h2T.t[:, kc, tt * 128:(tt + 1) * 128], rhs=u_.t[:, kc, :], start=(kc == 0), stop=(kc == KC - 1)),
                         [h2T, u_], [p2], inc=(kc == KC - 1))
                P.op("act", lambda e: e.activation(out=tA.t[:], in_=p1.t[:, :], func=AF.Sigmoid), [p1], [tA])
                P.op("dve", lambda e: e.tensor_tensor(out=tB.t[:], in0=p1.t[:, :], in1=tA.t[:], op=ALU.mult), [p1, tA], [tB])
                P.op("dve", lambda e: e.scalar_tensor_tensor(out=hb.t[:], in0=tB.t[:], scalar=wsel.t[:, t, ex:ex + 1], in1=p2.t[:, :], op0=ALU.mult, op1=ALU.mult),
                     [tB, wsel, p2], [hb])
                for j in range(4):
                    P.op("pe", lambda e: e.transpose(psT.t[:, j * 128:(j + 1) * 128], hb.t[:, j * 128:(j + 1) * 128], ident.t[:]), [hb, ident], [psT], inc=(j == 3))
                P.op("act", lambda e: e.copy(out=hT4.t[:], in_=psT.t[:, 0:512].rearrange("p (k n) -> p k n", n=128)), [psT], [hT4])
                for og in range(4):
                    p3 = ps3[k3 % 2]
                    k3 += 1
                    for hc in range(4):
                        P.op("pe", lambda e: e.matmul(p3.t[:, :], lhsT=hT4.t[:, hc, :], rhs=d_.t[:, hc, og * 512:(og + 1) * 512], start=(hc == 0), stop=(hc == 3)),
                             [hT4, d_], [p3], inc=(hc == 3))
                    ya = yacc.t[:, tt, og * 512:(og + 1) * 512]
                    if ex == 0:
                        P.op("dve", lambda e: e.tensor_copy(out=ya, in_=p3.t[:, :]), [p3], [yacc])
                    else:
                        P.op("dve", lambda e: e.tensor_tensor(out=ya, in0=ya, in1=p3.t[:, :], op=ALU.add), [p3, yacc], [yacc])
        for tt in range(BCH):
            t = ch * BCH + tt
            P.op("act", lambda e: e.copy(out=yb.t[:], in_=yacc.t[:, tt, :]), [yacc], [yb])
            P.dma("sp", lambda e: e.dma_start(out=out.t[t * 128:(t + 1) * 128, :], in_=yb.t[:]), [yb], [out], yb)
    P.finish()
    return nc


CAPB = 8
NBLK = 8 * CAPB
NSLOT = NBLK * 128
BIGF = 1.0e6


def build_B2():
    nc = bass.Bass("TRN2", target_bir_lowering=False)
    P = Prog(nc)
    ntile = NTOK // 128
    h2 = P.dram("h2", [NTOK, D], BF16, "ExternalInput")
    wsel_d = P.dram("wsel", [NTOK, 8], F32, "ExternalInput")
    wg_d = P.dram("wg", [8, D, 512], F32, "ExternalInput")
    wu_d = P.dram("wu", [8, D, 512], F32, "ExternalInput")
    wd_d = P.dram("wd", [8, 512, D], F32, "ExternalInput")
    cst_d = P.dram("cst", [128, 3, 128], BF16, "ExternalInput")
    thr_d = P.dram("thr", [128, 8], F32, "ExternalInput")
    out = P.dram("contrib", [NTOK, D], BF16, "ExternalOutput")
    xd = P.dram("xd", [NSLOT, D], BF16)
    yd = P.dram("yd", [NSLOT, D], BF16)

    cst = P.sb("cst_sb", [128, 3, 128], BF16)
    ident, ones, utri = (cst.t[:, i, :] for i in range(3))
    thr = P.sb("thr_sb", [128, 8], F32)
    Wt = P.sb("Wt", [128, ntile, 8], F32)
    Mb = P.sb("Mb", [128, ntile * 8], BF16)
    Mf = P.sb("Mf", [128, ntile, 8], F32)
    S = P.sb("S", [128, ntile, 8], F32)
    tot = P.sb("tot", [128, ntile, 8], F32)
    carry = P.sb("carry", [128, ntile + 1, 8], F32)
    slA = P.sb("slA", [128, ntile], F32)
    slB = P.sb("slB", [128, ntile], F32)
    wA = P.sb("wA", [128, ntile], F32)
    wB = P.sb("wB", [128, ntile], F32)
    slAi = P.sb("slAi", [128, ntile], I32)
    slBi = P.sb("slBi", [128, ntile], I32)
    ht = [P.sb("ht%d" % i, [128, D], BF16) for i in range(2)]
    ya = [P.sb("ya%d" % i, [128, D], BF16) for i in range(2)]
    yb = [P.sb("yb%d" % i, [128, D], BF16) for i in range(2)]
    yo = [P.sb("yo%d" % i, [128, D], BF16) for i in range(2)]
    xTs = [P.sb("xT%d" % i, [128, KC, 128], BF16) for i in range(2)]
    wg = [P.sb("wgS%d" % i, [128, KC, 512], BF16) for i in range(2)]
    wu = [P.sb("wuS%d" % i, [128, KC, 512], BF16) for i in range(2)]
    wd = [P.sb("wdS%d" % i, [128, 4, D], BF16) for i in range(2)]
    tAs = [P.sb("tA%d" % i, [128, 512], F32) for i in range(2)]
    tBs = [P.sb("tB%d" % i, [128, 512], F32) for i in range(2)]
    tC = P.sb("tC", [128, D], F32)
    hbs = [P.sb("hb%d" % i, [128, 512], BF16) for i in range(2)]
    hT4s = [P.sb("hT4%d" % i, [128, 4, 128], BF16) for i in range(2)]
    ps1 = [P.ps("ps1_%d" % i, [128, 512], F32) for i in range(2)]
    ps2 = [P.ps("ps2_%d" % i, [128, 512], F32) for i in range(2)]
    ps3 = [P.ps("ps3_%d" % i, [128, 512], F32) for i in range(2)]
    psTs = [P.ps("psT%d" % i, [128, 1024], BF16) for i in range(2)]
    psX = ps3[0]
    npt = [0]

    def next_psT():
        npt[0] += 1
        return psTs[npt[0] % 2]
    bnd = nc.gpsimd.to_reg(NSLOT - 1)

    P.dma("sp", lambda e: e.dma_start(out=cst.t[:], in_=cst_d.t[:, :, :]), [cst_d], [cst], cst)
    P.dma("sp", lambda e: e.dma_start(out=thr.t[:], in_=thr_d.t[:, :]), [thr_d], [thr], thr)
    P.dma("sp", lambda e: e.dma_start(out=Wt.t[:], in_=wsel_d.t[:, :].rearrange("(t p) e -> p t e", p=128)), [wsel_d], [Wt], Wt)

    def load_expert(ex):
        g_, u_, d_ = wg[ex % 2], wu[ex % 2], wd[ex % 2]
        P.dma("pool", lambda e: e.dma_start(out=g_.t[:], in_=wg_d.t[ex, :, :].rearrange("(kc p) n -> p kc n", p=128)), [wg_d], [g_], g_)
        P.dma("pool", lambda e: e.dma_start(out=u_.t[:], in_=wu_d.t[ex, :, :].rearrange("(kc p) n -> p kc n", p=128)), [wu_d], [u_], u_)
        P.dma("pool", lambda e: e.dma_start(out=d_.t[:], in_=wd_d.t[ex, :, :].rearrange("(c p) n -> p c n", p=128)), [wd_d], [d_], d_)
    load_expert(0)
    load_expert(1)
    Wt2 = Wt.t[:].rearrange("p t e -> p (t e)")
    Mf2 = Mf.t[:].rearrange("p t e -> p (t e)")
    S2 = S.t[:].rearrange("p t e -> p (t e)")
    tot2 = tot.t[:].rearrange("p t e -> p (t e)")
    P.op("dve", lambda e: e.tensor_single_scalar(out=Mf2, in_=Wt2, scalar=0.0, op=ALU.is_gt), [Wt], [Mf])
    P.op("dve", lambda e: e.tensor_copy(out=Mb.t[:], in_=Mf2), [Mf], [Mb])
    for (c0, cn) in [(0, 512), (512, 64)]:
        P.op("pe", lambda e: e.matmul(psX.t[:, 0:cn], lhsT=utri, rhs=Mb.t[:, c0:c0 + cn], start=True, stop=True), [cst, Mb], [psX])
        P.op("dve", lambda e: e.tensor_copy(out=S2[:, c0:c0 + cn], in_=psX.t[:, 0:cn]), [psX], [S])
        P.op("pe", lambda e: e.matmul(psX.t[:, 0:cn], lhsT=ones, rhs=Mb.t[:, c0:c0 + cn], start=True, stop=True), [cst, Mb], [psX])
        P.op("dve", lambda e: e.tensor_copy(out=tot2[:, c0:c0 + cn], in_=psX.t[:, 0:cn]), [psX], [tot])
    P.op("dve", lambda e: e.memset(carry.t[:, 0, :], 0.0), [], [carry])
    for i in range(ntile):
        P.op("dve", lambda e: e.tensor_tensor(out=carry.t[:, i + 1, :], in0=carry.t[:, i, :], in1=tot.t[:, i, :], op=ALU.add), [carry, tot], [carry])
    P.op("dve", lambda e: e.tensor_tensor(out=S.t[:], in0=S.t[:], in1=carry.t[:, 0:ntile, :], op=ALU.add), [S, carry], [S])
    P.op("dve", lambda e: e.tensor_single_scalar(out=tot2, in_=S2, scalar=float(CAPB * 128), op=ALU.is_lt), [S], [tot])
    P.op("dve", lambda e: e.tensor_tensor(out=Mf2, in0=Mf2, in1=tot2, op=ALU.mult), [Mf, tot], [Mf])
    for i in range(ntile):
        P.op("dve", lambda e: e.tensor_tensor(out=S.t[:, i, :], in0=S.t[:, i, :], in1=thr.t[:, :], op=ALU.add), [S, thr], [S])
    P.op("dve", lambda e: e.tensor_scalar(out=tot2, in0=S2, scalar1=-BIGF, scalar2=None, op0=ALU.add), [S], [tot])
    P.op("dve", lambda e: e.tensor_tensor(out=tot2, in0=tot2, in1=Mf2, op=ALU.mult), [tot, Mf], [tot])
    P.op("dve", lambda e: e.tensor_scalar(out=tot2, in0=tot2, scalar1=BIGF, scalar2=None, op0=ALU.add), [tot], [tot])
    P.op("dve", lambda e: e.tensor_reduce(out=slA.t[:], in_=tot.t[:], axis=AX.X, op=ALU.min), [tot], [slA])
    P.op("dve", lambda e: e.tensor_scalar(out=tot2, in0=S2, scalar1=1.0, scalar2=None, op0=ALU.add), [S], [tot])
    P.op("dve", lambda e: e.tensor_tensor(out=tot2, in0=tot2, in1=Mf2, op=ALU.mult), [tot, Mf], [tot])
    P.op("dve", lambda e: e.tensor_scalar(out=tot2, in0=tot2, scalar1=-1.0, scalar2=None, op0=ALU.add), [tot], [tot])
    P.op("dve", lambda e: e.tensor_reduce(out=slB.t[:], in_=tot.t[:], axis=AX.X, op=ALU.max), [tot], [slB])
    for (sl, wv) in ((slA, wA), (slB, wB)):
        for i in range(ntile):
            P.op("dve", lambda e: e.tensor_scalar(out=tot.t[:, i, :], in0=S.t[:, i, :], scalar1=sl.t[:, i:i + 1], scalar2=None, op0=ALU.is_equal), [S, sl], [tot])
        P.op("dve", lambda e: e.tensor_tensor(out=tot2, in0=tot2, in1=Wt2, op=ALU.mult), [tot, Wt], [tot])
        P.op("dve", lambda e: e.tensor_reduce(out=wv.t[:], in_=tot.t[:], axis=AX.X, op=ALU.add), [tot], [wv])
    P.op("dve", lambda e: e.tensor_copy(out=slAi.t[:], in_=slA.t[:]), [slA], [slAi])
    P.op("dve", lambda e: e.tensor_copy(out=slBi.t[:], in_=slB.t[:]), [slB], [slBi])

    for i in range(ntile):
        hb_ = ht[i % 2]
        P.dma("sp", lambda e: e.dma_start(out=hb_.t[:], in_=h2.t[i * 128:(i + 1) * 128, :]), [h2], [hb_], hb_)
        for sli in (slAi, slBi):
            P.dma("pool", lambda e: e.indirect_dma_start(
                out=xd.t[:, :], out_offset=bass.IndirectOffsetOnAxis(ap=sli.t[:, i:i + 1], axis=0),
                in_=hb_.t[:, :], in_offset=None, bounds_check=bnd, oob_is_err=False), [hb_, sli], [xd], hb_)

    k3 = [0]

    def st_load(b):
        xb_ = ht[b % 2]
        P.dma("sp", lambda e: e.dma_start(out=xb_.t[:], in_=xd.t[b * 128:(b + 1) * 128, :]), [xd], [xb_], xb_)

    def st_trx(b):
        xb_, xT = ht[b % 2], xTs[b % 2]
        for half in range(2):
            psT = next_psT()
            for j in range(8):
                kc = half * 8 + j
                P.op("pe", lambda e: e.transpose(psT.t[:, j * 128:(j + 1) * 128], xb_.t[:, kc * 128:(kc + 1) * 128], ident), [xb_, cst], [psT], inc=(j == 7))
            P.op("act", lambda e: e.copy(out=xT.t[:, half * 8:(half + 1) * 8, :], in_=psT.t[:, :].rearrange("p (k n) -> p k n", n=128)), [psT], [xT])

    def st_gu(b):
        ex = b // CAPB
        if b % CAPB == 0 and ex >= 2:
            load_expert(ex)
        g_, u_ = wg[ex % 2], wu[ex % 2]
        xT, tA, tB, hb = xTs[b % 2], tAs[b % 2], tBs[b % 2], hbs[b % 2]
        p1, p2 = ps1[b % 2], ps2[b % 2]
        for kc in range(KC):
            P.op("pe", lambda e: e.matmul(p1.t[:, :], lhsT=xT.t[:, kc, :], rhs=g_.t[:, kc, :], start=(kc == 0), stop=(kc == KC - 1)), [xT, g_], [p1], inc=(kc == KC - 1))
        for kc in range(KC):
            P.op("pe", lambda e: e.matmul(p2.t[:, :], lhsT=xT.t[:, kc, :], rhs=u_.t[:, kc, :], start=(kc == 0), stop=(kc == KC - 1)), [xT, u_], [p2], inc=(kc == KC - 1))
        P.op("act", lambda e: e.activation(out=tA.t[:], in_=p1.t[:, :], func=AF.Sigmoid), [p1], [tA])
        P.op("dve", lambda e: e.tensor_tensor(out=tB.t[:], in0=p1.t[:, :], in1=tA.t[:], op=ALU.mult), [p1, tA], [tB])
        P.op("dve", lambda e: e.tensor_tensor(out=hb.t[:], in0=tB.t[:], in1=p2.t[:, :], op=ALU.mult), [tB, p2], [hb])

    def st_trh(b):
        hb, hT4 = hbs[b % 2], hT4s[b % 2]
        psT = next_psT()
        for j in range(4):
            P.op("pe", lambda e: e.transpose(psT.t[:, j * 128:(j + 1) * 128], hb.t[:, j * 128:(j + 1) * 128], ident), [hb, cst], [psT], inc=(j == 3))
        P.op("act", lambda e: e.copy(out=hT4.t[:], in_=psT.t[:, 0:512].rearrange("p (k n) -> p k n", n=128)), [psT], [hT4])

    def st_down(b):
        ex = b // CAPB
        d_ = wd[ex % 2]
        hT4 = hT4s[b % 2]
        yo_ = yo[b % 2]
        for og in range(4):
            p3 = ps3[k3[0] % 2]
            k3[0] += 1
            for hc in range(4):
                P.op("pe", lambda e: e.matmul(p3.t[:, :], lhsT=hT4.t[:, hc, :], rhs=d_.t[:, hc, og * 512:(og + 1) * 512], start=(hc == 0), stop=(hc == 3)), [hT4, d_], [p3], inc=(hc == 3))
            if og % 2 == 0:
                P.op("act", lambda e: e.copy(out=yo_.t[:, og * 512:(og + 1) * 512], in_=p3.t[:, :]), [p3], [yo_])
            else:
                P.op("dve", lambda e: e.tensor_copy(out=yo_.t[:, og * 512:(og + 1) * 512], in_=p3.t[:, :]), [p3], [yo_])
        P.dma("sp", lambda e: e.dma_start(out=yd.t[b * 128:(b + 1) * 128, :], in_=yo_.t[:]), [yo_], [yd], yo_)

    st_load(0)
    st_load(1)
    st_trx(0)
    for b in range(NBLK):
        st_gu(b)
        if b + 1 < NBLK:
            st_trx(b + 1)
        st_trh(b)
        st_down(b)
        if b + 2 < NBLK:
            st_load(b + 2)

    for i in range(ntile):
        ya_, yb_, yo_ = ya[i % 2], yb[i % 2], yo[i % 2]
        if i < 2:
            P.op("pool", lambda e: e.memset(ya_.t[:], 0.0), [], [ya_])
            P.op("pool", lambda e: e.memset(yb_.t[:], 0.0), [], [yb_])
        P.dma("pool", lambda e: e.indirect_dma_start(out=ya_.t[:, :], out_offset=None, in_=yd.t[:, :],
                                                     in_offset=bass.IndirectOffsetOnAxis(ap=slAi.t[:, i:i + 1], axis=0),
                                                     bounds_check=bnd, oob_is_err=False), [yd, slAi], [ya_], ya_)
        P.dma("pool", lambda e: e.indirect_dma_start(out=yb_.t[:, :], out_offset=None, in_=yd.t[:, :],
                                                     in_offset=bass.IndirectOffsetOnAxis(ap=slBi.t[:, i:i + 1], axis=0),
                                                     bounds_check=bnd, oob_is_err=False), [yd, slBi], [yb_], yb_)
        P.op("act", lambda e: e.activation(out=tC.t[:], in_=ya_.t[:], func=AF.Copy, scale=wA.t[:, i:i + 1]), [ya_, wA], [tC])
        P.op("dve", lambda e: e.scalar_tensor_tensor(out=yo_.t[:], in0=yb_.t[:], scalar=wB.t[:, i:i + 1], in1=tC.t[:], op0=ALU.mult, op1=ALU.add), [yb_, wB, tC], [yo_])
        P.dma("sp", lambda e: e.dma_start(out=out.t[i * 128:(i + 1) * 128, :], in_=yo_.t[:]), [yo_], [out], yo_)
    P.finish()
    return nc


def _b2_consts():
    import ml_dtypes
    c = np.zeros((128, 3, 128), np.float32)
    c[:, 0, :] = np.eye(128)
    c[:, 1, :] = 1.0
    c[:, 2, :] = np.triu(np.ones((128, 128), np.float32), 1)
    thr = np.zeros((128, 8), np.float32)
    thr[:, :] = (np.arange(8) * CAPB * 128)[None, :]
    return c.astype(ml_dtypes.bfloat16), thr


def build_C(final):
    nc = bass.Bass("TRN2", target_bir_lowering=False)
    P = Prog(nc)
    x1 = P.dram("x1", [NOWN, D], F32, "ExternalInput")
    cb = P.dram("cb", [8, NOWN, D], BF16, "ExternalInput")
    rows = P.dram("rows", [3, D], F32, "ExternalInput")
    out = P.dram("x2", [NOWN, D], F32, "ExternalOutput")
    xt = [P.sb("xt%d" % i, [128, D], F32) for i in range(2)]
    cbs = [P.sb("cbs%d" % i, [128, 8, D], BF16) for i in range(2)]
    acc = P.sb("acc", [128, D], F32)
    sq = P.sb("sq", [128, D], BF16)
    st = P.sb("st", [128, 4], F32)
    rep = [P.sb("rep%d" % i, [128, D], F32) for i in range(3)]
    for i in range(3):
        P.dma("sp", lambda e: e.dma_start(out=rep[i].t[:], in_=rows.t[i:i + 1, :].partition_broadcast(128)), [rows], [rep[i]], rep[i])
    for t in range(9):
        xb, cbb = xt[t % 2], cbs[t % 2]
        who = 1 if t == 0 else 0
        P.dma("sp", lambda e: e.dma_start(out=xb.t[:], in_=x1.t[t * 128:(t + 1) * 128, :]), [x1], [xb], xb)
        P.dma("sp", lambda e: e.dma_start(out=cbb.t[:], in_=cb.t[:, t * 128:(t + 1) * 128, :].rearrange("g p d -> p g d")), [cb], [cbb], cbb)
        P.op("dve", lambda e: e.tensor_tensor(out=acc.t[:], in0=cbb.t[:, 0, :], in1=cbb.t[:, 1, :], op=ALU.add), [cbb], [acc])
        for g in range(2, 8):
            P.op("dve", lambda e: e.tensor_tensor(out=acc.t[:], in0=acc.t[:], in1=cbb.t[:, g, :], op=ALU.add), [cbb, acc], [acc])
        P.op("dve", lambda e: e.tensor_tensor(out=acc.t[:], in0=acc.t[:], in1=rep[who].t[:], op=ALU.mult), [acc, rep[who]], [acc])
        P.op("dve", lambda e: e.tensor_tensor(out=xb.t[:], in0=xb.t[:], in1=acc.t[:], op=ALU.add), [xb, acc], [xb])
        if final:
            P.op("act", lambda e: e.activation(out=sq.t[:], in_=xb.t[:], func=AF.Square, accum_out=st.t[:, 0:1]), [xb], [sq, st])
            P.op("dve", lambda e: e.tensor_scalar(out=st.t[:, 1:2], in0=st.t[:, 0:1], scalar1=1.0 / D, scalar2=EPS, op0=ALU.mult, op1=ALU.add), [st], [st])
            P.op("act", lambda e: e.activation(out=st.t[:, 1:2], in_=st.t[:, 1:2], func=AF.Sqrt), [st], [st])
            P.op("dve", lambda e: e.reciprocal(out=st.t[:, 1:2], in_=st.t[:, 1:2]), [st], [st])
            P.op("dve", lambda e: e.scalar_tensor_tensor(out=xb.t[:], in0=xb.t[:], scalar=st.t[:, 1:2], in1=rep[2].t[:], op0=ALU.mult, op1=ALU.mult), [xb, st, rep[2]], [xb])
        P.dma("sp", lambda e: e.dma_start(out=out.t[t * 128:(t + 1) * 128, :], in_=xb.t[:]), [xb], [out], xb)
    P.finish()
    return nc


def _fm(v, n):
    return np.ascontiguousarray(np.asarray(v, np.float32).reshape(n, 128).T)


def _rope_tab(rot_dim):
    q = rot_dim // 4
    t = np.arange(2048)
    rows = (t // 64).astype(np.float32)
    cols = (t % 64).astype(np.float32)
    inv = (np.float32(10000.0) ** (-np.arange(q, dtype=np.float32) / np.float32(q))).astype(np.float32)
    ar = rows[None, :] * inv[:, None]
    ac = cols[None, :] * inv[:, None]
    ang = np.concatenate([ar, ar, ac, ac], 0).astype(np.float32)
    C = np.cos(ang).astype(np.float32)
    S = np.sin(ang).astype(np.float32)
    R = np.zeros((rot_dim, rot_dim), np.float32)
    for base in (0, 2 * q):
        for i in range(q):
            R[base + i, base + i + q] = -1.0
            R[base + i + q, base + i] = 1.0
    return C, S, R


def _consts():
    import ml_dtypes
    c = np.zeros((128, 4, 128), np.float32)
    c[:, 0, :] = np.eye(128)
    c[:, 1, :] = 1.0
    _, _, RB = _rope_tab(128)
    _, _, RA = _rope_tab(64)
    c[:, 2, :] = RB.T
    c[:64, 3, :64] = RA.T
    return c.astype(ml_dtypes.bfloat16)


def _nab(rpb):
    out = np.full((128, 4, 15, 64), -30000.0, np.float32)
    qc = np.arange(64)
    cs = np.clip(qc - 8, 0, 48)
    kc = np.arange(64)
    col_in = (kc[:, None] >= cs[None, :]) & (kc[:, None] < cs[None, :] + 16)
    dc = np.clip(kc[:, None] - qc[None, :] + 15, 0, 30)
    for a in range(2):
        for m in range(15):
            dr = m - 7 + a
            if dr < -7 or dr > 7:
                continue
            for h in range(4):
                vals = rpb[h, dr + 7][dc]
                out[a * 64:(a + 1) * 64, h, m, :] = np.where(col_in, vals, np.float32(-30000.0))
    return out


def _ind(hf):
    out = np.zeros((128, 16, 6), np.float32)
    base = 16 * hf
    for lr in range(16):
        r = base + lr
        r0 = min(max(r - 4, 0), 24)
        Rs = lr if lr <= 12 else 12
        R0 = Rs - Rs % 2
        for s in range(6):
            for a in range(2):
                ab = base - 4 + R0 + 2 * s + a
                if 0 <= ab <= 31 and r0 <= ab <= r0 + 7:
                    out[a * 64:(a + 1) * 64, lr, s] = 1.0
    return out


_CACHE = {}


def _prog(name, fn):
    if name not in _CACHE:
        _CACHE[name] = fn()
    return _CACHE[name]


def kernel(x, c, ctx, c_ctx, w_mod, b_mod, g_mix, g_ffn, w_in, w_uq, g_qa, w_ukv, g_kva, g_qn, g_kn,
           rpb, w_conv, w_branch, w_o, w_group, b_group, w_router, b_router, w_gate_e, w_up_e, w_down_e, g_final):
    f32 = np.float32
    cores = list(range(NCORES))
    x = np.asarray(x, f32)
    ctx = np.asarray(ctx, f32)
    c5 = np.concatenate([np.asarray(c, f32), np.asarray(c_ctx, f32)[None]], 0)
    cT = np.ascontiguousarray(c5.T.reshape(KC, 128, 5).transpose(1, 0, 2))
    ims = [{"cT": cT, "wm": np.ascontiguousarray(w_mod[:, :, i * MCOL:(i + 1) * MCOL]), "bm": np.ascontiguousarray(b_mod[:, i * MCOL:(i + 1) * MCOL])}
           for i in cores]
    res = run_bass_kernel_spmd(_prog("M", build_M), ims, core_ids=cores)
    mod = np.concatenate([r["mod"] for r in res.results], axis=2)
    consts = _consts()
    CA_, SA_, _ = _rope_tab(64)
    CB_, SB_, _ = _rope_tab(128)
    xlat, xctx = x, ctx
    for l in range(2):
        nab = _nab(np.asarray(rpb[l], f32))
        wr = np.ascontiguousarray(np.concatenate([w_group[l], w_router[l]], 1), f32)
        brow = np.concatenate([b_group[l], b_router[l]])[None].astype(f32)
        ims = []
        for cid in cores:
            b, hf = cid // 2, cid % 2
            o = 1 - hf
            x_own = np.concatenate([xctx[b, hf * 128:(hf + 1) * 128], xlat[b, hf * 1024:(hf + 1) * 1024]], 0)
            x_oth = np.concatenate([xctx[b, o * 128:(o + 1) * 128], xlat[b, o * 1024:(o + 1) * 1024]], 0)
            ml = mod[l, b].reshape(6, D)
            mc = mod[l, 4].reshape(6, D)
            fm = np.zeros((128, FM_N), f32)
            fm[:, FM_GMIX:FM_GMIX + 16] = _fm(g_mix[l], 16)
            fm[:, FM_SC1:FM_SC1 + 16] = _fm(ml[1], 16)
            fm[:, FM_SC1 + 16:FM_SC1 + 32] = _fm(mc[1], 16)
            fm[:, FM_SH1:FM_SH1 + 16] = _fm(ml[0], 16)
            fm[:, FM_SH1 + 16:FM_SH1 + 32] = _fm(mc[0], 16)
            fm[:, FM_GQA:FM_GQA + 4] = _fm(g_qa[l], 4)
            fm[:, FM_GKVA:FM_GKVA + 4] = _fm(g_kva[l], 4)
            fm[:, FM_GQN] = g_qn[l]
            fm[:, FM_GKN] = g_kn[l]
            for ci in range(4):
                for tap in range(3):
                    fm[:, FM_WCONV + ci * 3 + tap] = w_conv[l, tap, ci * 128:(ci + 1) * 128]
            fm[:, FM_FLAGB] = 1.0 if hf == 1 else 0.0
            fm[:, FM_FLAGA] = 1.0 if hf == 0 else 0.0
            rows = np.stack([np.asarray(g_ffn[l], f32), ml[4], mc[4], ml[3], mc[3], ml[2], mc[2]]).astype(f32)
            so, sn = slice(hf * 1024, (hf + 1) * 1024), slice(o * 1024, (o + 1) * 1024)
            ropeA = np.ascontiguousarray(np.stack([np.stack([CA_[:, so], CA_[:, sn]], 1), np.stack([SA_[:, so], SA_[:, sn]], 1)], 1))
            ropeB = np.ascontiguousarray(np.stack([np.stack([CB_[:, so], CB_[:, sn]], 1), np.stack([SB_[:, so], SB_[:, sn]], 1)], 1))
            ims.append({"x_own": x_own, "x_oth": x_oth, "w_in": w_in[l], "w_uq": w_uq[l], "w_ukv": w_ukv[l], "fm": fm, "rows": rows,
                        "ropeA": ropeA, "ropeB": ropeB, "consts": consts, "nab": nab, "ind": _ind(hf), "wr": wr, "brow": brow,
                        "w_branch": w_branch[l], "w_o": w_o[l]})
        resA = run_bass_kernel_spmd(_prog("A", build_A), ims, core_ids=cores).results
        h2_all = np.concatenate([r["h2"] for r in resA], 0)
        wr_all = np.concatenate([r["wrout"] for r in resA], 0)
        cstB, thrB = _b2_consts()
        ims = [{"h2": h2_all, "wsel": np.ascontiguousarray(wr_all[:, 8 * g:8 * g + 8]), "wg": w_gate_e[l, 8 * g:8 * g + 8], "wu": w_up_e[l, 8 * g:8 * g + 8],
                "wd": w_down_e[l, 8 * g:8 * g + 8], "cst": cstB, "thr": thrB} for g in cores]
        resB = run_bass_kernel_spmd(_prog("B2", build_B2), ims, core_ids=cores).results
        ims = []
        for cid in cores:
            b = cid // 2
            cbk = np.stack([resB[g]["contrib"][cid * NOWN:(cid + 1) * NOWN] for g in cores], 0)
            rows = np.stack([mod[l, b].reshape(6, D)[5], mod[l, 4].reshape(6, D)[5], np.asarray(g_final, f32)]).astype(f32)
            ims.append({"x1": resA[cid]["x1"], "cb": cbk, "rows": rows})
        final = (l == 1)
        resC = run_bass_kernel_spmd(_prog("C%d" % final, lambda: build_C(final)), ims, core_ids=cores).results
        nl = np.empty_like(xlat)
        ncx = np.empty_like(xctx)
        for cid in cores:
            b, hf = cid // 2, cid % 2
            ncx[b, hf * 128:(hf + 1) * 128] = resC[cid]["x2"][0:128]
            nl[b, hf * 1024:(hf + 1) * 1024] = resC[cid]["x2"][128:]
        xlat, xctx = nl, ncx
    return xlat
```
